# Optimizing a Trainium2 kernel written in Bass

```python
import math
import jax
import jax.numpy as jnp
from jax import lax
import numpy as np

D_MODEL = 1024
BATCH = 8
SEQ = 8192
DEPTH = 2

GRID_W = 64
CTX_LEN = 256

RWKV_HEAD = 64
RWKV_WIDTH = D_MODEL
RWKV_HEADS = RWKV_WIDTH // RWKV_HEAD
LORA_W = 64
LORA_A = 64
LORA_G = 160
GN_EPS = RWKV_HEAD * 1e-5

CONV_CH = D_MODEL
CONV_K = 31

PEER_HEADS = 8
PEER_NKEYS = 128
PEER_EXPERTS = PEER_NKEYS * PEER_NKEYS
PEER_QDIM = 256
PEER_HALF = PEER_QDIM // 2
PEER_TOPK = 16
PEER_CHUNK = 256

N_BRANCH = 2
NORM_EPS = 1e-6
LN_EPS = 1e-5

P_RWKV = 3 * RWKV_WIDTH + 2 * LORA_W + 2 * LORA_A + LORA_G
P_CONV = 2 * CONV_CH
P_GATE = N_BRANCH * D_MODEL
P_IN = P_RWKV + P_CONV + P_GATE
RWKV_SPLITS = (RWKV_WIDTH, 2 * RWKV_WIDTH, 3 * RWKV_WIDTH,
               3 * RWKV_WIDTH + LORA_W, 3 * RWKV_WIDTH + 2 * LORA_W,
               3 * RWKV_WIDTH + 2 * LORA_W + LORA_A, 3 * RWKV_WIDTH + 2 * LORA_W + 2 * LORA_A)

kernel_name = 'hybrid_rwkv7_conformer_peer_flow_block'


def rmsnorm(x, g):
    xf = x.astype(jnp.float32)
    y = xf * lax.rsqrt(jnp.mean(xf * xf, axis=-1, keepdims=True) + NORM_EPS)
    return (y * g.astype(jnp.float32)).astype(x.dtype)


def layernorm(x, g, b):
    xf = x.astype(jnp.float32)
    mu = jnp.mean(xf, axis=-1, keepdims=True)
    var = jnp.mean(jnp.square(xf - mu), axis=-1, keepdims=True)
    return ((xf - mu) * lax.rsqrt(var + LN_EPS) * g + b).astype(x.dtype)


def modulate(x, g, shift, scale):
    return rmsnorm(x, g) * (1 + scale) + shift


def token_shift(z, mu):
    zp = jnp.pad(z[:, :-1], ((0, 0), (1, 0), (0, 0)))
    zn = jnp.pad(z[:, 1:], ((0, 0), (0, 1), (0, 0)))
    return z + mu[0] * (zp - z) + mu[1] * (zn - z)


def rwkv_scan(decay, k, v, kk, a, r, s0, reverse):
    emit = r is not None
    seq = (decay, k, v, kk, a) + ((r,) if emit else ())
    xs = tuple(jnp.moveaxis(t.astype(jnp.float32), 1, 0) for t in seq)

    def step(S, inp):
        w_t, k_t, v_t, kk_t, a_t = inp[:5]
        sa = jnp.einsum('bhvk,bhk->bhv', S, kk_t)
        S = (S * w_t[:, :, None, :]
             - sa[..., None] * (kk_t * a_t)[:, :, None, :]
             + v_t[..., None] * k_t[:, :, None, :])
        y = jnp.einsum('bhvk,bhk->bhv', S, inp[5]) if emit else None
        return S, y

    S, ys = lax.scan(step, s0, xs, reverse=reverse)
    return S, (jnp.moveaxis(ys, 0, 1) if emit else None)


def rwkv_branch(zr, p, s0, emit):
    B, L, _ = zr.shape
    H, N = RWKV_HEADS, RWKV_HEAD
    r, k, v, w1f, w1b, a1f, a1b, g1 = jnp.split(zr, RWKV_SPLITS, axis=-1)
    heads = lambda t: t.reshape(B, L, H, N)
    kkr = heads(k * p['k_k']).astype(jnp.float32)
    kk = kkr / jnp.maximum(jnp.sqrt(jnp.sum(kkr * kkr, axis=-1, keepdims=True)), 1e-12)
    states, outs, bonus = [], [], []
    for d, (w1d, a1d) in enumerate(((w1f, a1f), (w1b, a1b))):
        wlog = -jax.nn.softplus(-(p['w0'][d] + jnp.tanh(w1d) @ p['w2'][d])) - 0.5
        decay = jnp.exp(-jnp.exp(wlog.astype(jnp.float32)))
        a = jax.nn.sigmoid(a1d @ p['a2'][d] + p['a0'][d])
        kd = k * (1 + (a - 1) * p['k_a'])
        S, y = rwkv_scan(heads(decay), heads(kd), heads(v), kk, heads(a),
                         heads(r) if emit else None, s0[d], reverse=(d == 1))
        states.append(S)
        if emit:
            outs.append(y)
            bonus.append(jnp.sum(heads(r * kd) * p['r_k'], axis=-1, keepdims=True))
    if not emit:
        return None, (states[0], states[1])
    o = outs[0] + outs[1]
    mu = jnp.mean(o, axis=-1, keepdims=True)
    var = jnp.mean(jnp.square(o - mu), axis=-1, keepdims=True)
    o = ((o - mu) * lax.rsqrt(var + GN_EPS)).reshape(B, L, RWKV_WIDTH) * p['lnx_g'] + p['lnx_b']
    o = o + ((bonus[0] + bonus[1]).astype(jnp.float32) * heads(v).astype(jnp.float32)).reshape(B, L, RWKV_WIDTH)
    g = jax.nn.sigmoid(g1) @ p['g2']
    y = (o.astype(zr.dtype) * g) @ p['w_oA']
    return y, (states[0], states[1])


def conv_branch(zc, p, rows):
    B, L, _ = zc.shape
    u = zc[..., :CONV_CH] * jax.nn.sigmoid(zc[..., CONV_CH:])
    img = u.reshape(B, rows, GRID_W, CONV_CH) if rows is not None else u[:, :, None, :]
    rhs = p['conv_w'][:, None, None, :].astype(img.dtype)
    out = lax.conv_general_dilated(img, rhs, window_strides=(1, 1),
                                   padding=((CONV_K // 2, CONV_K // 2), (0, 0)),
                                   dimension_numbers=('NHWC', 'HWIO', 'NHWC'),
                                   feature_group_count=CONV_CH)
    out = layernorm(out.reshape(B, L, CONV_CH), p['cnorm_g'], p['cnorm_b'])
    return jax.nn.silu(out) @ p['w_oB']


def token_mixer(h, p, rows, s0, emit):
    w_in = p['w_in'] if emit else p['w_in'][:, :P_RWKV]
    z = h @ w_in
    zr = token_shift(z[..., :P_RWKV], p['shift_mu'])
    y_a, states = rwkv_branch(zr, p, s0, emit)
    if not emit:
        return None, states
    y_b = conv_branch(z[..., P_RWKV:P_RWKV + P_CONV], p, rows)
    gates = jax.nn.sigmoid(z[..., P_RWKV + P_CONV:] + p['gate_b'])
    m = gates[..., :D_MODEL] * y_a + gates[..., D_MODEL:] * y_b
    return m @ p['w_out'], states


def peer(h, p):
    B, L, D = h.shape
    T = B * L
    chunk = math.gcd(T, PEER_CHUNK)
    hb = h.reshape(T // chunk, chunk, D)

    def block(hc):
        q = (hc @ p['w_q']).reshape(chunk, PEER_HEADS, 2, PEER_HALF)
        s = jnp.einsum('thcd,hcnd->thcn', q, p['sub_keys']).astype(jnp.float32)
        s1, i1 = lax.top_k(s[:, :, 0], PEER_TOPK)
        s2, i2 = lax.top_k(s[:, :, 1], PEER_TOPK)
        cand = (s1[..., :, None] + s2[..., None, :]).reshape(chunk, PEER_HEADS, PEER_TOPK * PEER_TOPK)
        cidx = (i1[..., :, None] * PEER_NKEYS + i2[..., None, :]).reshape(chunk, PEER_HEADS, PEER_TOPK * PEER_TOPK)
        best, pos = lax.top_k(cand, PEER_TOPK)
        eidx = jnp.take_along_axis(cidx, pos, axis=-1)
        gw = jax.nn.softmax(best, axis=-1)
        u = jnp.take(p['peer_u'], eidx, axis=0)
        act = jax.nn.gelu(jnp.einsum('td,thkd->thk', hc, u).astype(jnp.float32), approximate=False) * gw
        v = jnp.take(p['peer_v'], eidx, axis=0)
        return jnp.einsum('thk,thkd->td', act.astype(hc.dtype), v)

    return lax.map(block, hb).reshape(B, L, D)


def setup_inputs(seed: int = 0) -> dict:
    key = jax.random.key(seed)
    ks = jax.random.split(key, 40)
    f32 = jnp.float32
    nrm = lambda k, shape, s: jax.random.normal(k, shape, f32) * s
    Ly, D, W, H = DEPTH, D_MODEL, RWKV_WIDTH, RWKV_HEADS
    return {
        'x': nrm(ks[0], (BATCH, SEQ, D), 1.0),
        'c': nrm(ks[1], (BATCH, D), 1.0),
        'ctx': nrm(ks[2], (BATCH, CTX_LEN, D), 1.0),
        'c_ctx': nrm(ks[3], (D,), 1.0),
        'ada_w': nrm(ks[4], (Ly, D, 6 * D), 0.5 * D ** -0.5),
        'ada_b': nrm(ks[5], (Ly, 6 * D), 0.02),
        'norm1_g': 1.0 + nrm(ks[6], (Ly, D), 0.02),
        'norm2_g': 1.0 + nrm(ks[7], (Ly, D), 0.02),
        'w_in': nrm(ks[8], (Ly, D, P_IN), D ** -0.5),
        'shift_mu': jax.random.uniform(ks[9], (Ly, 2, P_RWKV), f32, 0.0, 0.5),
        'w0': jax.random.uniform(ks[10], (Ly, 2, W), f32, -6.0, 1.0),
        'w2': nrm(ks[11], (Ly, 2, LORA_W, W), 0.5 * LORA_W ** -0.5),
        'a0': nrm(ks[12], (Ly, 2, W), 0.5),
        'a2': nrm(ks[13], (Ly, 2, LORA_A, W), 0.5 * LORA_A ** -0.5),
        'g2': nrm(ks[14], (Ly, LORA_G, W), LORA_G ** -0.5),
        'k_k': 0.85 + nrm(ks[15], (Ly, W), 0.05),
        'k_a': 1.0 + nrm(ks[16], (Ly, W), 0.05),
        'r_k': nrm(ks[17], (Ly, H, RWKV_HEAD), 0.1),
        'lnx_g': 1.0 + nrm(ks[18], (Ly, W), 0.02),
        'lnx_b': nrm(ks[19], (Ly, W), 0.02),
        'w_oA': nrm(ks[20], (Ly, W, D), W ** -0.5),
        'conv_w': nrm(ks[21], (Ly, CONV_K, CONV_CH), CONV_K ** -0.5),
        'cnorm_g': 1.0 + nrm(ks[22], (Ly, CONV_CH), 0.02),
        'cnorm_b': nrm(ks[23], (Ly, CONV_CH), 0.02),
        'w_oB': nrm(ks[24], (Ly, CONV_CH, D), CONV_CH ** -0.5),
        'gate_b': nrm(ks[25], (Ly, P_GATE), 0.02),
        'w_out': nrm(ks[26], (Ly, D, D), D ** -0.5),
        'w_q': nrm(ks[27], (Ly, D, PEER_HEADS * PEER_QDIM), D ** -0.5),
        'sub_keys': nrm(ks[28], (Ly, PEER_HEADS, 2, PEER_NKEYS, PEER_HALF), PEER_HALF ** -0.5),
        'peer_u': nrm(ks[29], (Ly, PEER_EXPERTS, D), D ** -0.5),
        'peer_v': nrm(ks[30], (Ly, PEER_EXPERTS, D), 0.5),
        'final_g': 1.0 + nrm(ks[31], (D,), 0.02),
    }


def reference(x, c, ctx, c_ctx, ada_w, ada_b, norm1_g, norm2_g, w_in, shift_mu, w0, w2, a0, a2, g2,
              k_k, k_a, r_k, lnx_g, lnx_b, w_oA, conv_w, cnorm_g, cnorm_b, w_oB, gate_b, w_out,
              w_q, sub_keys, peer_u, peer_v, final_g):
    B = x.shape[0]
    rows = x.shape[1] // GRID_W
    xc = ctx
    zero_state = jnp.zeros((B, RWKV_HEADS, RWKV_HEAD, RWKV_HEAD), jnp.float32)
    for l in range(DEPTH):
        last = l == DEPTH - 1
        p = dict(w_in=w_in[l], shift_mu=shift_mu[l], w0=w0[l], w2=w2[l], a0=a0[l], a2=a2[l],
                 g2=g2[l], k_k=k_k[l], k_a=k_a[l], r_k=r_k[l], lnx_g=lnx_g[l], lnx_b=lnx_b[l],
                 w_oA=w_oA[l], conv_w=conv_w[l], cnorm_g=cnorm_g[l], cnorm_b=cnorm_b[l],
                 w_oB=w_oB[l], gate_b=gate_b[l], w_out=w_out[l], w_q=w_q[l],
                 sub_keys=sub_keys[l], peer_u=peer_u[l], peer_v=peer_v[l])
        mod = jax.nn.silu(c) @ ada_w[l] + ada_b[l]
        sh1, sc1, gt1, sh2, sc2, gt2 = jnp.split(mod[:, None, :], 6, axis=-1)
        modc = jax.nn.silu(c_ctx) @ ada_w[l] + ada_b[l]
        csh1, csc1, cgt1, csh2, csc2, cgt2 = jnp.split(modc, 6, axis=-1)

        hc = modulate(xc, norm1_g[l], csh1, csc1)
        yc, ctx_states = token_mixer(hc, p, None, (zero_state, zero_state), emit=not last)

        h = modulate(x, norm1_g[l], sh1, sc1)
        y, _ = token_mixer(h, p, rows, ctx_states, emit=True)
        x = x + gt1 * y
        x = x + gt2 * peer(modulate(x, norm2_g[l], sh2, sc2), p)

        if not last:
            xc = xc + cgt1 * yc
            xc = xc + cgt2 * peer(modulate(xc, norm2_g[l], csh2, csc2), p)
    return rmsnorm(x, final_g)
```

```python
from contextlib import ExitStack
import math
import numpy as np
import concourse.bass as bass
import concourse.mybir as mybir
from concourse.bass_utils import run_bass_kernel_spmd

F32 = mybir.dt.float32
BF16 = mybir.dt.bfloat16
AF = mybir.ActivationFunctionType
ALU = mybir.AluOpType
AX = mybir.AxisListType

D = 1024
NCH = 8
HEADS = 16
HD = 64
LORA_G = 160
CONV_K = 31
GRID_W = 64
P_IN_PAD = 7680
NFC = 60
PH, PNK, PHALF, PTOPK = 8, 128, 128, 16
NEXP = PNK * PNK
NORM_EPS = 1e-6
LN_EPS = 1e-5
GN_EPS = HD * 1e-5
CH = 64
EXPM05 = math.exp(-0.5)
F32R = mybir.dt.float32r
RWKV_F32R = True


class Sched:
    GEN = 20000

    def __init__(self, nc, stack, ndma=24):
        self.nc = nc
        self.stack = stack
        self.eng = {'pe': nc.tensor, 'act': nc.scalar, 'dve': nc.vector, 'pool': nc.gpsimd, 'sp': nc.sync}
        self.prog = {e: [] for e in self.eng}
        self.cnt = {e: 0 for e in self.eng}
        self.gen = {e: -1 for e in self.eng}
        self.semh = {}
        for e in self.eng:
            self._newgen(e)
        self.ndma = ndma
        self.dmaval = [0] * ndma
        self.dmarr = 0
        for i in range(ndma):
            self.semh[('dma', i)] = stack.enter_context(nc.semaphore(f"sdma{i}"))
        self.waited = {}
        self.lastw = {}
        self.readers = {}
        self.nops = 0

    def _newgen(self, e):
        self.gen[e] += 1
        self.cnt[e] = 0
        self.semh[(e, self.gen[e])] = self.stack.enter_context(self.nc.semaphore(f"s{e}{self.gen[e]}"))

    def _wait(self, eng, key, val):
        if self.waited.get((eng, key), 0) >= val:
            return
        self.waited[(eng, key)] = val
        self.prog[eng].append(('wait', self.semh[key], val))

    def _deps(self, eng, reads, writes):
        toks = {}

        def add(d):
            for k, v in d.items():
                if toks.get(k, 0) < v:
                    toks[k] = v
        for b in reads:
            add(self.lastw.get(b, {}))
        for b in writes:
            add(self.lastw.get(b, {}))
            add(self.readers.get(b, {}))
        for k, v in toks.items():
            if eng == 'pe' and k[0] == 'pe':
                continue
            self._wait(eng, k, v)

    def _record(self, key, val, reads, writes):
        for b in writes:
            self.lastw[b] = {key: val}
            self.readers[b] = {}
        for b in reads:
            if b in writes:
                continue
            r = self.readers.setdefault(b, {})
            if r.get(key, 0) < val:
                r[key] = val

    def op(self, eng, fn, reads, writes):
        pr = [r for r in reads if r.startswith('pb') and r not in writes]
        if pr:
            writes = list(writes) + pr
        self._deps(eng, reads, writes)
        if self.cnt[eng] >= self.GEN:
            self._newgen(eng)
        self.cnt[eng] += 1
        key = (eng, self.gen[eng])
        val = self.cnt[eng]
        self.prog[eng].append(('op', fn, self.semh[key]))
        self._record(key, val, reads, writes)
        self.nops += 1

    def dma(self, q, out, in_, reads=None, writes=None):
        reads = [in_.tensor.name] if reads is None else reads
        writes = [out.tensor.name] if writes is None else writes
        self._deps(q, reads, writes)
        i = self.dmarr
        self.dmarr = (i + 1) % self.ndma
        key = ('dma', i)
        self._wait(q, key, self.dmaval[i])
        self.dmaval[i] += 16
        self.prog[q].append(('dma', out, in_, self.semh[key]))
        self._record(key, self.dmaval[i], reads, writes)
        self.nops += 1

    def barrier(self):
        for e in self.eng:
            for o in self.eng:
                if o != e and self.cnt[o] > 0:
                    self._wait(e, (o, self.gen[o]), self.cnt[o])
            for i in range(self.ndma):
                if self.dmaval[i] > 0:
                    self._wait(e, ('dma', i), self.dmaval[i])

    def finish(self, q='sp'):
        for i in range(self.ndma):
            self._wait(q, ('dma', i), self.dmaval[i])
        for e in self.eng:
            if e != q and self.cnt[e] > 0:
                self._wait(q, (e, self.gen[e]), self.cnt[e])

    def emit(self):
        nc = self.nc
        prog = self.prog

        def replay(e, items):
            for it in items:
                if it[0] == 'wait':
                    e.wait_ge(it[1], it[2])
                elif it[0] == 'op':
                    it[1](e).then_inc(it[2], 1)
                else:
                    e.dma_start(out=it[1], in_=it[2]).then_inc(it[3], 16)
        with nc.Block() as block:
            @block.tensor
            def _(e):
                replay(e, prog['pe'])

            @block.scalar
            def _(e):
                replay(e, prog['act'])

            @block.vector
            def _(e):
                replay(e, prog['dve'])

            @block.gpsimd
            def _(e):
                replay(e, prog['pool'])

            @block.sync
            def _(e):
                replay(e, prog['sp'])


def _nm(*aps):
    out = []
    for a in aps:
        if hasattr(a, 'tensor'):
            n = a.tensor.name
            if n not in out:
                out.append(n)
    return out


class K:
    SB_BASE, SB_LIMIT = 16512, 229344

    def __init__(self, nc, S, stack):
        self.nc, self.S, self.stack = nc, S, stack
        self.rr = 0
        self.top = self.SB_BASE
        self.marks = []
        self.uid = 0

    def sb(self, name, shape, dt=F32):
        isz = 2 if dt == BF16 else 4
        size = isz
        for s_ in shape[1:]:
            size *= s_
        size = (size + 63) // 64 * 64
        off = self.top
        self.top += size
        assert self.top <= self.SB_LIMIT, f"SBUF arena overflow allocating {name} {shape}: top={self.top}"
        self.uid += 1
        return self.nc.alloc_sbuf_tensor_at(f"{name}_u{self.uid}", list(shape), dt, offset=off)

    def push(self):
        self.marks.append(self.top)

    def pop(self):
        self.top = self.marks.pop()
        self.S.barrier()

    def ps(self, name, shape, dt=F32):
        return self.stack.enter_context(self.nc.psum_tensor(name, list(shape), dt))

    def dram(self, name, shape, dt=F32, kind="Internal"):
        return self.nc.dram_tensor(name, list(shape), dt, kind=kind)

    def ve(self):
        self.rr ^= 1
        return 'dve' if self.rr else 'pool'

    def mm(self, out, lhsT, rhs, start=True, stop=True, skip=False):
        if skip:
            self.S.op('pe', lambda e: e.matmul(out, lhsT, rhs, start=start, stop=stop, skip_group_check=True),
                      _nm(lhsT, rhs), _nm(out))
        else:
            self.S.op('pe', lambda e: e.matmul(out, lhsT, rhs, start=start, stop=stop), _nm(lhsT, rhs), _nm(out))

    def tr(self, out, in_, ident):
        self.S.op('pe', lambda e: e.transpose(out, in_, ident), _nm(in_, ident), _nm(out))

    def act(self, out, in_, func, bias=None, scale=None, accum_out=None):
        kw = {}
        if bias is not None:
            kw['bias'] = bias
        if scale is not None:
            kw['scale'] = scale
        if accum_out is not None:
            kw['accum_out'] = accum_out
        self.S.op('act', lambda e: e.activation(out, in_, func, **kw), _nm(in_, bias, scale), _nm(out, accum_out))

    def tt(self, eng, out, in0, in1, op):
        self.S.op(eng, lambda e: e.tensor_tensor(out, in0, in1, op), _nm(in0, in1), _nm(out))

    def ts(self, eng, out, in0, s1, op0, s2=None, op1=None, accum_out=None):
        kw = {}
        if accum_out is not None:
            kw['accum_out'] = accum_out
        if op1 is None:
            self.S.op(eng, lambda e: e.tensor_scalar(out, in0, s1, None, op0, **kw), _nm(in0, s1), _nm(out, accum_out))
        else:
            self.S.op(eng, lambda e: e.tensor_scalar(out, in0, s1, s2, op0, op1, **kw), _nm(in0, s1, s2), _nm(out, accum_out))

    def stt(self, eng, out, in0, scalar, in1, op0, op1, accum_out=None):
        eng = 'dve'
        kw = {}
        if accum_out is not None:
            kw['accum_out'] = accum_out
        self.S.op(eng, lambda e: e.scalar_tensor_tensor(out, in0, scalar, in1, op0, op1, **kw),
                  _nm(in0, scalar, in1), _nm(out, accum_out))

    def copy(self, eng, out, in_):
        if eng == 'act':
            self.S.op('act', lambda e: e.copy(out, in_), _nm(in_), _nm(out))
        else:
            self.S.op(eng, lambda e: e.tensor_copy(out, in_), _nm(in_), _nm(out))

    def recip(self, out, in_):
        self.S.op('dve', lambda e: e.reciprocal(out, in_), _nm(in_), _nm(out))

    def max8(self, out, in_):
        self.S.op('dve', lambda e: e.max(out, in_), _nm(in_), _nm(out))

    def match_replace(self, out, in_to_replace, in_values, imm):
        self.S.op('dve', lambda e: e.match_replace(out, in_to_replace, in_values, imm), _nm(in_to_replace, in_values), _nm(out))

    def scan(self, eng, out, d0, d1, init, op0, op1):
        eng = 'dve'
        self.S.op(eng, lambda e: e.tensor_tensor_scan(out, d0, d1, init, op0, op1), _nm(d0, d1), _nm(out))

    def reduce(self, eng, out, in_, axis, op):
        self.S.op(eng, lambda e: e.tensor_reduce(out, in_, axis, op), _nm(in_), _nm(out))

    def memset(self, eng, out, val):
        self.S.op(eng, lambda e: e.memset(out, val), [], _nm(out))

    def dma(self, out, in_, q='sp'):
        self.S.dma(q, out, in_)


VEC = {}
_off = 0
for _n, _w in [('norm1_g', 8), ('norm2_g', 8), ('k_k', 8), ('k_a', 8), ('omka', 8), ('r_k', 8), ('lnx_g', 8), ('lnx_b', 8),
               ('cnorm_g', 8), ('cnorm_b', 8), ('gate_b', 16), ('w0', 16), ('a0', 16), ('final_g', 8),
               ('mu0', 28), ('mu1', 28), ('muc', 28), ('ada_b', 48), ('conv_w', 8 * CONV_K)]:
    VEC[_n] = (_off, _w)
    _off += _w
NV = _off


def token_blocks(CTX, SEQ, bs=512):
    blocks = []
    t = 0
    while t < CTX:
        n = min(bs, CTX - t)
        blocks.append((t, n, 0))
        t += n
    while t < CTX + SEQ:
        n = min(bs, CTX + SEQ - t)
        blocks.append((t, n, 1))
        t += n
    return blocks


def build(cfg):
    CTX, SEQ, NL = cfg['CTX'], cfg['SEQ'], cfg['NL']
    stop_after = cfg.get('stop_after', None)
    T = CTX + SEQ
    nc = bass.Bass("TRN2", target_bir_lowering=False)
    stack = ExitStack()
    with stack:
        S = Sched(nc, stack)
        k = K(nc, S, stack)
        stack.enter_context(nc.allow_low_precision("bf16 matmuls with fp32 accumulation (tolerance allows)"))
        inp = lambda name, shape: nc.dram_tensor(name, list(shape), F32, kind="ExternalInput").ap()
        xT = inp("xT", [D, T])
        cc = inp("cc", [128, NCH, 2])
        ada_w = inp("ada_w", [NL, 48, 128, NCH, 128])
        vec = inp("vec", [NL, 128, NV])
        w_in = inp("w_in", [NL, NFC, 128, NCH, 128])
        w2 = inp("w2", [NL, 128, D])
        a2 = inp("a2", [NL, 128, D])
        g2 = inp("g2", [NL, 2, 128, D])
        w_oA = inp("w_oA", [NL, 128, NCH, D])
        w_oB = inp("w_oB", [NL, 128, NCH, D])
        w_out = inp("w_out", [NL, 128, NCH, D])
        w_q = inp("w_q", [NL, 128, NCH, 2 * D])
        skT = inp("skT", [NL, 16, 128, 128])
        puT = inp("puT", [NL, 32, 128, NCH, 512])
        pv = inp("pv", [NL, 32, 128, 4, D])
        consts = inp("consts", [128, 10, 128])
        outT = nc.dram_tensor("outT", [D, SEQ], F32, kind="ExternalOutput").ap()
        dbg = {}

        def scratch(name, shape, dt=F32):
            kind = "ExternalOutput" if cfg.get('debug') else "Internal"
            t_ = nc.dram_tensor(name, list(shape), dt, kind=kind).ap()
            dbg[name] = t_
            return t_
        xres = scratch("xres", [D, T])
        zT = scratch("zT", [P_IN_PAD, T])
        y0T = scratch("y0T", [D, T])
        bv0T = scratch("bv0T", [D, T])
        ogT = scratch("ogT", [D, T])
        cvT = scratch("cvT", [D, T])
        puTb = scratch("puTb", [32, 128, NCH, 512], BF16)
        pvb = scratch("pvb", [32, 128, 4, D], BF16)

        cst = k.sb("cst", [128, 10, 128])
        k.dma(cst[:], consts)
        ident = cst[:, 0, :]
        onesblk = cst[:, 1, :]
        onesD = cst[:, 2, :]
        identblk = cst[:, 7, 0:64]
        ones64 = cst[:, 8, :]
        rst = k.sb("rst", [128, 512])
        k.memset('dve', rst[:], 1.0)
        k.memset('dve', rst[:].rearrange("p (c j) -> p c j", j=CH)[:, :, 0:1], 0.0)
        mA = k.sb("mA", [128, 2, 2, 128])
        mB = k.sb("mB", [128, 2, 2, 128])
        mT = k.sb("mT", [128, 2, 128])
        for d in range(2):
            k.copy('dve', mA[:, d, 0, :], cst[:, 3 + 2 * d, :])
            k.ts('dve', mA[:, d, 1, :], cst[:, 4 + 2 * d, :], -1.0, ALU.mult)
            k.copy('dve', mB[:, d, 0, :], cst[:, 3 + 2 * d, :])
            k.copy('dve', mB[:, d, 1, :], cst[:, 4 + 2 * d, :])
            k.copy('dve', mT[:, d, :], cst[:, 5 - 2 * d, :])

        pb = [k.ps(f"pb{i}", [128, 512]) for i in range(8)]
        ccs = k.sb("ccs", [128, NCH, 2])
        k.dma(ccs[:], cc)
        k.act(ccs[:], ccs[:], AF.Silu)
        vecs = k.sb("vecs", [128, NV])
        modT = k.sb("modT", [128, 48, 2])

        def V(name, j=0, w=1):
            o, _ = VEC[name]
            return vecs[:, o + j:o + j + w]

        blocks = token_blocks(CTX, SEQ)
        sqs = k.sb("sqs", [128, 512])
        rstd = k.sb("rstd", [128, 512])
        xn = k.sb("xn", [128, 512])
        epsn = k.sb("epsn", [128, 1])
        k.memset('dve', epsn[:], NORM_EPS)
        gm = k.sb("gm", [128, 2, NCH, 2])

        for l in range(NL):
            last = (l == NL - 1)
            src = xT if l == 0 else xres
            k.dma(vecs[:], vec[l])
            if True:
                k.push()
                aw = [k.sb(f"aw{l}_{i}", [128, NCH, 128]) for i in range(2)]
                for j in range(48):
                    a_ = aw[j % 2]
                    k.dma(a_[:], ada_w[l, j])
                    pm = pb[j % 2]
                    for dh in range(NCH):
                        k.mm(pm[:, 0:2], a_[:, dh, :], ccs[:, dh, :], start=(dh == 0), stop=(dh == NCH - 1))
                    k.ts('dve', modT[:, j, :], pm[:, 0:2], V('ada_b', j), ALU.add)
                for w_, (nn, sci) in enumerate((('norm1_g', 1), ('norm2_g', 4))):
                    for c in range(NCH):
                        k.ts('dve', gm[:, w_, c, :], modT[:, sci * 8 + c, :], 1.0, ALU.add, V(nn, c), ALU.mult)
                k.pop()

            def modulate_block(dst, xb, n, which, seg, tagp):
                pss = pb[7]
                for c in range(NCH):
                    k.act(sqs[:, :n], xb[:, c, :n], AF.Square)
                    k.mm(pss[:, :n], onesD, sqs[:, :n], start=(c == 0), stop=(c == NCH - 1))
                k.act(rstd[:, :n], pss[:, :n], AF.Sqrt, bias=epsn[:, 0:1])
                k.recip(rstd[:, :n], rstd[:, :n])
                shi = 0 if which == 0 else 3
                for c in range(NCH):
                    e_ = k.ve()
                    k.tt(e_, xn[:, :n], xb[:, c, :n], rstd[:, :n], ALU.mult)
                    k.ts(e_, dst[:, c, :n], xn[:, :n], gm[:, which, c, seg:seg + 1], ALU.mult,
                         modT[:, shi * 8 + c, seg:seg + 1], ALU.add)


            if True:
                k.push()
                HT = k.sb(f"HT{l}", [128, NCH, T], BF16)
                xb = [k.sb(f"xb{l}_{i}", [128, NCH, 512]) for i in range(2)]
                for bi, (t0, n, seg) in enumerate(blocks):
                    xb_ = xb[bi % 2]
                    k.dma(xb_[:, :, :n], src[:, t0:t0 + n].rearrange("(c p) t -> p c t", p=128))
                    if l == 0:
                        k.dma(xres[:, t0:t0 + n].rearrange("(c p) t -> p c t", p=128), xb_[:, :, :n], q='act')
                    modulate_block(HT[:, :, t0:t0 + n], xb_, n, 0, 1 - seg, f"A{l}")
                wf = [k.sb(f"wf{l}_{i}", [128, NCH, 128]) for i in range(2)]
                wb = [k.sb(f"wb{l}_{i}", [128, NCH, 128], BF16) for i in range(2)]
                zst = [k.sb(f"zst{l}_{i}", [128, 512]) for i in range(4)]
                cnt = 0
                nfc = 28 if (last and False) else NFC
                for fc in range(nfc):
                    wf_, wb_ = wf[fc % 2], wb[fc % 2]
                    k.dma(wf_[:], w_in[l, fc])
                    k.copy('pool', wb_[:], wf_[:])
                    for (t0, n, seg) in blocks:
                        if last and seg == 0 and fc >= 28:
                            continue
                        pz = pb[cnt % 4]
                        for dh in range(NCH):
                            k.mm(pz[:, :n], wb_[:, dh, :], HT[:, dh, t0:t0 + n], start=(dh == 0), stop=(dh == NCH - 1))
                        z_ = zst[cnt % 4]
                        k.copy('act' if cnt % 2 else 'dve', z_[:, :n], pz[:, :n])
                        k.dma(zT[fc * 128:(fc + 1) * 128, t0:t0 + n], z_[:, :n], q='act')
                        cnt += 1
                k.pop()
            if stop_after == 'B':
                break
            k.push()
            k.tt('dve', V('muc', 0, 28), V('mu0', 0, 28), V('mu1', 0, 28), ALU.add)
            k.ts('dve', V('muc', 0, 28), V('muc', 0, 28), -1.0, ALU.mult, 1.0, ALU.add)
            k.ts('dve', V('omka', 0, 8), V('k_a', 0, 8), -1.0, ALU.mult, 1.0, ALU.add)
            TW = T + 4
            zw = [k.sb(f"zw{l}_{i}", [128, TW]) for i in range(2)]
            zo = [k.sb(f"zo{l}_{i}", [128, TW]) for i in range(2)]
            for i in range(2):
                k.memset('pool', zw[i][:, 0:1], 0.0)
                k.memset('pool', zw[i][:, CTX + 1:CTX + 3], 0.0)
                k.memset('pool', zw[i][:, TW - 1:TW], 0.0)

            def b2_load(fc):
                rows = slice(fc * 128, (fc + 1) * 128)
                k.dma(zw[fc % 2][:, 1:1 + CTX], zT[rows, 0:CTX])
                k.dma(zw[fc % 2][:, CTX + 3:CTX + 3 + SEQ], zT[rows, CTX:T])
            b2_load(0)
            for fc in range(28):
                rows = slice(fc * 128, (fc + 1) * 128)
                if fc + 1 < 28:
                    b2_load(fc + 1)
                zw_, zo_ = zw[fc % 2], zo[fc % 2]
                k.ts('dve', zo_[:, 1:TW - 1], zw_[:, 1:TW - 1], V('muc', fc), ALU.mult)
                k.stt('pool', zo_[:, 1:TW - 1], zw_[:, 0:TW - 2], V('mu0', fc), zo_[:, 1:TW - 1], ALU.mult, ALU.add)
                k.stt('dve', zo_[:, 1:TW - 1], zw_[:, 2:TW], V('mu1', fc), zo_[:, 1:TW - 1], ALU.mult, ALU.add)
                if fc == 24:
                    k.act(zo_[:, 1:TW - 1], zo_[:, 1:TW - 1], AF.Tanh)
                if fc in (26, 27):
                    k.act(zo_[:, 1:TW - 1], zo_[:, 1:TW - 1], AF.Sigmoid)
                k.dma(zT[rows, 0:CTX], zo_[:, 1:1 + CTX], q='act')
                k.dma(zT[rows, CTX:T], zo_[:, CTX + 3:CTX + 3 + SEQ], q='act')
            k.pop()
            if stop_after == 'B2':
                break

            k.push()
            w2s = k.sb(f"w2s{l}", [128, D])
            a2s = k.sb(f"a2s{l}", [128, D])
            g2s = k.sb(f"g2s{l}", [128, 2, D])
            k.dma(w2s[:], w2[l])
            k.dma(a2s[:], a2[l])
            k.dma(g2s[:], g2[l].rearrange("c p f -> p c f"))
            RDT = F32R if RWKV_F32R else F32
            Sst = [k.sb(f"Sst{l}_{g}", [128, HD], RDT) for g in range(8)]
            gneps = k.sb(f"gneps{l}", [128, 1])
            k.memset('dve', gneps[:], GN_EPS)
            NS = 2
            hmask = [onesblk[:, 0:1], onesblk[:, 64:65]]

            class Obj:
                pass
            Ps, Us = [], []
            for s in range(NS):
                P = Obj()
                for nm_ in ('rr', 'kx', 'vv', 'kk', 'aa', 'kd', 'bb', 'lw', 'lam', 'pex', 'rem', 'Lb', 'BQ', 'nBQP', 'KQP',
                            'BQ0', 'BQ1', 'KQ0', 'KQ1', 'KP0', 'KP1',
                            't1', 't2', 'e1', 'e3', 'Yb', 'bv'):
                    setattr(P, nm_, k.sb(f"P{l}_{s}_{nm_}", [128, 512],
                                         RDT if nm_ in ('BQ', 'BQ0', 'BQ1', 'KQ0', 'KQ1', 'KP0', 'KP1') else F32))
                P.e2, P.e4, P.Lb = P.aa, P.t1, P.kx
                P.KR = k.sb(f"P{l}_{s}_KR", [128, 2, 512], RDT)
                P.PC = k.sb(f"P{l}_{s}_PC", [128, 8])
                Ps.append(P)
                U = Obj()
                U.TM = k.sb(f"U{l}_{s}_TM", [128, 4, 128], RDT)
                U.ZA = k.sb(f"U{l}_{s}_ZA", [128, 2, 2, 128], RDT)
                U.ZB = k.sb(f"U{l}_{s}_ZB", [128, 2, 2, 128], RDT)
                U.AA = k.sb(f"U{l}_{s}_AA", [128, 2, 128], RDT)
                U.ZZ = [k.sb(f"U{l}_{s}_ZZ{j}", [128, 2, 2, 128], RDT) for j in range(5)]
                U.X = [k.sb(f"U{l}_{s}_X{j}", [128, 2, 128], RDT) for j in range(2)]
                U.MT = k.sb(f"U{l}_{s}_MT", [128, 2, 64], RDT)
                U.NN = k.sb(f"U{l}_{s}_NN", [128, 2, 64], RDT)
                U.RPp = k.sb(f"U{l}_{s}_RPp", [128, 128], RDT)
                U.banks = [pb[2 + 3 * s], pb[3 + 3 * s]]
                U.ybank = pb[4 + 3 * s]
                U.bi = 0
                Us.append(U)
            lor = [[k.sb(f"lor{l}_{i}_{j}", [128, 512]) for j in range(4)] for i in range(1)]
            prep_cnt = [0]

            class KR:
                def __getattr__(self, a):
                    return getattr(k, a)

                def mm(self, out, lhsT, rhs, start=True, stop=True, skip=False):
                    if out.base_partition() != 0 or out.shape[0] != 128:
                        lhsT, rhs = lhsT.bitcast(F32), rhs.bitcast(F32)
                    k.mm(out, lhsT, rhs, start=start, stop=stop, skip=skip)
            kr = KR()

            def prep_bank():
                prep_cnt[0] += 1
                return pb[prep_cnt[0] % 2]

            def ubank(U):
                U.bi += 1
                return U.banks[U.bi % 2]

            def prep(P, d, t0, n, g, tw, awt):
                k.dma(P.rr[:, :n], zT[g * 128:(g + 1) * 128, t0:t0 + n])
                k.dma(P.kx[:, :n], zT[(8 + g) * 128:(9 + g) * 128, t0:t0 + n])
                k.dma(P.vv[:, :n], zT[(16 + g) * 128:(17 + g) * 128, t0:t0 + n])
                k.ts('pool', P.t1[:, :n], P.kx[:, :n], V('k_k', g), ALU.mult)
                k.tt('pool', P.t2[:, :n], P.t1[:, :n], P.t1[:, :n], ALU.mult)
                pq = prep_bank()
                k.mm(pq[:, :n], onesblk, P.t2[:, :n])
                k.act(P.t2[:, :n], pq[:, :n], AF.Sqrt)
                k.ts('dve', P.t2[:, :n], P.t2[:, :n], 1e-12, ALU.max)
                k.recip(P.t2[:, :n], P.t2[:, :n])
                k.tt('pool', P.kk[:, :n], P.t1[:, :n], P.t2[:, :n], ALU.mult)
                ds = slice(d * 64, (d + 1) * 64)
                gs = slice(g * 128, (g + 1) * 128)
                pq = prep_bank()
                k.mm(pq[:, :n], w2s[ds, gs], tw[ds, :n])
                k.act(P.lw[:, :n], pq[:, :n], AF.Sigmoid, bias=V('w0', d * 8 + g))
                k.ts('pool', P.lw[:, :n], P.lw[:, :n], -EXPM05, ALU.mult)
                pq = prep_bank()
                k.mm(pq[:, :n], a2s[ds, gs], awt[ds, :n])
                k.act(P.aa[:, :n], pq[:, :n], AF.Sigmoid, bias=V('a0', d * 8 + g))
                k.ts('pool', P.t1[:, :n], P.aa[:, :n], V('k_a', g), ALU.mult, V('omka', g), ALU.add)
                k.tt('pool', P.kd[:, :n], P.t1[:, :n], P.kx[:, :n], ALU.mult)
                k.tt('pool', P.bb[:, :n], P.aa[:, :n], P.kk[:, :n], ALU.mult)
                k.tt('pool', P.t1[:, :n], P.rr[:, :n], P.kd[:, :n], ALU.mult)
                k.ts('pool', P.t1[:, :n], P.t1[:, :n], V('r_k', g), ALU.mult)
                pq = prep_bank()
                k.mm(pq[:, :n], onesblk, P.t1[:, :n])
                k.tt('dve', P.bv[:, :n], pq[:, :n], P.vv[:, :n], ALU.mult)
                nchk = n // CH
                k.scan('dve', P.lam[:, :n], rst[:, :n], P.lw[:, :n], 0.0, ALU.mult, ALU.add)
                lam3 = P.lam[:, :n].rearrange("p (c j) -> p c j", j=CH)
                tot_b = lam3[:, :, CH - 1:CH].to_broadcast([128, nchk, CH])
                k.tt('pool', P.pex[:, :n], P.lam[:, :n], P.lw[:, :n], ALU.subtract)
                k.tt('pool', P.rem[:, :n].rearrange("p (c j) -> p c j", j=CH), tot_b, lam3, ALU.subtract)
                if d == 0:
                    L, Lex, Lrem = P.lam, P.pex, P.rem
                else:
                    k.tt('pool', P.Lb[:, :n], P.rem[:, :n], P.lw[:, :n], ALU.add)
                    L, Lex, Lrem = P.Lb, P.rem, P.pex
                k.act(P.e1[:, :n], Lex[:, :n], AF.Exp)
                k.tt('pool', P.KR[:, 0, :n], P.kk[:, :n], P.e1[:, :n], ALU.mult)
                k.stt('dve', P.KP0[:, :n], P.kk[:, :n], hmask[0], P.e1[:, :n], ALU.mult, ALU.mult)
                k.stt('dve', P.KP1[:, :n], P.kk[:, :n], hmask[1], P.e1[:, :n], ALU.mult, ALU.mult)
                k.act(P.e2[:, :n], L[:, :n], AF.Exp)
                k.tt('pool', P.KR[:, 1, :n], P.rr[:, :n], P.e2[:, :n], ALU.mult)
                k.act(P.e3[:, :n], L[:, :n], AF.Exp, scale=-1.0)
                k.tt('pool', P.BQ[:, :n], P.bb[:, :n], P.e3[:, :n], ALU.mult)
                k.stt('dve', P.BQ0[:, :n], P.bb[:, :n], hmask[0], P.e3[:, :n], ALU.mult, ALU.mult)
                k.stt('dve', P.BQ1[:, :n], P.bb[:, :n], hmask[1], P.e3[:, :n], ALU.mult, ALU.mult)
                k.stt('dve', P.KQ0[:, :n], P.kd[:, :n], hmask[0], P.e3[:, :n], ALU.mult, ALU.mult)
                k.stt('dve', P.KQ1[:, :n], P.kd[:, :n], hmask[1], P.e3[:, :n], ALU.mult, ALU.mult)
                k.act(P.e4[:, :n], Lrem[:, :n], AF.Exp)
                k.stt('dve', P.nBQP[:, :n], P.bb[:, :n], -1.0, P.e4[:, :n], ALU.mult, ALU.mult)
                k.tt('pool', P.KQP[:, :n], P.kd[:, :n], P.e4[:, :n], ALU.mult)
                k.act(P.PC[:, :nchk], lam3[:, :, CH - 1], AF.Exp)

            def unit(P, U, d, g, ti):
                k = kr
                sl = slice(ti * 128, (ti + 1) * 128)
                H = [slice(0, 64), slice(64, 128)]
                pT = ubank(U)
                pT4 = pT[:, :].rearrange("p (a t) -> p a t", a=4)
                k.tr(pT4[:, 0, :], P.vv[:, sl], ident)
                k.tr(pT4[:, 1, :], P.KR[:, 0, sl].bitcast(F32), ident)
                k.tr(pT4[:, 2, :], P.nBQP[:, sl], ident)
                k.tr(pT4[:, 3, :], P.KQP[:, sl], ident)
                k.copy('act', U.TM[:], pT4)
                yield
                pA = ubank(U)
                pA4 = pA[:, :].rearrange("p (h w t) -> p h w t", h=2, w=2)
                BQh, KQh, KPh = [P.BQ0, P.BQ1], [P.KQ0, P.KQ1], [P.KP0, P.KP1]
                for hh in range(2):
                    k.mm(pA4[:, hh], BQh[hh][:, sl], P.KR[:, :, sl])
                k.tt('dve', U.ZA[:], pA4, mA[:, d].unsqueeze(1).to_broadcast([128, 2, 2, 128]), ALU.mult)
                pB = ubank(U)
                pB4 = pB[:, :].rearrange("p (h w t) -> p h w t", h=2, w=2)
                for hh in range(2):
                    k.mm(pB4[:, hh], KQh[hh][:, sl], P.KR[:, :, sl])
                k.tt('dve', U.ZB[:], pB4, mB[:, d].unsqueeze(1).to_broadcast([128, 2, 2, 128]), ALU.mult)
                pC = ubank(U)
                pC3 = pC[:, 0:256].rearrange("p (h t) -> p h t", h=2)
                for hh in range(2):
                    k.mm(pC3[:, hh], KPh[hh][:, sl], P.BQ[:, sl])
                k.tt('dve', U.AA[:], pC3, mT[:, d].unsqueeze(1).to_broadcast([128, 2, 128]), ALU.mult)
                yield
                Zc = [U.ZA[:, 0, 0, :], U.ZA[:, 1, 0, :]]
                Ac = [U.AA[:, 0, :], U.AA[:, 1, :]]
                Zp = [Zc]
                for lev in range(5):
                    pQ = ubank(U)
                    pQ4 = pQ[:, :].rearrange("p (w h t) -> p w h t", w=2, h=2)
                    for hh in range(2):
                        k.mm(pQ4[:, 0, hh], Ac[hh], Zc[hh])
                        if lev < 4:
                            k.mm(pQ4[:, 1, hh], Zc[hh], Ac[hh])
                    ZZl = U.ZZ[lev]
                    if lev < 4:
                        k.copy('act' if lev % 2 else 'dve', ZZl[:], pQ4)
                    else:
                        k.copy('dve', ZZl[:, 0], pQ4[:, 0])
                    Zc = [ZZl[:, 0, 0, :], ZZl[:, 0, 1, :]]
                    Ac = [ZZl[:, 1, 0, :], ZZl[:, 1, 1, :]]
                    Zp.append(Zc)
                    yield
                pX = ubank(U)
                pX3 = pX[:, 0:256].rearrange("p (h c) -> p h c", h=2)
                for hh in range(2):
                    k.mm(pX3[:, hh, 0:64], U.ZB[:, hh, 0, :], U.TM[:, 0, H[hh]])
                X = U.X[0]
                k.copy('act', X[:, :, 0:64], pX3[:, :, 0:64])
                k.copy('pool', X[:, :, 64:128], U.TM[:, 1, :].rearrange("p (h c) -> p h c", h=2))
                yield
                for lev in range(6):
                    pP = ubank(U)
                    pP3 = pP[:, 0:256].rearrange("p (h c) -> p h c", h=2)
                    for hh in range(2):
                        k.mm(pP3[:, hh, :], Zp[lev][hh], X[:, hh, :])
                    Xn = U.X[(lev + 1) % 2]
                    k.tt('dve', Xn[:], X[:], pP3, ALU.subtract if lev == 0 else ALU.add)
                    X = Xn
                    yield
                pMNs = [ubank(U), ubank(U)]
                for c2 in range(2):
                    cs = H[c2]
                    pMN = pMNs[c2]
                    for hh in range(2):
                        k.mm(pMN[H[hh], 0:64], X[cs, hh, 64:128], U.TM[cs, 2, H[hh]])
                        k.mm(pMN[H[hh], 64:128], U.TM[cs, 3, H[hh]], U.TM[cs, 0, H[hh]], start=True, stop=False)
                        k.mm(pMN[H[hh], 64:128], U.TM[cs, 2, H[hh]], X[cs, hh, 0:64], start=False, stop=True)
                for c2 in range(2):
                    k.stt('dve', U.MT[:, c2, :], identblk, P.PC[:, ti * 2 + c2:ti * 2 + c2 + 1],
                          pMNs[c2][:, 0:64], ALU.mult, ALU.add)
                    k.copy('act', U.NN[:, c2, :], pMNs[c2][:, 64:128])
                pR = ubank(U)
                for hh in range(2):
                    k.mm(pR[H[hh], 0:128], X[:, hh, 64:128], U.ZA[:, hh, 1, :])
                k.tt('dve', U.RPp[:], P.KR[:, 1, sl], pR[:, 0:128], ALU.add)
                pY = U.ybank
                for hh in range(2):
                    k.mm(pY[H[hh], 0:128], U.TM[:, 0, H[hh]], U.ZB[:, hh, 1, :], start=True, stop=False, skip=True)
                    k.mm(pY[H[hh], 0:128], X[:, hh, 0:64], U.ZA[:, hh, 1, :], start=False, stop=False, skip=True)
                yield
                for c2 in ((0, 1) if d == 0 else (1, 0)):
                    cs = H[c2]
                    for hh in range(2):
                        k.mm(pY[H[hh], cs], Sst[g][H[hh], :], U.RPp[H[hh], cs], start=False, stop=True, skip=True)
                    pS = ubank(U)
                    for hh in range(2):
                        k.mm(pS[H[hh], 0:64], U.MT[H[hh], c2, :], Sst[g][H[hh], :])
                    k.tt('dve', Sst[g][:], pS[:, 0:64], U.NN[:, c2, :], ALU.add)
                    yield
                k.copy('act', P.Yb[:, sl], pY[:, 0:128])
                yield

            def block_gen(P, U, d, g, n):
                tiles = list(range(n // 128))
                if d == 1:
                    tiles = tiles[::-1]
                for ti in tiles:
                    yield from unit(P, U, d, g, ti)

            def finalize(P, d, t0, n, g, sga, sgb):
                rows = slice(g * 128, (g + 1) * 128)
                if d == 0:
                    k.dma(y0T[rows, t0:t0 + n], P.Yb[:, :n], q='act')
                    k.dma(bv0T[rows, t0:t0 + n], P.bv[:, :n], q='act')
                    return
                k.dma(P.t1[:, :n], y0T[rows, t0:t0 + n])
                k.dma(P.t2[:, :n], bv0T[rows, t0:t0 + n])
                k.tt('pool', P.Yb[:, :n], P.Yb[:, :n], P.t1[:, :n], ALU.add)
                k.tt('pool', P.bv[:, :n], P.bv[:, :n], P.t2[:, :n], ALU.add)
                pq = prep_bank()
                k.mm(pq[:, :n], ones64, P.Yb[:, :n])
                k.copy('act', P.e1[:, :n], pq[:, :n])
                k.tt('pool', P.kk[:, :n], P.Yb[:, :n], P.Yb[:, :n], ALU.mult)
                pq2 = prep_bank()
                k.mm(pq2[:, :n], ones64, P.kk[:, :n])
                k.tt('pool', P.e3[:, :n], P.e1[:, :n], P.e1[:, :n], ALU.mult)
                k.tt('dve', P.e3[:, :n], pq2[:, :n], P.e3[:, :n], ALU.subtract)
                k.act(P.e3[:, :n], P.e3[:, :n], AF.Sqrt, bias=gneps[:, 0:1])
                k.recip(P.e3[:, :n], P.e3[:, :n])
                k.tt('pool', P.kd[:, :n], P.Yb[:, :n], P.e1[:, :n], ALU.subtract)
                k.tt('pool', P.kd[:, :n], P.kd[:, :n], P.e3[:, :n], ALU.mult)
                k.ts('pool', P.kd[:, :n], P.kd[:, :n], V('lnx_g', g), ALU.mult, V('lnx_b', g), ALU.add)
                k.tt('pool', P.kd[:, :n], P.kd[:, :n], P.bv[:, :n], ALU.add)
                pq3 = prep_bank()
                k.mm(pq3[:, :n], g2s[:, 0, rows], sga[:, :n], start=True, stop=False)
                k.mm(pq3[:, :n], g2s[0:32, 1, rows], sgb[0:32, :n], start=False, stop=True)
                k.tt('dve', P.e1[:, :n], P.kd[:, :n], pq3[:, :n], ALU.mult)
                k.dma(ogT[rows, t0:t0 + n], P.e1[:, :n], q='act')

            ctxb = [b_ for b_ in blocks if b_[2] == 0]
            latb = [b_ for b_ in blocks if b_[2] == 1]
            for d in range(2):
                for g in range(8):
                    k.memset('pool', Sst[g][:].bitcast(F32), 0.0)
                order = (ctxb + latb) if d == 0 else (ctxb[::-1] + latb[::-1])
                for bi, (t0, n, seg) in enumerate(order):
                    tw, awt, sga, sgb = lor[0]
                    k.dma(tw[:, :n], zT[24 * 128:25 * 128, t0:t0 + n])
                    k.dma(awt[:, :n], zT[25 * 128:26 * 128, t0:t0 + n])
                    if d == 1:
                        k.dma(sga[:, :n], zT[26 * 128:27 * 128, t0:t0 + n])
                        k.dma(sgb[:, :n], zT[27 * 128:28 * 128, t0:t0 + n])
                    for g0 in range(0, 8, NS):
                        gens = []
                        for s in range(NS):
                            prep(Ps[s], d, t0, n, g0 + s, tw, awt)
                            gens.append(block_gen(Ps[s], Us[s], d, g0 + s, n))
                        alive = list(range(NS))
                        while alive:
                            for s in list(alive):
                                try:
                                    next(gens[s])
                                except StopIteration:
                                    alive.remove(s)
                        for s in range(NS):
                            finalize(Ps[s], d, t0, n, g0 + s, sga, sgb)
            k.pop()
            if stop_after == 'C':
                break
            act_blocks = [b_ for b_ in blocks if not (last and b_[2] == 0)]
            k.push()
            cza = k.sb(f"cza{l}", [128, T])
            czb = k.sb(f"czb{l}", [128, T])
            cacc = [k.sb(f"cacc{l}_{i}", [128, T]) for i in range(2)]
            tlo = CTX if last else 0
            for c in range(NCH):
                k.dma(cza[:, tlo:T], zT[(28 + c) * 128:(29 + c) * 128, tlo:T])
                k.dma(czb[:, tlo:T], zT[(36 + c) * 128:(37 + c) * 128, tlo:T])
                k.act(czb[:, tlo:T], czb[:, tlo:T], AF.Sigmoid)
                k.tt('pool', cza[:, tlo:T], cza[:, tlo:T], czb[:, tlo:T], ALU.mult)
                segs = [(CTX, SEQ, GRID_W)] + ([] if last else [(0, CTX, 1)])
                for (s0, sl_, stride) in segs:
                    used = [False, False]
                    for kk_ in range(CONV_K):
                        off = (kk_ - CONV_K // 2) * stride
                        lo, hi = max(0, -off), min(sl_, sl_ - off)
                        if hi <= lo:
                            continue
                        ai = 0 if kk_ == CONV_K // 2 else 1 + 0 * kk_
                        ai = kk_ % 2 if kk_ != CONV_K // 2 else 0
                        e_ = 'dve' if ai == 0 else 'pool'
                        wcol = V('conv_w', c * CONV_K + kk_)
                        if kk_ == CONV_K // 2:
                            pass
                        dst = cacc[ai][:, s0 + lo:s0 + hi]
                        srcu = cza[:, s0 + lo + off:s0 + hi + off]
                        if not used[ai]:
                            k.memset(e_, cacc[ai][:, s0:s0 + sl_], 0.0)
                            used[ai] = True
                        k.stt(e_, dst, srcu, wcol, dst, ALU.mult, ALU.add)
                    if used[1]:
                        k.tt('dve', cacc[0][:, s0:s0 + sl_], cacc[0][:, s0:s0 + sl_], cacc[1][:, s0:s0 + sl_], ALU.add)
                k.dma(cvT[c * 128:(c + 1) * 128, tlo:T], cacc[0][:, tlo:T], q='act')
            k.pop()
            if stop_after == 'D':
                break

            k.push()
            wstage = k.sb(f"wstage{l}", [128, NCH, D])
            wts = {}
            for nme, src_w in (('oA', w_oA), ('oB', w_oB), ('out', w_out)):
                wts[nme] = k.sb(f"w{nme}{l}", [128, NCH, D], BF16)
                k.dma(wstage[:], src_w[l])
                k.copy('pool', wts[nme][:], wstage[:])
            lneps = k.sb(f"lneps{l}", [128, 1])
            k.memset('dve', lneps[:], LN_EPS)
            cvb = k.sb(f"cvb{l}", [128, NCH, 512])
            ogb = k.sb(f"ogb{l}", [128, NCH, 512])
            xbe = k.sb(f"xbe{l}", [128, NCH, 512])
            sB = k.sb(f"sB{l}", [128, NCH, 512], BF16)
            oB = k.sb(f"oB{l}", [128, NCH, 512], BF16)
            mTb = k.sb(f"mTb{l}", [128, NCH, 512], BF16)
            gat = [k.sb(f"gat{l}_{i}", [128, 512]) for i in range(4)]
            tE = [k.sb(f"tE{l}_{i}", [128, 512]) for i in range(4)]
            mean = k.sb(f"mean{l}", [128, 512])
            rsd = k.sb(f"rsd{l}", [128, 512])
            for (t0, n, seg) in act_blocks:
                sg = 1 - seg
                k.dma(cvb[:, :, :n], cvT[:, t0:t0 + n].rearrange("(c p) t -> p c t", p=128))
                k.dma(ogb[:, :, :n], ogT[:, t0:t0 + n].rearrange("(c p) t -> p c t", p=128))
                k.dma(xbe[:, :, :n], xres[:, t0:t0 + n].rearrange("(c p) t -> p c t", p=128))
                pm_, pq_ = pb[0], pb[1]
                for c in range(NCH):
                    k.mm(pm_[:, :n], onesD, cvb[:, c, :n], start=(c == 0), stop=(c == NCH - 1))
                for c in range(NCH):
                    k.act(tE[c % 2][:, :n], cvb[:, c, :n], AF.Square)
                    k.mm(pq_[:, :n], onesD, tE[c % 2][:, :n], start=(c == 0), stop=(c == NCH - 1))
                k.copy('act', mean[:, :n], pm_[:, :n])
                k.tt('pool', rsd[:, :n], mean[:, :n], mean[:, :n], ALU.mult)
                k.tt('dve', rsd[:, :n], pq_[:, :n], rsd[:, :n], ALU.subtract)
                k.act(rsd[:, :n], rsd[:, :n], AF.Sqrt, bias=lneps[:, 0:1])
                k.recip(rsd[:, :n], rsd[:, :n])
                for c in range(NCH):
                    e_ = k.ve()
                    t_ = tE[2 + c % 2]
                    k.tt(e_, t_[:, :n], cvb[:, c, :n], mean[:, :n], ALU.subtract)
                    k.tt(e_, t_[:, :n], t_[:, :n], rsd[:, :n], ALU.mult)
                    k.ts(e_, t_[:, :n], t_[:, :n], V('cnorm_g', c), ALU.mult, V('cnorm_b', c), ALU.add)
                    k.act(sB[:, c, :n], t_[:, :n], AF.Silu)
                    k.copy(e_, oB[:, c, :n], ogb[:, c, :n])
                for oc in range(NCH):
                    ocs = slice(oc * 128, (oc + 1) * 128)
                    ga_, gb_ = gat[(oc % 2) * 2], gat[(oc % 2) * 2 + 1]
                    k.dma(ga_[:, :n], zT[(44 + oc) * 128:(45 + oc) * 128, t0:t0 + n])
                    k.dma(gb_[:, :n], zT[(52 + oc) * 128:(53 + oc) * 128, t0:t0 + n])
                    k.act(ga_[:, :n], ga_[:, :n], AF.Sigmoid, bias=V('gate_b', oc))
                    k.act(gb_[:, :n], gb_[:, :n], AF.Sigmoid, bias=V('gate_b', 8 + oc))
                    pa_, pb_ = pb[2 + (oc % 2) * 2], pb[3 + (oc % 2) * 2]
                    for c in range(NCH):
                        k.mm(pa_[:, :n], wts['oA'][:, c, ocs], oB[:, c, :n], start=(c == 0), stop=(c == NCH - 1))
                    for c in range(NCH):
                        k.mm(pb_[:, :n], wts['oB'][:, c, ocs], sB[:, c, :n], start=(c == 0), stop=(c == NCH - 1))
                    k.tt('dve', ga_[:, :n], pa_[:, :n], ga_[:, :n], ALU.mult)
                    k.tt('dve', gb_[:, :n], pb_[:, :n], gb_[:, :n], ALU.mult)
                    k.tt('pool', mTb[:, oc, :n], ga_[:, :n], gb_[:, :n], ALU.add)
                for oc in range(NCH):
                    ocs = slice(oc * 128, (oc + 1) * 128)
                    po_ = pb[6 + oc % 2]
                    for c in range(NCH):
                        k.mm(po_[:, :n], wts['out'][:, c, ocs], mTb[:, c, :n], start=(c == 0), stop=(c == NCH - 1))
                    k.stt('dve', xbe[:, oc, :n], po_[:, :n], modT[:, 2 * 8 + oc, sg:sg + 1], xbe[:, oc, :n], ALU.mult, ALU.add)
                k.dma(xres[:, t0:t0 + n].rearrange("(c p) t -> p c t", p=128), xbe[:, :, :n], q='act')
            k.pop()
            if stop_after == 'E':
                break
            k.push()
            cf = [k.sb(f"pcf{l}_{i}", [128, 4096]) for i in range(2)]
            cbf = [k.sb(f"pcb{l}_{i}", [128, 4096], BF16) for i in range(2)]
            ci = 0
            for grp in range(32):
                for which in range(2):
                    f_, b_ = cf[ci % 2], cbf[ci % 2]
                    if which == 0:
                        k.dma(f_[:, :].rearrange("p (a b) -> p a b", a=NCH), puT[l, grp])
                    else:
                        k.dma(f_[:, :].rearrange("p (a b) -> p a b", a=4), pv[l, grp])
                    k.copy('pool' if ci % 2 else 'dve', b_[:], f_[:])
                    if which == 0:
                        k.dma(puTb[grp], b_[:, :].rearrange("p (a b) -> p a b", a=NCH), q='act')
                    else:
                        k.dma(pvb[grp], b_[:, :].rearrange("p (a b) -> p a b", a=4), q='act')
                    ci += 1
            k.pop()
            k.push()
            wqs = k.sb(f"wqs{l}", [128, NCH, 2 * D], BF16)
            skTs = k.sb(f"skTs{l}", [128, 16, 128], BF16)
            identb = k.sb(f"identb{l}", [128, 128], BF16)
            k.copy('dve', identb[:], ident)
            k.push()
            wqst = [k.sb(f"wqst{l}_{i}", [128, NCH, 512]) for i in range(2)]
            for j in range(4):
                k.dma(wqst[j % 2][:], w_q[l][:, :, j * 512:(j + 1) * 512])
                k.copy('pool' if j % 2 else 'dve', wqs[:, :, j * 512:(j + 1) * 512], wqst[j % 2][:])
            skst = k.sb(f"skst{l}", [128, 16, 128])
            k.dma(skst[:], skT[l].rearrange("q d j -> d q j"))
            k.copy('dve', skTs[:], skst[:])
            k.pop()
            Gt = [k.sb(f"G{l}_{i}", [128, NEXP], BF16) for i in range(2)]
            xbf = k.sb(f"xbf{l}", [128, NCH, 256])
            h2 = k.sb(f"h2{l}", [128, NCH, 256], BF16)
            qT = k.sb(f"qT{l}", [128, 16, 256], BF16)
            ssb = k.sb(f"ssb{l}", [128, 16, 128])
            Eb = ssb
            T16 = k.sb(f"T16{l}", [128, 16, 16])
            tmpk = k.sb(f"tmpk{l}", [128, 128])
            negm = k.sb(f"negm{l}", [128, 16])
            cand = k.sb(f"cand{l}", [128, 256])
            candt = k.sb(f"candt{l}", [128, 256])
            c16 = k.sb(f"c16{l}", [128, 8, 16])
            w16 = k.sb(f"w16{l}", [128, 8, 16])
            smx = k.sb(f"smx{l}", [128, 8])
            Zs = k.sb(f"Zs{l}", [128, 8])
            rZ = k.sb(f"rZ{l}", [128, 8])
            thn = k.sb(f"thn{l}", [128, 8])
            Pq = [k.sb(f"Pq{l}_{i}", [128, 8, 128]) for i in range(2)]
            Gh = [k.sb(f"Gh{l}_{i}", [128, 8, 128], BF16) for i in range(2)]
            UTs = [k.sb(f"UTs{l}_{i}", [128, NCH, 512], BF16) for i in range(2)]
            Vs = [k.sb(f"Vs{l}_{i}", [128, 4, D], BF16) for i in range(2)]
            gab = [k.sb(f"gab{l}_{i}", [128, 256]) for i in range(2)]
            WT = [k.sb(f"WT{l}_{i}", [128, 256], BF16) for i in range(2)]
            pblocks = [b_ for b_ in token_blocks(CTX, SEQ, bs=256) if not (last and b_[2] == 0)]
            for (t0, n, seg) in pblocks:
                sg = 1 - seg
                ntile = n // 128
                k.dma(xbf[:, :, :n], xres[:, t0:t0 + n].rearrange("(c p) t -> p c t", p=128))
                modulate_block(h2, xbf, n, 1, sg, f"F{l}")
                for qc in range(16):
                    pq = pb[4 + qc % 4]
                    for dh in range(NCH):
                        k.mm(pq[:, :n], wqs[:, dh, qc * 128:(qc + 1) * 128], h2[:, dh, :n], start=(dh == 0), stop=(dh == NCH - 1))
                    k.copy('act' if qc % 2 else 'dve', qT[:, qc, :n], pq[:, :n])
                for ti in range(ntile):
                    tsl = slice(ti * 128, (ti + 1) * 128)
                    for qc in range(16):
                        k.mm(pb[qc // 4][:, (qc % 4) * 128:(qc % 4 + 1) * 128], qT[:, qc, tsl], skTs[:, qc, :])
                    for b4 in range(4):
                        k.copy('act' if b4 % 2 else 'dve', ssb[:, b4 * 4:(b4 + 1) * 4, :],
                               pb[b4][:, :].rearrange("p (q j) -> p q j", q=4))
                    for qc in range(16):
                        k.max8(T16[:, qc, 0:8], ssb[:, qc, :])
                        k.match_replace(tmpk[:], T16[:, qc, 0:8], ssb[:, qc, :], -1e30)
                        k.max8(T16[:, qc, 8:16], tmpk[:])
                    T16v = T16[:, :, :].rearrange("p (h c) k -> p h c k", c=2)
                    for h in range(PH):
                        k.tt('pool', cand[:, :].rearrange("p (a b) -> p a b", a=16),
                             T16v[:, h, 0, :].unsqueeze(2).to_broadcast([128, 16, 16]),
                             T16v[:, h, 1, :].unsqueeze(1).to_broadcast([128, 16, 16]), ALU.add)
                        k.max8(c16[:, h, 0:8], cand[:, :])
                        k.match_replace(candt[:], c16[:, h, 0:8], cand[:, :], -1e30)
                        k.max8(c16[:, h, 8:16], candt[:])
                    k.tt('pool', smx[:], T16v[:, :, 0, 0], T16v[:, :, 1, 0], ALU.add)
                    k.tt('pool', w16[:], c16[:], smx[:, :].unsqueeze(2).to_broadcast([128, 8, 16]), ALU.subtract)
                    k.act(w16[:], w16[:], AF.Exp)
                    k.reduce('dve', Zs[:], w16[:], AX.X, ALU.add)
                    k.recip(rZ[:], Zs[:])
                    k.stt('pool', thn[:], w16[:, :, 15], 0.999, rZ[:], ALU.mult, ALU.mult)
                    k.ts('pool', negm[:], T16[:, :, 0], -1.0, ALU.mult)
                    for qc in range(16):
                        k.act(Eb[:, qc, :], ssb[:, qc, :], AF.Exp, bias=negm[:, qc:qc + 1])
                    for h in range(PH):
                        k.ts('pool', Eb[:, 2 * h, :], Eb[:, 2 * h, :], rZ[:, h:h + 1], ALU.mult)
                    cnt_p = 0
                    for e16 in range(16):
                        isl = slice(e16 * 8, (e16 + 1) * 8)
                        Gs = Gt[ti][:, e16 * 1024:(e16 + 1) * 1024].rearrange("p (i j) -> p i j", i=8)
                        for h in range(PH):
                            P_, Gh_ = Pq[cnt_p % 2], Gh[cnt_p % 2]
                            cnt_p += 1
                            k.tt('pool', P_[:], Eb[:, 2 * h, isl].unsqueeze(2).to_broadcast([128, 8, 128]),
                                 Eb[:, 2 * h + 1, :].unsqueeze(1).to_broadcast([128, 8, 128]), ALU.mult)
                            if h == 0:
                                k.stt('dve', Gs, P_[:], thn[:, h:h + 1], P_[:], ALU.is_ge, ALU.mult)
                            else:
                                k.stt('dve', Gh_[:], P_[:], thn[:, h:h + 1], P_[:], ALU.is_ge, ALU.mult)
                                k.tt('dve' if h % 2 else 'pool', Gs, Gs, Gh_[:], ALU.add)
                for grp in range(32):
                    UT_, V_ = UTs[grp % 2], Vs[grp % 2]
                    k.dma(UT_[:], puTb[grp])
                    k.dma(V_[:], pvb[grp])
                    for ec in range(4):
                        e = grp * 4 + ec
                        pa = pb[e % 2]
                        for dh in range(NCH):
                            k.mm(pa[:, :n], UT_[:, dh, ec * 128:(ec + 1) * 128], h2[:, dh, :n], start=(dh == 0), stop=(dh == NCH - 1))
                        ga_ = gab[e % 2]
                        k.act(ga_[:, :n], pa[:, :n], AF.Gelu)
                        pgb = pb[2 + e % 2][:, :].bitcast(BF16)
                        for ti in range(ntile):
                            k.tr(pgb[:, ti * 128:(ti + 1) * 128], Gt[ti][:, e * 128:(e + 1) * 128], identb[:])
                        WT_ = WT[e % 2]
                        k.tt('dve', WT_[:, :n], ga_[:, :n], pgb[:, :n], ALU.mult)
                        for oc in range(NCH):
                            acc = pb[4 + oc // 2][:, (oc % 2) * 256:(oc % 2) * 256 + n]
                            k.mm(acc, V_[:, ec, oc * 128:(oc + 1) * 128], WT_[:, :n],
                                 start=(e == 0 and oc % 2 == 0), stop=(e == 127), skip=True)
                for oc in range(NCH):
                    acc = pb[4 + oc // 2][:, (oc % 2) * 256:(oc % 2) * 256 + n]
                    k.stt('dve', xbf[:, oc, :n], acc, modT[:, 5 * 8 + oc, sg:sg + 1], xbf[:, oc, :n], ALU.mult, ALU.add)
                k.dma(xres[:, t0:t0 + n].rearrange("(c p) t -> p c t", p=128), xbf[:, :, :n], q='act')
            k.pop()
            if stop_after == 'F':
                break

        if stop_after is None:
            k.push()
            l = NL
            xfb = [k.sb(f"xfb{i}", [128, NCH, 512]) for i in range(2)]
            for bi, (t0, n, seg) in enumerate([b_ for b_ in blocks if b_[2] == 1]):
                xb_ = xfb[bi % 2]
                k.dma(xb_[:, :, :n], xres[:, t0:t0 + n].rearrange("(c p) t -> p c t", p=128))
                pss = pb[bi % 2]
                for c in range(NCH):
                    k.act(sqs[:, :n], xb_[:, c, :n], AF.Square)
                    k.mm(pss[:, :n], onesD, sqs[:, :n], start=(c == 0), stop=(c == NCH - 1))
                k.act(rstd[:, :n], pss[:, :n], AF.Sqrt, bias=epsn[:, 0:1])
                k.recip(rstd[:, :n], rstd[:, :n])
                for c in range(NCH):
                    k.stt(k.ve(), xb_[:, c, :n], xb_[:, c, :n], V('final_g', c), rstd[:, :n], ALU.mult, ALU.mult)
                k.dma(outT[:, t0 - CTX:t0 - CTX + n].rearrange("(c p) t -> p c t", p=128), xb_[:, :, :n], q='act')
            k.pop()
        S.finish()
        S.emit()
        nc._dbg = dbg
        nc._nops = S.nops
    return nc


def _fm(v):
    v = np.asarray(v, np.float32).reshape(-1, 128)
    return np.ascontiguousarray(v.T)


def make_consts():
    c = np.zeros((128, 10, 128), np.float32)
    i = np.arange(128)[:, None]
    t = np.arange(128)[None, :]
    same = (i // 64) == (t // 64)
    c[:, 0, :] = (i == t)
    c[:, 1, :] = same
    c[:, 2, :] = 1.0 / D
    c[:, 3, :] = same & (i < t)
    c[:, 4, :] = same & (i <= t)
    c[:, 5, :] = same & (i > t)
    c[:, 6, :] = same & (i >= t)
    c[:, 7, :] = ((i % 64) == t)
    c[:, 8, :] = same / 64.0
    return c


def prepare_shared(inp, NL):
    f32 = lambda a: np.asarray(a, np.float32)
    sh = {}
    ada_w = f32(inp['ada_w'])
    sh['ada_w'] = np.ascontiguousarray(ada_w.reshape(NL, 8, 128, 48, 128).transpose(0, 3, 2, 1, 4))
    vec = np.zeros((NL, 128, NV), np.float32)

    def put(l, name, arr):
        o, w = VEC[name]
        assert arr.shape == (128, w), (name, arr.shape, w)
        vec[l, :, o:o + w] = arr
    for l in range(NL):
        for nme in ('norm1_g', 'norm2_g', 'k_k', 'k_a', 'lnx_g', 'lnx_b', 'cnorm_g', 'cnorm_b', 'gate_b'):
            put(l, nme, _fm(f32(inp[nme])[l]))
        put(l, 'r_k', _fm(f32(inp['r_k'])[l].reshape(-1)))
        put(l, 'w0', _fm(f32(inp['w0'])[l].reshape(-1)))
        put(l, 'a0', _fm(f32(inp['a0'])[l].reshape(-1)))
        put(l, 'final_g', _fm(f32(inp['final_g'])))
        mu = np.zeros((2, 3584), np.float32)
        mu[:, :3488] = f32(inp['shift_mu'])[l]
        put(l, 'mu0', _fm(mu[0]))
        put(l, 'mu1', _fm(mu[1]))
        put(l, 'ada_b', _fm(f32(inp['ada_b'])[l]))
        cw = f32(inp['conv_w'])[l]
        put(l, 'conv_w', np.ascontiguousarray(cw.T.reshape(8, 128, CONV_K).transpose(1, 0, 2)).reshape(128, 8 * CONV_K))
    sh['vec'] = vec
    w_in = f32(inp['w_in'])
    wp = np.zeros((NL, D, P_IN_PAD), np.float32)
    wp[:, :, :3488] = w_in[:, :, :3488]
    wp[:, :, 3584:] = w_in[:, :, 3488:]
    sh['w_in'] = np.ascontiguousarray(wp.reshape(NL, 8, 128, NFC, 128).transpose(0, 3, 2, 1, 4))
    sh['w2'] = np.ascontiguousarray(f32(inp['w2']).reshape(NL, 128, D))
    sh['a2'] = np.ascontiguousarray(f32(inp['a2']).reshape(NL, 128, D))
    g2p = np.zeros((NL, 256, D), np.float32)
    g2p[:, :LORA_G] = f32(inp['g2'])
    sh['g2'] = g2p.reshape(NL, 2, 128, D)
    for nme in ('w_oA', 'w_oB', 'w_out'):
        sh[nme] = np.ascontiguousarray(f32(inp[nme]).reshape(NL, 8, 128, D).transpose(0, 2, 1, 3))
    sh['w_q'] = np.ascontiguousarray(f32(inp['w_q']).reshape(NL, 8, 128, 2 * D).transpose(0, 2, 1, 3))
    sh['skT'] = np.ascontiguousarray(f32(inp['sub_keys']).reshape(NL, 16, 128, 128).transpose(0, 1, 3, 2))
    sh['puT'] = np.ascontiguousarray(f32(inp['peer_u']).reshape(NL, 32, 512, 8, 128).transpose(0, 1, 4, 3, 2))
    sh['pv'] = np.ascontiguousarray(f32(inp['peer_v']).reshape(NL, 32, 4, 128, D).transpose(0, 1, 3, 2, 4))
    sh['consts'] = make_consts()
    return sh


def prepare_core(inp, b):
    f32 = lambda a: np.asarray(a, np.float32)
    xT = np.ascontiguousarray(np.concatenate([f32(inp['ctx'])[b], f32(inp['x'])[b]], axis=0).T)
    cc = np.stack([_fm(f32(inp['c'])[b]), _fm(f32(inp['c_ctx']))], axis=-1)
    return {'xT': xT, 'cc': np.ascontiguousarray(cc)}


def kernel(**inputs):
    B, SEQ, _ = inputs['x'].shape
    CTX = inputs['ctx'].shape[1]
    NL = inputs['w_in'].shape[0]
    cfg = dict(CTX=CTX, SEQ=SEQ, NL=NL)
    nc = build(cfg)
    sh = prepare_shared(inputs, NL)
    in_maps = []
    for b in range(B):
        m = dict(sh)
        m.update(prepare_core(inputs, b))
        in_maps.append(m)
    res = run_bass_kernel_spmd(nc, in_maps, core_ids=list(range(B)))
    out = np.stack([np.ascontiguousarray(np.asarray(r['outT']).T) for r in res.results], axis=0)
    return out.astype(np.float32)
```

```python
from contextlib import ExitStack
import math
import numpy as np
import concourse.bass as bass
import concourse.mybir as mybir
from concourse.bass_utils import run_bass_kernel_spmd

F32 = mybir.dt.float32
BF16 = mybir.dt.bfloat16
AF = mybir.ActivationFunctionType
ALU = mybir.AluOpType
AX = mybir.AxisListType

D = 1024
NCH = 8
HEADS = 16
HD = 64
LORA_G = 160
CONV_K = 31
GRID_W = 64
P_IN_PAD = 7680
NFC = 60
PH, PNK, PHALF, PTOPK = 8, 128, 128, 16
NEXP = PNK * PNK
NORM_EPS = 1e-6
LN_EPS = 1e-5
GN_EPS = HD * 1e-5
CH = 64
EXPM05 = math.exp(-0.5)
F32R = mybir.dt.float32r
RWKV_F32R = True


class Sched:
    GEN = 20000

    def __init__(self, nc, stack, ndma=24):
        self.nc = nc
        self.stack = stack
        self.eng = {'pe': nc.tensor, 'act': nc.scalar, 'dve': nc.vector, 'pool': nc.gpsimd, 'sp': nc.sync}
        self.prog = {e: [] for e in self.eng}
        self.cnt = {e: 0 for e in self.eng}
        self.gen = {e: -1 for e in self.eng}
        self.semh = {}
        for e in self.eng:
            self._newgen(e)
        self.ndma = ndma
        self.dmaval = [0] * ndma
        self.dmarr = 0
        for i in range(ndma):
            self.semh[('dma', i)] = stack.enter_context(nc.semaphore(f"sdma{i}"))
        self.waited = {}
        self.lastw = {}
        self.readers = {}
        self.nops = 0

    def _newgen(self, e):
        self.gen[e] += 1
        self.cnt[e] = 0
        self.semh[(e, self.gen[e])] = self.stack.enter_context(self.nc.semaphore(f"s{e}{self.gen[e]}"))

    def _wait(self, eng, key, val):
        if self.waited.get((eng, key), 0) >= val:
            return
        self.waited[(eng, key)] = val
        self.prog[eng].append(('wait', self.semh[key], val))

    def _deps(self, eng, reads, writes):
        toks = {}

        def add(d):
            for k, v in d.items():
                if toks.get(k, 0) < v:
                    toks[k] = v
        for b in reads:
            add(self.lastw.get(b, {}))
        for b in writes:
            add(self.lastw.get(b, {}))
            add(self.readers.get(b, {}))
        for k, v in toks.items():
            if eng == 'pe' and k[0] == 'pe':
                continue
            self._wait(eng, k, v)

    def _record(self, key, val, reads, writes):
        for b in writes:
            self.lastw[b] = {key: val}
            self.readers[b] = {}
        for b in reads:
            if b in writes:
                continue
            r = self.readers.setdefault(b, {})
            if r.get(key, 0) < val:
                r[key] = val

    def op(self, eng, fn, reads, writes):
        pr = [r for r in reads if r.startswith('pb') and r not in writes]
        if pr:
            writes = list(writes) + pr
        self._deps(eng, reads, writes)
        if self.cnt[eng] >= self.GEN:
            self._newgen(eng)
        self.cnt[eng] += 1
        key = (eng, self.gen[eng])
        val = self.cnt[eng]
        self.prog[eng].append(('op', fn, self.semh[key]))
        self._record(key, val, reads, writes)
        self.nops += 1

    def dma(self, q, out, in_, reads=None, writes=None):
        reads = [in_.tensor.name] if reads is None else reads
        writes = [out.tensor.name] if writes is None else writes
        self._deps(q, reads, writes)
        i = self.dmarr
        self.dmarr = (i + 1) % self.ndma
        key = ('dma', i)
        self._wait(q, key, self.dmaval[i])
        self.dmaval[i] += 16
        self.prog[q].append(('dma', out, in_, self.semh[key]))
        self._record(key, self.dmaval[i], reads, writes)
        self.nops += 1

    def barrier(self):
        for e in self.eng:
            for o in self.eng:
                if o != e and self.cnt[o] > 0:
                    self._wait(e, (o, self.gen[o]), self.cnt[o])
            for i in range(self.ndma):
                if self.dmaval[i] > 0:
                    self._wait(e, ('dma', i), self.dmaval[i])

    def finish(self, q='sp'):
        for i in range(self.ndma):
            self._wait(q, ('dma', i), self.dmaval[i])
        for e in self.eng:
            if e != q and self.cnt[e] > 0:
                self._wait(q, (e, self.gen[e]), self.cnt[e])

    def emit(self):
        nc = self.nc
        prog = self.prog

        def replay(e, items):
            for it in items:
                if it[0] == 'wait':
                    e.wait_ge(it[1], it[2])
                elif it[0] == 'op':
                    it[1](e).then_inc(it[2], 1)
                else:
                    e.dma_start(out=it[1], in_=it[2]).then_inc(it[3], 16)
        with nc.Block() as block:
            @block.tensor
            def _(e):
                replay(e, prog['pe'])

            @block.scalar
            def _(e):
                replay(e, prog['act'])

            @block.vector
            def _(e):
                replay(e, prog['dve'])

            @block.gpsimd
            def _(e):
                replay(e, prog['pool'])

            @block.sync
            def _(e):
                replay(e, prog['sp'])


def _nm(*aps):
    out = []
    for a in aps:
        if hasattr(a, 'tensor'):
            n = a.tensor.name
            if n not in out:
                out.append(n)
    return out


class K:
    SB_BASE, SB_LIMIT = 16512, 229344

    def __init__(self, nc, S, stack):
        self.nc, self.S, self.stack = nc, S, stack
        self.rr = 0
        self.top = self.SB_BASE
        self.marks = []
        self.uid = 0

    def sb(self, name, shape, dt=F32):
        isz = 2 if dt == BF16 else 4
        size = isz
        for s_ in shape[1:]:
            size *= s_
        size = (size + 63) // 64 * 64
        off = self.top
        self.top += size
        assert self.top <= self.SB_LIMIT, f"SBUF arena overflow allocating {name} {shape}: top={self.top}"
        self.uid += 1
        return self.nc.alloc_sbuf_tensor_at(f"{name}_u{self.uid}", list(shape), dt, offset=off)

    def push(self):
        self.marks.append(self.top)

    def pop(self):
        self.top = self.marks.pop()
        self.S.barrier()

    def ps(self, name, shape, dt=F32):
        return self.stack.enter_context(self.nc.psum_tensor(name, list(shape), dt))

    def dram(self, name, shape, dt=F32, kind="Internal"):
        return self.nc.dram_tensor(name, list(shape), dt, kind=kind)

    def ve(self):
        self.rr ^= 1
        return 'dve' if self.rr else 'pool'

    def mm(self, out, lhsT, rhs, start=True, stop=True, skip=False):
        if skip:
            self.S.op('pe', lambda e: e.matmul(out, lhsT, rhs, start=start, stop=stop, skip_group_check=True),
                      _nm(lhsT, rhs), _nm(out))
        else:
            self.S.op('pe', lambda e: e.matmul(out, lhsT, rhs, start=start, stop=stop), _nm(lhsT, rhs), _nm(out))

    def tr(self, out, in_, ident):
        self.S.op('pe', lambda e: e.transpose(out, in_, ident), _nm(in_, ident), _nm(out))

    def act(self, out, in_, func, bias=None, scale=None, accum_out=None):
        kw = {}
        if bias is not None:
            kw['bias'] = bias
        if scale is not None:
            kw['scale'] = scale
        if accum_out is not None:
            kw['accum_out'] = accum_out
        self.S.op('act', lambda e: e.activation(out, in_, func, **kw), _nm(in_, bias, scale), _nm(out, accum_out))

    def tt(self, eng, out, in0, in1, op):
        self.S.op(eng, lambda e: e.tensor_tensor(out, in0, in1, op), _nm(in0, in1), _nm(out))

    def ts(self, eng, out, in0, s1, op0, s2=None, op1=None, accum_out=None):
        kw = {}
        if accum_out is not None:
            kw['accum_out'] = accum_out
        if op1 is None:
            self.S.op(eng, lambda e: e.tensor_scalar(out, in0, s1, None, op0, **kw), _nm(in0, s1), _nm(out, accum_out))
        else:
            self.S.op(eng, lambda e: e.tensor_scalar(out, in0, s1, s2, op0, op1, **kw), _nm(in0, s1, s2), _nm(out, accum_out))

    def stt(self, eng, out, in0, scalar, in1, op0, op1, accum_out=None):
        eng = 'dve'
        kw = {}
        if accum_out is not None:
            kw['accum_out'] = accum_out
        self.S.op(eng, lambda e: e.scalar_tensor_tensor(out, in0, scalar, in1, op0, op1, **kw),
                  _nm(in0, scalar, in1), _nm(out, accum_out))

    def copy(self, eng, out, in_):
        if eng == 'act':
            self.S.op('act', lambda e: e.copy(out, in_), _nm(in_), _nm(out))
        else:
            self.S.op(eng, lambda e: e.tensor_copy(out, in_), _nm(in_), _nm(out))

    def recip(self, out, in_):
        self.S.op('dve', lambda e: e.reciprocal(out, in_), _nm(in_), _nm(out))

    def max8(self, out, in_):
        self.S.op('dve', lambda e: e.max(out, in_), _nm(in_), _nm(out))

    def match_replace(self, out, in_to_replace, in_values, imm):
        self.S.op('dve', lambda e: e.match_replace(out, in_to_replace, in_values, imm), _nm(in_to_replace, in_values), _nm(out))

    def scan(self, eng, out, d0, d1, init, op0, op1):
        eng = 'dve'
        self.S.op(eng, lambda e: e.tensor_tensor_scan(out, d0, d1, init, op0, op1), _nm(d0, d1), _nm(out))

    def reduce(self, eng, out, in_, axis, op):
        self.S.op(eng, lambda e: e.tensor_reduce(out, in_, axis, op), _nm(in_), _nm(out))

    def memset(self, eng, out, val):
        self.S.op(eng, lambda e: e.memset(out, val), [], _nm(out))

    def dma(self, out, in_, q='sp'):
        self.S.dma(q, out, in_)


VEC = {}
_off = 0
for _n, _w in [('norm1_g', 8), ('norm2_g', 8), ('k_k', 8), ('k_a', 8), ('omka', 8), ('r_k', 8), ('lnx_g', 8), ('lnx_b', 8),
               ('cnorm_g', 8), ('cnorm_b', 8), ('gate_b', 16), ('w0', 16), ('a0', 16), ('final_g', 8),
               ('mu0', 28), ('mu1', 28), ('muc', 28), ('ada_b', 48), ('conv_w', 8 * CONV_K)]:
    VEC[_n] = (_off, _w)
    _off += _w
NV = _off


def token_blocks(CTX, SEQ, bs=512):
    blocks = []
    t = 0
    while t < CTX:
        n = min(bs, CTX - t)
        blocks.append((t, n, 0))
        t += n
    while t < CTX + SEQ:
        n = min(bs, CTX + SEQ - t)
        blocks.append((t, n, 1))
        t += n
    return blocks


def build(cfg):
    CTX, SEQ, NL = cfg['CTX'], cfg['SEQ'], cfg['NL']
    stop_after = cfg.get('stop_after', None)
    T = CTX + SEQ
    nc = bass.Bass("TRN2", target_bir_lowering=False)
    stack = ExitStack()
    with stack:
        S = Sched(nc, stack)
        k = K(nc, S, stack)
        stack.enter_context(nc.allow_low_precision("bf16 matmuls with fp32 accumulation (tolerance allows)"))
        inp = lambda name, shape: nc.dram_tensor(name, list(shape), F32, kind="ExternalInput").ap()
        xT = inp("xT", [D, T])
        cc = inp("cc", [128, NCH, 2])
        ada_w = inp("ada_w", [NL, 48, 128, NCH, 128])
        vec = inp("vec", [NL, 128, NV])
        w_in = inp("w_in", [NL, NFC, 128, NCH, 128])
        w2 = inp("w2", [NL, 128, D])
        a2 = inp("a2", [NL, 128, D])
        g2 = inp("g2", [NL, 2, 128, D])
        w_oA = inp("w_oA", [NL, 128, NCH, D])
        w_oB = inp("w_oB", [NL, 128, NCH, D])
        w_out = inp("w_out", [NL, 128, NCH, D])
        w_q = inp("w_q", [NL, 128, NCH, 2 * D])
        skT = inp("skT", [NL, 16, 128, 128])
        puT = inp("puT", [NL, 32, 128, NCH, 512])
        pv = inp("pv", [NL, 32, 128, 4, D])
        consts = inp("consts", [128, 10, 128])
        outT = nc.dram_tensor("outT", [D, SEQ], F32, kind="ExternalOutput").ap()
        dbg = {}

        def scratch(name, shape, dt=F32):
            kind = "ExternalOutput" if cfg.get('debug') else "Internal"
            t_ = nc.dram_tensor(name, list(shape), dt, kind=kind).ap()
            dbg[name] = t_
            return t_
        xres = scratch("xres", [D, T])
        zT = scratch("zT", [P_IN_PAD, T])
        y0T = scratch("y0T", [D, T])
        bv0T = scratch("bv0T", [D, T])
        ogT = scratch("ogT", [D, T])
        cvT = scratch("cvT", [D, T])
        puTb = scratch("puTb", [32, 128, NCH, 512], BF16)
        pvb = scratch("pvb", [32, 128, 4, D], BF16)

        cst = k.sb("cst", [128, 10, 128])
        k.dma(cst[:], consts)
        ident = cst[:, 0, :]
        onesblk = cst[:, 1, :]
        onesD = cst[:, 2, :]
        identblk = cst[:, 7, 0:64]
        ones64 = cst[:, 8, :]
        rst = k.sb("rst", [128, 512])
        k.memset('dve', rst[:], 1.0)
        k.memset('dve', rst[:].rearrange("p (c j) -> p c j", j=CH)[:, :, 0:1], 0.0)
        mA = k.sb("mA", [128, 2, 2, 128])
        mB = k.sb("mB", [128, 2, 2, 128])
        mT = k.sb("mT", [128, 2, 128])
        for d in range(2):
            k.copy('dve', mA[:, d, 0, :], cst[:, 3 + 2 * d, :])
            k.ts('dve', mA[:, d, 1, :], cst[:, 4 + 2 * d, :], -1.0, ALU.mult)
            k.copy('dve', mB[:, d, 0, :], cst[:, 3 + 2 * d, :])
            k.copy('dve', mB[:, d, 1, :], cst[:, 4 + 2 * d, :])
            k.copy('dve', mT[:, d, :], cst[:, 5 - 2 * d, :])

        pb = [k.ps(f"pb{i}", [128, 512]) for i in range(8)]
        ccs = k.sb("ccs", [128, NCH, 2])
        k.dma(ccs[:], cc)
        k.act(ccs[:], ccs[:], AF.Silu)
        vecs = k.sb("vecs", [128, NV])
        modT = k.sb("modT", [128, 48, 2])

        def V(name, j=0, w=1):
            o, _ = VEC[name]
            return vecs[:, o + j:o + j + w]

        blocks = token_blocks(CTX, SEQ)
        sqs = k.sb("sqs", [128, 512])
        rstd = k.sb("rstd", [128, 512])
        xn = k.sb("xn", [128, 512])
        epsn = k.sb("epsn", [128, 1])
        k.memset('dve', epsn[:], NORM_EPS)
        gm = k.sb("gm", [128, 2, NCH, 2])

        for l in range(NL):
            last = (l == NL - 1)
            src = xT if l == 0 else xres
            k.dma(vecs[:], vec[l])
            if True:
                k.push()
                aw = [k.sb(f"aw{l}_{i}", [128, NCH, 128]) for i in range(2)]
                for j in range(48):
                    a_ = aw[j % 2]
                    k.dma(a_[:], ada_w[l, j])
                    pm = pb[j % 2]
                    for dh in range(NCH):
                        k.mm(pm[:, 0:2], a_[:, dh, :], ccs[:, dh, :], start=(dh == 0), stop=(dh == NCH - 1))
                    k.ts('dve', modT[:, j, :], pm[:, 0:2], V('ada_b', j), ALU.add)
                for w_, (nn, sci) in enumerate((('norm1_g', 1), ('norm2_g', 4))):
                    for c in range(NCH):
                        k.ts('dve', gm[:, w_, c, :], modT[:, sci * 8 + c, :], 1.0, ALU.add, V(nn, c), ALU.mult)
                k.pop()

            def modulate_block(dst, xb, n, which, seg, tagp):
                pss = pb[7]
                for c in range(NCH):
                    k.act(sqs[:, :n], xb[:, c, :n], AF.Square)
                    k.mm(pss[:, :n], onesD, sqs[:, :n], start=(c == 0), stop=(c == NCH - 1))
                k.act(rstd[:, :n], pss[:, :n], AF.Sqrt, bias=epsn[:, 0:1])
                k.recip(rstd[:, :n], rstd[:, :n])
                shi = 0 if which == 0 else 3
                for c in range(NCH):
                    e_ = k.ve()
                    k.tt(e_, xn[:, :n], xb[:, c, :n], rstd[:, :n], ALU.mult)
                    k.ts(e_, dst[:, c, :n], xn[:, :n], gm[:, which, c, seg:seg + 1], ALU.mult,
                         modT[:, shi * 8 + c, seg:seg + 1], ALU.add)


            if True:
                k.push()
                HT = k.sb(f"HT{l}", [128, NCH, T], BF16)
                xb = [k.sb(f"xb{l}_{i}", [128, NCH, 512]) for i in range(2)]
                for bi, (t0, n, seg) in enumerate(blocks):
                    xb_ = xb[bi % 2]
                    k.dma(xb_[:, :, :n], src[:, t0:t0 + n].rearrange("(c p) t -> p c t", p=128))
                    if l == 0:
                        k.dma(xres[:, t0:t0 + n].rearrange("(c p) t -> p c t", p=128), xb_[:, :, :n], q='act')
                    modulate_block(HT[:, :, t0:t0 + n], xb_, n, 0, 1 - seg, f"A{l}")
                wf = [k.sb(f"wf{l}_{i}", [128, NCH, 128]) for i in range(2)]
                wb = [k.sb(f"wb{l}_{i}", [128, NCH, 128], BF16) for i in range(2)]
                zst = [k.sb(f"zst{l}_{i}", [128, 512]) for i in range(4)]
                cnt = 0
                nfc = 28 if (last and False) else NFC
                for fc in range(nfc):
                    wf_, wb_ = wf[fc % 2], wb[fc % 2]
                    k.dma(wf_[:], w_in[l, fc])
                    k.copy('pool', wb_[:], wf_[:])
                    for (t0, n, seg) in blocks:
                        if last and seg == 0 and fc >= 28:
                            continue
                        pz = pb[cnt % 4]
                        for dh in range(NCH):
                            k.mm(pz[:, :n], wb_[:, dh, :], HT[:, dh, t0:t0 + n], start=(dh == 0), stop=(dh == NCH - 1))
                        z_ = zst[cnt % 4]
                        k.copy('act' if cnt % 2 else 'dve', z_[:, :n], pz[:, :n])
                        k.dma(zT[fc * 128:(fc + 1) * 128, t0:t0 + n], z_[:, :n], q='act')
                        cnt += 1
                k.pop()
            if stop_after == 'B':
                break
            k.push()
            k.tt('dve', V('muc', 0, 28), V('mu0', 0, 28), V('mu1', 0, 28), ALU.add)
            k.ts('dve', V('muc', 0, 28), V('muc', 0, 28), -1.0, ALU.mult, 1.0, ALU.add)
            k.ts('dve', V('omka', 0, 8), V('k_a', 0, 8), -1.0, ALU.mult, 1.0, ALU.add)
            TW = T + 4
            zw = [k.sb(f"zw{l}_{i}", [128, TW]) for i in range(2)]
            zo = [k.sb(f"zo{l}_{i}", [128, TW]) for i in range(2)]
            for i in range(2):
                k.memset('pool', zw[i][:, 0:1], 0.0)
                k.memset('pool', zw[i][:, CTX + 1:CTX + 3], 0.0)
                k.memset('pool', zw[i][:, TW - 1:TW], 0.0)

            def b2_load(fc):
                rows = slice(fc * 128, (fc + 1) * 128)
                k.dma(zw[fc % 2][:, 1:1 + CTX], zT[rows, 0:CTX])
                k.dma(zw[fc % 2][:, CTX + 3:CTX + 3 + SEQ], zT[rows, CTX:T])
            b2_load(0)
            for fc in range(28):
                rows = slice(fc * 128, (fc + 1) * 128)
                if fc + 1 < 28:
                    b2_load(fc + 1)
                zw_, zo_ = zw[fc % 2], zo[fc % 2]
                k.ts('dve', zo_[:, 1:TW - 1], zw_[:, 1:TW - 1], V('muc', fc), ALU.mult)
                k.stt('pool', zo_[:, 1:TW - 1], zw_[:, 0:TW - 2], V('mu0', fc), zo_[:, 1:TW - 1], ALU.mult, ALU.add)
                k.stt('dve', zo_[:, 1:TW - 1], zw_[:, 2:TW], V('mu1', fc), zo_[:, 1:TW - 1], ALU.mult, ALU.add)
                if fc == 24:
                    k.act(zo_[:, 1:TW - 1], zo_[:, 1:TW - 1], AF.Tanh)
                if fc in (26, 27):
                    k.act(zo_[:, 1:TW - 1], zo_[:, 1:TW - 1], AF.Sigmoid)
                k.dma(zT[rows, 0:CTX], zo_[:, 1:1 + CTX], q='act')
                k.dma(zT[rows, CTX:T], zo_[:, CTX + 3:CTX + 3 + SEQ], q='act')
            k.pop()
            if stop_after == 'B2':
                break

            k.push()
            w2s = k.sb(f"w2s{l}", [128, D])
            a2s = k.sb(f"a2s{l}", [128, D])
            g2s = k.sb(f"g2s{l}", [128, 2, D])
            k.dma(w2s[:], w2[l])
            k.dma(a2s[:], a2[l])
            k.dma(g2s[:], g2[l].rearrange("c p f -> p c f"))
            RDT = F32R if RWKV_F32R else F32
            Sst = [k.sb(f"Sst{l}_{g}", [128, HD], RDT) for g in range(8)]
            gneps = k.sb(f"gneps{l}", [128, 1])
            k.memset('dve', gneps[:], GN_EPS)
            NS = 2
            hmask = [onesblk[:, 0:1], onesblk[:, 64:65]]

            class Obj:
                pass
            Ps, Us = [], []
            for s in range(NS):
                P = Obj()
                for nm_ in ('rr', 'kx', 'vv', 'kk', 'aa', 'kd', 'bb', 'lw', 'lam', 'pex', 'rem', 'Lb', 'BQ', 'nBQP', 'KQP',
                            'BQ0', 'BQ1', 'KQ0', 'KQ1', 'KP0', 'KP1',
                            't1', 't2', 'e1', 'e3', 'Yb', 'bv'):
                    setattr(P, nm_, k.sb(f"P{l}_{s}_{nm_}", [128, 512],
                                         RDT if nm_ in ('BQ', 'BQ0', 'BQ1', 'KQ0', 'KQ1', 'KP0', 'KP1') else F32))
                P.e2, P.e4, P.Lb = P.aa, P.t1, P.kx
                P.KR = k.sb(f"P{l}_{s}_KR", [128, 2, 512], RDT)
                P.PC = k.sb(f"P{l}_{s}_PC", [128, 8])
                Ps.append(P)
                U = Obj()
                U.TM = k.sb(f"U{l}_{s}_TM", [128, 4, 128], RDT)
                U.ZA = k.sb(f"U{l}_{s}_ZA", [128, 2, 2, 128], RDT)
                U.ZB = k.sb(f"U{l}_{s}_ZB", [128, 2, 2, 128], RDT)
                U.AA = k.sb(f"U{l}_{s}_AA", [128, 2, 128], RDT)
                U.ZZ = [k.sb(f"U{l}_{s}_ZZ{j}", [128, 2, 2, 128], RDT) for j in range(5)]
                U.X = [k.sb(f"U{l}_{s}_X{j}", [128, 2, 128], RDT) for j in range(2)]
                U.MT = k.sb(f"U{l}_{s}_MT", [128, 2, 64], RDT)
                U.NN = k.sb(f"U{l}_{s}_NN", [128, 2, 64], RDT)
                U.RPp = k.sb(f"U{l}_{s}_RPp", [128, 128], RDT)
                U.banks = [pb[2 + 3 * s], pb[3 + 3 * s]]
                U.ybank = pb[4 + 3 * s]
                U.bi = 0
                Us.append(U)
            lor = [[k.sb(f"lor{l}_{i}_{j}", [128, 512]) for j in range(4)] for i in range(1)]
            prep_cnt = [0]

            class KR:
                def __getattr__(self, a):
                    return getattr(k, a)

                def mm(self, out, lhsT, rhs, start=True, stop=True, skip=False):
                    if out.base_partition() != 0 or out.shape[0] != 128:
                        lhsT, rhs = lhsT.bitcast(F32), rhs.bitcast(F32)
                    k.mm(out, lhsT, rhs, start=start, stop=stop, skip=skip)
            kr = KR()

            def prep_bank():
                prep_cnt[0] += 1
                return pb[prep_cnt[0] % 2]

            def ubank(U):
                U.bi += 1
                return U.banks[U.bi % 2]

            def prep(P, d, t0, n, g, tw, awt):
                k.dma(P.rr[:, :n], zT[g * 128:(g + 1) * 128, t0:t0 + n])
                k.dma(P.kx[:, :n], zT[(8 + g) * 128:(9 + g) * 128, t0:t0 + n])
                k.dma(P.vv[:, :n], zT[(16 + g) * 128:(17 + g) * 128, t0:t0 + n])
                k.ts('pool', P.t1[:, :n], P.kx[:, :n], V('k_k', g), ALU.mult)
                k.tt('pool', P.t2[:, :n], P.t1[:, :n], P.t1[:, :n], ALU.mult)
                pq = prep_bank()
                k.mm(pq[:, :n], onesblk, P.t2[:, :n])
                k.act(P.t2[:, :n], pq[:, :n], AF.Sqrt)
                k.ts('dve', P.t2[:, :n], P.t2[:, :n], 1e-12, ALU.max)
                k.recip(P.t2[:, :n], P.t2[:, :n])
                k.tt('pool', P.kk[:, :n], P.t1[:, :n], P.t2[:, :n], ALU.mult)
                ds = slice(d * 64, (d + 1) * 64)
                gs = slice(g * 128, (g + 1) * 128)
                pq = prep_bank()
                k.mm(pq[:, :n], w2s[ds, gs], tw[ds, :n])
                k.act(P.lw[:, :n], pq[:, :n], AF.Sigmoid, bias=V('w0', d * 8 + g))
                k.ts('pool', P.lw[:, :n], P.lw[:, :n], -EXPM05, ALU.mult)
                pq = prep_bank()
                k.mm(pq[:, :n], a2s[ds, gs], awt[ds, :n])
                k.act(P.aa[:, :n], pq[:, :n], AF.Sigmoid, bias=V('a0', d * 8 + g))
                k.ts('pool', P.t1[:, :n], P.aa[:, :n], V('k_a', g), ALU.mult, V('omka', g), ALU.add)
                k.tt('pool', P.kd[:, :n], P.t1[:, :n], P.kx[:, :n], ALU.mult)
                k.tt('pool', P.bb[:, :n], P.aa[:, :n], P.kk[:, :n], ALU.mult)
                k.tt('pool', P.t1[:, :n], P.rr[:, :n], P.kd[:, :n], ALU.mult)
                k.ts('pool', P.t1[:, :n], P.t1[:, :n], V('r_k', g), ALU.mult)
                pq = prep_bank()
                k.mm(pq[:, :n], onesblk, P.t1[:, :n])
                k.tt('dve', P.bv[:, :n], pq[:, :n], P.vv[:, :n], ALU.mult)
                nchk = n // CH
                k.scan('dve', P.lam[:, :n], rst[:, :n], P.lw[:, :n], 0.0, ALU.mult, ALU.add)
                lam3 = P.lam[:, :n].rearrange("p (c j) -> p c j", j=CH)
                tot_b = lam3[:, :, CH - 1:CH].to_broadcast([128, nchk, CH])
                k.tt('pool', P.pex[:, :n], P.lam[:, :n], P.lw[:, :n], ALU.subtract)
                k.tt('pool', P.rem[:, :n].rearrange("p (c j) -> p c j", j=CH), tot_b, lam3, ALU.subtract)
                if d == 0:
                    L, Lex, Lrem = P.lam, P.pex, P.rem
                else:
                    k.tt('pool', P.Lb[:, :n], P.rem[:, :n], P.lw[:, :n], ALU.add)
                    L, Lex, Lrem = P.Lb, P.rem, P.pex
                k.act(P.e1[:, :n], Lex[:, :n], AF.Exp)
                k.tt('pool', P.KR[:, 0, :n], P.kk[:, :n], P.e1[:, :n], ALU.mult)
                k.stt('dve', P.KP0[:, :n], P.kk[:, :n], hmask[0], P.e1[:, :n], ALU.mult, ALU.mult)
                k.stt('dve', P.KP1[:, :n], P.kk[:, :n], hmask[1], P.e1[:, :n], ALU.mult, ALU.mult)
                k.act(P.e2[:, :n], L[:, :n], AF.Exp)
                k.tt('pool', P.KR[:, 1, :n], P.rr[:, :n], P.e2[:, :n], ALU.mult)
                k.act(P.e3[:, :n], L[:, :n], AF.Exp, scale=-1.0)
                k.tt('pool', P.BQ[:, :n], P.bb[:, :n], P.e3[:, :n], ALU.mult)
                k.stt('dve', P.BQ0[:, :n], P.bb[:, :n], hmask[0], P.e3[:, :n], ALU.mult, ALU.mult)
                k.stt('dve', P.BQ1[:, :n], P.bb[:, :n], hmask[1], P.e3[:, :n], ALU.mult, ALU.mult)
                k.stt('dve', P.KQ0[:, :n], P.kd[:, :n], hmask[0], P.e3[:, :n], ALU.mult, ALU.mult)
                k.stt('dve', P.KQ1[:, :n], P.kd[:, :n], hmask[1], P.e3[:, :n], ALU.mult, ALU.mult)
                k.act(P.e4[:, :n], Lrem[:, :n], AF.Exp)
                k.stt('dve', P.nBQP[:, :n], P.bb[:, :n], -1.0, P.e4[:, :n], ALU.mult, ALU.mult)
                k.tt('pool', P.KQP[:, :n], P.kd[:, :n], P.e4[:, :n], ALU.mult)
                k.act(P.PC[:, :nchk], lam3[:, :, CH - 1], AF.Exp)

            def unit(P, U, d, g, ti):
                k = kr
                sl = slice(ti * 128, (ti + 1) * 128)
                H = [slice(0, 64), slice(64, 128)]
                pT = ubank(U)
                pT4 = pT[:, :].rearrange("p (a t) -> p a t", a=4)
                k.tr(pT4[:, 0, :], P.vv[:, sl], ident)
                k.tr(pT4[:, 1, :], P.KR[:, 0, sl].bitcast(F32), ident)
                k.tr(pT4[:, 2, :], P.nBQP[:, sl], ident)
                k.tr(pT4[:, 3, :], P.KQP[:, sl], ident)
                k.copy('act', U.TM[:], pT4)
                yield
                pA = ubank(U)
                pA4 = pA[:, :].rearrange("p (h w t) -> p h w t", h=2, w=2)
                BQh, KQh, KPh = [P.BQ0, P.BQ1], [P.KQ0, P.KQ1], [P.KP0, P.KP1]
                for hh in range(2):
                    k.mm(pA4[:, hh], BQh[hh][:, sl], P.KR[:, :, sl])
                k.tt('dve', U.ZA[:], pA4, mA[:, d].unsqueeze(1).to_broadcast([128, 2, 2, 128]), ALU.mult)
                pB = ubank(U)
                pB4 = pB[:, :].rearrange("p (h w t) -> p h w t", h=2, w=2)
                for hh in range(2):
                    k.mm(pB4[:, hh], KQh[hh][:, sl], P.KR[:, :, sl])
                k.tt('dve', U.ZB[:], pB4, mB[:, d].unsqueeze(1).to_broadcast([128, 2, 2, 128]), ALU.mult)
                pC = ubank(U)
                pC3 = pC[:, 0:256].rearrange("p (h t) -> p h t", h=2)
                for hh in range(2):
                    k.mm(pC3[:, hh], KPh[hh][:, sl], P.BQ[:, sl])
                k.tt('dve', U.AA[:], pC3, mT[:, d].unsqueeze(1).to_broadcast([128, 2, 128]), ALU.mult)
                yield
                Zc = [U.ZA[:, 0, 0, :], U.ZA[:, 1, 0, :]]
                Ac = [U.AA[:, 0, :], U.AA[:, 1, :]]
                Zp = [Zc]
                for lev in range(5):
                    pQ = ubank(U)
                    pQ4 = pQ[:, :].rearrange("p (w h t) -> p w h t", w=2, h=2)
                    for hh in range(2):
                        k.mm(pQ4[:, 0, hh], Ac[hh], Zc[hh])
                        if lev < 4:
                            k.mm(pQ4[:, 1, hh], Zc[hh], Ac[hh])
                    ZZl = U.ZZ[lev]
                    if lev < 4:
                        k.copy('act' if lev % 2 else 'dve', ZZl[:], pQ4)
                    else:
                        k.copy('dve', ZZl[:, 0], pQ4[:, 0])
                    Zc = [ZZl[:, 0, 0, :], ZZl[:, 0, 1, :]]
                    Ac = [ZZl[:, 1, 0, :], ZZl[:, 1, 1, :]]
                    Zp.append(Zc)
                    yield
                pX = ubank(U)
                pX3 = pX[:, 0:256].rearrange("p (h c) -> p h c", h=2)
                for hh in range(2):
                    k.mm(pX3[:, hh, 0:64], U.ZB[:, hh, 0, :], U.TM[:, 0, H[hh]])
                X = U.X[0]
                k.copy('act', X[:, :, 0:64], pX3[:, :, 0:64])
                k.copy('pool', X[:, :, 64:128], U.TM[:, 1, :].rearrange("p (h c) -> p h c", h=2))
                yield
                for lev in range(6):
                    pP = ubank(U)
                    pP3 = pP[:, 0:256].rearrange("p (h c) -> p h c", h=2)
                    for hh in range(2):
                        k.mm(pP3[:, hh, :], Zp[lev][hh], X[:, hh, :])
                    Xn = U.X[(lev + 1) % 2]
                    k.tt('dve', Xn[:], X[:], pP3, ALU.subtract if lev == 0 else ALU.add)
                    X = Xn
                    yield
                pMNs = [ubank(U), ubank(U)]
                for c2 in range(2):
                    cs = H[c2]
                    pMN = pMNs[c2]
                    for hh in range(2):
                        k.mm(pMN[H[hh], 0:64], X[cs, hh, 64:128], U.TM[cs, 2, H[hh]])
                        k.mm(pMN[H[hh], 64:128], U.TM[cs, 3, H[hh]], U.TM[cs, 0, H[hh]], start=True, stop=False)
                        k.mm(pMN[H[hh], 64:128], U.TM[cs, 2, H[hh]], X[cs, hh, 0:64], start=False, stop=True)
                for c2 in range(2):
                    k.stt('dve', U.MT[:, c2, :], identblk, P.PC[:, ti * 2 + c2:ti * 2 + c2 + 1],
                          pMNs[c2][:, 0:64], ALU.mult, ALU.add)
                    k.copy('act', U.NN[:, c2, :], pMNs[c2][:, 64:128])
                pR = ubank(U)
                for hh in range(2):
                    k.mm(pR[H[hh], 0:128], X[:, hh, 64:128], U.ZA[:, hh, 1, :])
                k.tt('dve', U.RPp[:], P.KR[:, 1, sl], pR[:, 0:128], ALU.add)
                pY = U.ybank
                for hh in range(2):
                    k.mm(pY[H[hh], 0:128], U.TM[:, 0, H[hh]], U.ZB[:, hh, 1, :], start=True, stop=False, skip=True)
                    k.mm(pY[H[hh], 0:128], X[:, hh, 0:64], U.ZA[:, hh, 1, :], start=False, stop=False, skip=True)
                yield
                for c2 in ((0, 1) if d == 0 else (1, 0)):
                    cs = H[c2]
                    for hh in range(2):
                        k.mm(pY[H[hh], cs], Sst[g][H[hh], :], U.RPp[H[hh], cs], start=False, stop=True, skip=True)
                    pS = ubank(U)
                    for hh in range(2):
                        k.mm(pS[H[hh], 0:64], U.MT[H[hh], c2, :], Sst[g][H[hh], :])
                    k.tt('dve', Sst[g][:], pS[:, 0:64], U.NN[:, c2, :], ALU.add)
                    yield
                k.copy('act', P.Yb[:, sl], pY[:, 0:128])
                yield

            def block_gen(P, U, d, g, n):
                tiles = list(range(n // 128))
                if d == 1:
                    tiles = tiles[::-1]
                for ti in tiles:
                    yield from unit(P, U, d, g, ti)

            def finalize(P, d, t0, n, g, sga, sgb):
                rows = slice(g * 128, (g + 1) * 128)
                if d == 0:
                    k.dma(y0T[rows, t0:t0 + n], P.Yb[:, :n], q='act')
                    k.dma(bv0T[rows, t0:t0 + n], P.bv[:, :n], q='act')
                    return
                k.dma(P.t1[:, :n], y0T[rows, t0:t0 + n])
                k.dma(P.t2[:, :n], bv0T[rows, t0:t0 + n])
                k.tt('pool', P.Yb[:, :n], P.Yb[:, :n], P.t1[:, :n], ALU.add)
                k.tt('pool', P.bv[:, :n], P.bv[:, :n], P.t2[:, :n], ALU.add)
                pq = prep_bank()
                k.mm(pq[:, :n], ones64, P.Yb[:, :n])
                k.copy('act', P.e1[:, :n], pq[:, :n])
                k.tt('pool', P.kk[:, :n], P.Yb[:, :n], P.Yb[:, :n], ALU.mult)
                pq2 = prep_bank()
                k.mm(pq2[:, :n], ones64, P.kk[:, :n])
                k.tt('pool', P.e3[:, :n], P.e1[:, :n], P.e1[:, :n], ALU.mult)
                k.tt('dve', P.e3[:, :n], pq2[:, :n], P.e3[:, :n], ALU.subtract)
                k.act(P.e3[:, :n], P.e3[:, :n], AF.Sqrt, bias=gneps[:, 0:1])
                k.recip(P.e3[:, :n], P.e3[:, :n])
                k.tt('pool', P.kd[:, :n], P.Yb[:, :n], P.e1[:, :n], ALU.subtract)
                k.tt('pool', P.kd[:, :n], P.kd[:, :n], P.e3[:, :n], ALU.mult)
                k.ts('pool', P.kd[:, :n], P.kd[:, :n], V('lnx_g', g), ALU.mult, V('lnx_b', g), ALU.add)
                k.tt('pool', P.kd[:, :n], P.kd[:, :n], P.bv[:, :n], ALU.add)
                pq3 = prep_bank()
                k.mm(pq3[:, :n], g2s[:, 0, rows], sga[:, :n], start=True, stop=False)
                k.mm(pq3[:, :n], g2s[0:32, 1, rows], sgb[0:32, :n], start=False, stop=True)
                k.tt('dve', P.e1[:, :n], P.kd[:, :n], pq3[:, :n], ALU.mult)
                k.dma(ogT[rows, t0:t0 + n], P.e1[:, :n], q='act')

            ctxb = [b_ for b_ in blocks if b_[2] == 0]
            latb = [b_ for b_ in blocks if b_[2] == 1]
            for d in range(2):
                for g in range(8):
                    k.memset('pool', Sst[g][:].bitcast(F32), 0.0)
                order = (ctxb + latb) if d == 0 else (ctxb[::-1] + latb[::-1])
                for bi, (t0, n, seg) in enumerate(order):
                    tw, awt, sga, sgb = lor[0]
                    k.dma(tw[:, :n], zT[24 * 128:25 * 128, t0:t0 + n])
                    k.dma(awt[:, :n], zT[25 * 128:26 * 128, t0:t0 + n])
                    if d == 1:
                        k.dma(sga[:, :n], zT[26 * 128:27 * 128, t0:t0 + n])
                        k.dma(sgb[:, :n], zT[27 * 128:28 * 128, t0:t0 + n])
                    for g0 in range(0, 8, NS):
                        gens = []
                        for s in range(NS):
                            prep(Ps[s], d, t0, n, g0 + s, tw, awt)
                            gens.append(block_gen(Ps[s], Us[s], d, g0 + s, n))
                        alive = list(range(NS))
                        while alive:
                            for s in list(alive):
                                try:
                                    next(gens[s])
                                except StopIteration:
                                    alive.remove(s)
                        for s in range(NS):
                            finalize(Ps[s], d, t0, n, g0 + s, sga, sgb)
            k.pop()
            if stop_after == 'C':
                break
            act_blocks = [b_ for b_ in blocks if not (last and b_[2] == 0)]
            k.push()
            cza = k.sb(f"cza{l}", [128, T])
            czb = k.sb(f"czb{l}", [128, T])
            cacc = [k.sb(f"cacc{l}_{i}", [128, T]) for i in range(2)]
            tlo = CTX if last else 0
            for c in range(NCH):
                k.dma(cza[:, tlo:T], zT[(28 + c) * 128:(29 + c) * 128, tlo:T])
                k.dma(czb[:, tlo:T], zT[(36 + c) * 128:(37 + c) * 128, tlo:T])
                k.act(czb[:, tlo:T], czb[:, tlo:T], AF.Sigmoid)
                k.tt('pool', cza[:, tlo:T], cza[:, tlo:T], czb[:, tlo:T], ALU.mult)
                segs = [(CTX, SEQ, GRID_W)] + ([] if last else [(0, CTX, 1)])
                for (s0, sl_, stride) in segs:
                    used = [False, False]
                    for kk_ in range(CONV_K):
                        off = (kk_ - CONV_K // 2) * stride
                        lo, hi = max(0, -off), min(sl_, sl_ - off)
                        if hi <= lo:
                            continue
                        ai = 0 if kk_ == CONV_K // 2 else 1 + 0 * kk_
                        ai = kk_ % 2 if kk_ != CONV_K // 2 else 0
                        e_ = 'dve' if ai == 0 else 'pool'
                        wcol = V('conv_w', c * CONV_K + kk_)
                        if kk_ == CONV_K // 2:
                            pass
                        dst = cacc[ai][:, s0 + lo:s0 + hi]
                        srcu = cza[:, s0 + lo + off:s0 + hi + off]
                        if not used[ai]:
                            k.memset(e_, cacc[ai][:, s0:s0 + sl_], 0.0)
                            used[ai] = True
                        k.stt(e_, dst, srcu, wcol, dst, ALU.mult, ALU.add)
                    if used[1]:
                        k.tt('dve', cacc[0][:, s0:s0 + sl_], cacc[0][:, s0:s0 + sl_], cacc[1][:, s0:s0 + sl_], ALU.add)
                k.dma(cvT[c * 128:(c + 1) * 128, tlo:T], cacc[0][:, tlo:T], q='act')
            k.pop()
            if stop_after == 'D':
                break

            k.push()
            wstage = k.sb(f"wstage{l}", [128, NCH, D])
            wts = {}
            for nme, src_w in (('oA', w_oA), ('oB', w_oB), ('out', w_out)):
                wts[nme] = k.sb(f"w{nme}{l}", [128, NCH, D], BF16)
                k.dma(wstage[:], src_w[l])
                k.copy('pool', wts[nme][:], wstage[:])
            lneps = k.sb(f"lneps{l}", [128, 1])
            k.memset('dve', lneps[:], LN_EPS)
            cvb = k.sb(f"cvb{l}", [128, NCH, 512])
            ogb = k.sb(f"ogb{l}", [128, NCH, 512])
            xbe = k.sb(f"xbe{l}", [128, NCH, 512])
            sB = k.sb(f"sB{l}", [128, NCH, 512], BF16)
            oB = k.sb(f"oB{l}", [128, NCH, 512], BF16)
            mTb = k.sb(f"mTb{l}", [128, NCH, 512], BF16)
            gat = [k.sb(f"gat{l}_{i}", [128, 512]) for i in range(4)]
            tE = [k.sb(f"tE{l}_{i}", [128, 512]) for i in range(4)]
            mean = k.sb(f"mean{l}", [128, 512])
            rsd = k.sb(f"rsd{l}", [128, 512])
            for (t0, n, seg) in act_blocks:
                sg = 1 - seg
                k.dma(cvb[:, :, :n], cvT[:, t0:t0 + n].rearrange("(c p) t -> p c t", p=128))
                k.dma(ogb[:, :, :n], ogT[:, t0:t0 + n].rearrange("(c p) t -> p c t", p=128))
                k.dma(xbe[:, :, :n], xres[:, t0:t0 + n].rearrange("(c p) t -> p c t", p=128))
                pm_, pq_ = pb[0], pb[1]
                for c in range(NCH):
                    k.mm(pm_[:, :n], onesD, cvb[:, c, :n], start=(c == 0), stop=(c == NCH - 1))
                for c in range(NCH):
                    k.act(tE[c % 2][:, :n], cvb[:, c, :n], AF.Square)
                    k.mm(pq_[:, :n], onesD, tE[c % 2][:, :n], start=(c == 0), stop=(c == NCH - 1))
                k.copy('act', mean[:, :n], pm_[:, :n])
                k.tt('pool', rsd[:, :n], mean[:, :n], mean[:, :n], ALU.mult)
                k.tt('dve', rsd[:, :n], pq_[:, :n], rsd[:, :n], ALU.subtract)
                k.act(rsd[:, :n], rsd[:, :n], AF.Sqrt, bias=lneps[:, 0:1])
                k.recip(rsd[:, :n], rsd[:, :n])
                for c in range(NCH):
                    e_ = k.ve()
                    t_ = tE[2 + c % 2]
                    k.tt(e_, t_[:, :n], cvb[:, c, :n], mean[:, :n], ALU.subtract)
                    k.tt(e_, t_[:, :n], t_[:, :n], rsd[:, :n], ALU.mult)
                    k.ts(e_, t_[:, :n], t_[:, :n], V('cnorm_g', c), ALU.mult, V('cnorm_b', c), ALU.add)
                    k.act(sB[:, c, :n], t_[:, :n], AF.Silu)
                    k.copy(e_, oB[:, c, :n], ogb[:, c, :n])
                for oc in range(NCH):
                    ocs = slice(oc * 128, (oc + 1) * 128)
                    ga_, gb_ = gat[(oc % 2) * 2], gat[(oc % 2) * 2 + 1]
                    k.dma(ga_[:, :n], zT[(44 + oc) * 128:(45 + oc) * 128, t0:t0 + n])
                    k.dma(gb_[:, :n], zT[(52 + oc) * 128:(53 + oc) * 128, t0:t0 + n])
                    k.act(ga_[:, :n], ga_[:, :n], AF.Sigmoid, bias=V('gate_b', oc))
                    k.act(gb_[:, :n], gb_[:, :n], AF.Sigmoid, bias=V('gate_b', 8 + oc))
                    pa_, pb_ = pb[2 + (oc % 2) * 2], pb[3 + (oc % 2) * 2]
                    for c in range(NCH):
                        k.mm(pa_[:, :n], wts['oA'][:, c, ocs], oB[:, c, :n], start=(c == 0), stop=(c == NCH - 1))
                    for c in range(NCH):
                        k.mm(pb_[:, :n], wts['oB'][:, c, ocs], sB[:, c, :n], start=(c == 0), stop=(c == NCH - 1))
                    k.tt('dve', ga_[:, :n], pa_[:, :n], ga_[:, :n], ALU.mult)
                    k.tt('dve', gb_[:, :n], pb_[:, :n], gb_[:, :n], ALU.mult)
                    k.tt('pool', mTb[:, oc, :n], ga_[:, :n], gb_[:, :n], ALU.add)
                for oc in range(NCH):
                    ocs = slice(oc * 128, (oc + 1) * 128)
                    po_ = pb[6 + oc % 2]
                    for c in range(NCH):
                        k.mm(po_[:, :n], wts['out'][:, c, ocs], mTb[:, c, :n], start=(c == 0), stop=(c == NCH - 1))
                    k.stt('dve', xbe[:, oc, :n], po_[:, :n], modT[:, 2 * 8 + oc, sg:sg + 1], xbe[:, oc, :n], ALU.mult, ALU.add)
                k.dma(xres[:, t0:t0 + n].rearrange("(c p) t -> p c t", p=128), xbe[:, :, :n], q='act')
            k.pop()
            if stop_after == 'E':
                break
            k.push()
            cf = [k.sb(f"pcf{l}_{i}", [128, 4096]) for i in range(2)]
            cbf = [k.sb(f"pcb{l}_{i}", [128, 4096], BF16) for i in range(2)]
            ci = 0
            for grp in range(32):
                for which in range(2):
                    f_, b_ = cf[ci % 2], cbf[ci % 2]
                    if which == 0:
                        k.dma(f_[:, :].rearrange("p (a b) -> p a b", a=NCH), puT[l, grp])
                    else:
                        k.dma(f_[:, :].rearrange("p (a b) -> p a b", a=4), pv[l, grp])
                    k.copy('pool' if ci % 2 else 'dve', b_[:], f_[:])
                    if which == 0:
                        k.dma(puTb[grp], b_[:, :].rearrange("p (a b) -> p a b", a=NCH), q='act')
                    else:
                        k.dma(pvb[grp], b_[:, :].rearrange("p (a b) -> p a b", a=4), q='act')
                    ci += 1
            k.pop()
            k.push()
            wqs = k.sb(f"wqs{l}", [128, NCH, 2 * D], BF16)
            skTs = k.sb(f"skTs{l}", [128, 16, 128], BF16)
            identb = k.sb(f"identb{l}", [128, 128], BF16)
            k.copy('dve', identb[:], ident)
            k.push()
            wqst = [k.sb(f"wqst{l}_{i}", [128, NCH, 512]) for i in range(2)]
            for j in range(4):
                k.dma(wqst[j % 2][:], w_q[l][:, :, j * 512:(j + 1) * 512])
                k.copy('pool' if j % 2 else 'dve', wqs[:, :, j * 512:(j + 1) * 512], wqst[j % 2][:])
            skst = k.sb(f"skst{l}", [128, 16, 128])
            k.dma(skst[:], skT[l].rearrange("q d j -> d q j"))
            k.copy('dve', skTs[:], skst[:])
            k.pop()
            Gt = [k.sb(f"G{l}_{i}", [128, NEXP], BF16) for i in range(2)]
            xbf = k.sb(f"xbf{l}", [128, NCH, 256])
            h2 = k.sb(f"h2{l}", [128, NCH, 256], BF16)
            qT = k.sb(f"qT{l}", [128, 16, 256], BF16)
            ssb = k.sb(f"ssb{l}", [128, 16, 128])
            Eb = ssb
            T16 = k.sb(f"T16{l}", [128, 16, 16])
            tmpk = k.sb(f"tmpk{l}", [128, 128])
            negm = k.sb(f"negm{l}", [128, 16])
            cand = k.sb(f"cand{l}", [128, 256])
            candt = k.sb(f"candt{l}", [128, 256])
            c16 = k.sb(f"c16{l}", [128, 8, 16])
            w16 = k.sb(f"w16{l}", [128, 8, 16])
            smx = k.sb(f"smx{l}", [128, 8])
            Zs = k.sb(f"Zs{l}", [128, 8])
            rZ = k.sb(f"rZ{l}", [128, 8])
            thn = k.sb(f"thn{l}", [128, 8])
            Pq = [k.sb(f"Pq{l}_{i}", [128, 8, 128]) for i in range(2)]
            Gh = [k.sb(f"Gh{l}_{i}", [128, 8, 128], BF16) for i in range(2)]
            UTs = [k.sb(f"UTs{l}_{i}", [128, NCH, 512], BF16) for i in range(2)]
            Vs = [k.sb(f"Vs{l}_{i}", [128, 4, D], BF16) for i in range(2)]
            gab = [k.sb(f"gab{l}_{i}", [128, 256]) for i in range(2)]
            WT = [k.sb(f"WT{l}_{i}", [128, 256], BF16) for i in range(2)]
            pblocks = [b_ for b_ in token_blocks(CTX, SEQ, bs=256) if not (last and b_[2] == 0)]
            for (t0, n, seg) in pblocks:
                sg = 1 - seg
                ntile = n // 128
                k.dma(xbf[:, :, :n], xres[:, t0:t0 + n].rearrange("(c p) t -> p c t", p=128))
                modulate_block(h2, xbf, n, 1, sg, f"F{l}")
                for qc in range(16):
                    pq = pb[4 + qc % 4]
                    for dh in range(NCH):
                        k.mm(pq[:, :n], wqs[:, dh, qc * 128:(qc + 1) * 128], h2[:, dh, :n], start=(dh == 0), stop=(dh == NCH - 1))
                    k.copy('act' if qc % 2 else 'dve', qT[:, qc, :n], pq[:, :n])
                for ti in range(ntile):
                    tsl = slice(ti * 128, (ti + 1) * 128)
                    for qc in range(16):
                        k.mm(pb[qc // 4][:, (qc % 4) * 128:(qc % 4 + 1) * 128], qT[:, qc, tsl], skTs[:, qc, :])
                    for b4 in range(4):
                        k.copy('act' if b4 % 2 else 'dve', ssb[:, b4 * 4:(b4 + 1) * 4, :],
                               pb[b4][:, :].rearrange("p (q j) -> p q j", q=4))
                    for qc in range(16):
                        k.max8(T16[:, qc, 0:8], ssb[:, qc, :])
                        k.match_replace(tmpk[:], T16[:, qc, 0:8], ssb[:, qc, :], -1e30)
                        k.max8(T16[:, qc, 8:16], tmpk[:])
                    T16v = T16[:, :, :].rearrange("p (h c) k -> p h c k", c=2)
                    for h in range(PH):
                        k.tt('pool', cand[:, :].rearrange("p (a b) -> p a b", a=16),
                             T16v[:, h, 0, :].unsqueeze(2).to_broadcast([128, 16, 16]),
                             T16v[:, h, 1, :].unsqueeze(1).to_broadcast([128, 16, 16]), ALU.add)
                        k.max8(c16[:, h, 0:8], cand[:, :])
                        k.match_replace(candt[:], c16[:, h, 0:8], cand[:, :], -1e30)
                        k.max8(c16[:, h, 8:16], candt[:])
                    k.tt('pool', smx[:], T16v[:, :, 0, 0], T16v[:, :, 1, 0], ALU.add)
                    k.tt('pool', w16[:], c16[:], smx[:, :].unsqueeze(2).to_broadcast([128, 8, 16]), ALU.subtract)
                    k.act(w16[:], w16[:], AF.Exp)
                    k.reduce('dve', Zs[:], w16[:], AX.X, ALU.add)
                    k.recip(rZ[:], Zs[:])
                    k.stt('pool', thn[:], w16[:, :, 15], 0.999, rZ[:], ALU.mult, ALU.mult)
                    k.ts('pool', negm[:], T16[:, :, 0], -1.0, ALU.mult)
                    for qc in range(16):
                        k.act(Eb[:, qc, :], ssb[:, qc, :], AF.Exp, bias=negm[:, qc:qc + 1])
                    for h in range(PH):
                        k.ts('pool', Eb[:, 2 * h, :], Eb[:, 2 * h, :], rZ[:, h:h + 1], ALU.mult)
                    cnt_p = 0
                    for e16 in range(16):
                        isl = slice(e16 * 8, (e16 + 1) * 8)
                        Gs = Gt[ti][:, e16 * 1024:(e16 + 1) * 1024].rearrange("p (i j) -> p i j", i=8)
                        for h in range(PH):
                            P_, Gh_ = Pq[cnt_p % 2], Gh[cnt_p % 2]
                            cnt_p += 1
                            k.tt('pool' if cnt_p % 5 in (0, 2, 4) else 'dve', P_[:],
                                 Eb[:, 2 * h, isl].unsqueeze(2).to_broadcast([128, 8, 128]),
                                 Eb[:, 2 * h + 1, :].unsqueeze(1).to_broadcast([128, 8, 128]), ALU.mult)
                            if h == 0:
                                k.stt('dve', Gs, P_[:], thn[:, h:h + 1], P_[:], ALU.is_ge, ALU.mult)
                            else:
                                k.stt('dve', Gh_[:], P_[:], thn[:, h:h + 1], P_[:], ALU.is_ge, ALU.mult)
                                k.tt('pool' if h in (2, 5, 7) else 'dve', Gs, Gs, Gh_[:], ALU.add)
                for grp in range(32):
                    UT_, V_ = UTs[grp % 2], Vs[grp % 2]
                    k.dma(UT_[:], puTb[grp])
                    k.dma(V_[:], pvb[grp])
                    for ec in range(4):
                        e = grp * 4 + ec
                        pa = pb[e % 2]
                        for dh in range(NCH):
                            k.mm(pa[:, :n], UT_[:, dh, ec * 128:(ec + 1) * 128], h2[:, dh, :n], start=(dh == 0), stop=(dh == NCH - 1))
                        ga_ = gab[e % 2]
                        k.act(ga_[:, :n], pa[:, :n], AF.Gelu)
                        pgb = pb[2 + e % 2][:, :].bitcast(BF16)
                        for ti in range(ntile):
                            k.tr(pgb[:, ti * 128:(ti + 1) * 128], Gt[ti][:, e * 128:(e + 1) * 128], identb[:])
                        WT_ = WT[e % 2]
                        k.tt('dve', WT_[:, :n], ga_[:, :n], pgb[:, :n], ALU.mult)
                        for oc in range(NCH):
                            acc = pb[4 + oc // 2][:, (oc % 2) * 256:(oc % 2) * 256 + n]
                            k.mm(acc, V_[:, ec, oc * 128:(oc + 1) * 128], WT_[:, :n],
                                 start=(e == 0 and oc % 2 == 0), stop=(e == 127), skip=True)
                for oc in range(NCH):
                    acc = pb[4 + oc // 2][:, (oc % 2) * 256:(oc % 2) * 256 + n]
                    k.stt('dve', xbf[:, oc, :n], acc, modT[:, 5 * 8 + oc, sg:sg + 1], xbf[:, oc, :n], ALU.mult, ALU.add)
                k.dma(xres[:, t0:t0 + n].rearrange("(c p) t -> p c t", p=128), xbf[:, :, :n], q='act')
            k.pop()
            if stop_after == 'F':
                break

        if stop_after is None:
            k.push()
            l = NL
            xfb = [k.sb(f"xfb{i}", [128, NCH, 512]) for i in range(2)]
            for bi, (t0, n, seg) in enumerate([b_ for b_ in blocks if b_[2] == 1]):
                xb_ = xfb[bi % 2]
                k.dma(xb_[:, :, :n], xres[:, t0:t0 + n].rearrange("(c p) t -> p c t", p=128))
                pss = pb[bi % 2]
                for c in range(NCH):
                    k.act(sqs[:, :n], xb_[:, c, :n], AF.Square)
                    k.mm(pss[:, :n], onesD, sqs[:, :n], start=(c == 0), stop=(c == NCH - 1))
                k.act(rstd[:, :n], pss[:, :n], AF.Sqrt, bias=epsn[:, 0:1])
                k.recip(rstd[:, :n], rstd[:, :n])
                for c in range(NCH):
                    k.stt(k.ve(), xb_[:, c, :n], xb_[:, c, :n], V('final_g', c), rstd[:, :n], ALU.mult, ALU.mult)
                k.dma(outT[:, t0 - CTX:t0 - CTX + n].rearrange("(c p) t -> p c t", p=128), xb_[:, :, :n], q='act')
            k.pop()
        S.finish()
        S.emit()
        nc._dbg = dbg
        nc._nops = S.nops
    return nc


def _fm(v):
    v = np.asarray(v, np.float32).reshape(-1, 128)
    return np.ascontiguousarray(v.T)


def make_consts():
    c = np.zeros((128, 10, 128), np.float32)
    i = np.arange(128)[:, None]
    t = np.arange(128)[None, :]
    same = (i // 64) == (t // 64)
    c[:, 0, :] = (i == t)
    c[:, 1, :] = same
    c[:, 2, :] = 1.0 / D
    c[:, 3, :] = same & (i < t)
    c[:, 4, :] = same & (i <= t)
    c[:, 5, :] = same & (i > t)
    c[:, 6, :] = same & (i >= t)
    c[:, 7, :] = ((i % 64) == t)
    c[:, 8, :] = same / 64.0
    return c


def prepare_shared(inp, NL):
    f32 = lambda a: np.asarray(a, np.float32)
    sh = {}
    ada_w = f32(inp['ada_w'])
    sh['ada_w'] = np.ascontiguousarray(ada_w.reshape(NL, 8, 128, 48, 128).transpose(0, 3, 2, 1, 4))
    vec = np.zeros((NL, 128, NV), np.float32)

    def put(l, name, arr):
        o, w = VEC[name]
        assert arr.shape == (128, w), (name, arr.shape, w)
        vec[l, :, o:o + w] = arr
    for l in range(NL):
        for nme in ('norm1_g', 'norm2_g', 'k_k', 'k_a', 'lnx_g', 'lnx_b', 'cnorm_g', 'cnorm_b', 'gate_b'):
            put(l, nme, _fm(f32(inp[nme])[l]))
        put(l, 'r_k', _fm(f32(inp['r_k'])[l].reshape(-1)))
        put(l, 'w0', _fm(f32(inp['w0'])[l].reshape(-1)))
        put(l, 'a0', _fm(f32(inp['a0'])[l].reshape(-1)))
        put(l, 'final_g', _fm(f32(inp['final_g'])))
        mu = np.zeros((2, 3584), np.float32)
        mu[:, :3488] = f32(inp['shift_mu'])[l]
        put(l, 'mu0', _fm(mu[0]))
        put(l, 'mu1', _fm(mu[1]))
        put(l, 'ada_b', _fm(f32(inp['ada_b'])[l]))
        cw = f32(inp['conv_w'])[l]
        put(l, 'conv_w', np.ascontiguousarray(cw.T.reshape(8, 128, CONV_K).transpose(1, 0, 2)).reshape(128, 8 * CONV_K))
    sh['vec'] = vec
    w_in = f32(inp['w_in'])
    wp = np.zeros((NL, D, P_IN_PAD), np.float32)
    wp[:, :, :3488] = w_in[:, :, :3488]
    wp[:, :, 3584:] = w_in[:, :, 3488:]
    sh['w_in'] = np.ascontiguousarray(wp.reshape(NL, 8, 128, NFC, 128).transpose(0, 3, 2, 1, 4))
    sh['w2'] = np.ascontiguousarray(f32(inp['w2']).reshape(NL, 128, D))
    sh['a2'] = np.ascontiguousarray(f32(inp['a2']).reshape(NL, 128, D))
    g2p = np.zeros((NL, 256, D), np.float32)
    g2p[:, :LORA_G] = f32(inp['g2'])
    sh['g2'] = g2p.reshape(NL, 2, 128, D)
    for nme in ('w_oA', 'w_oB', 'w_out'):
        sh[nme] = np.ascontiguousarray(f32(inp[nme]).reshape(NL, 8, 128, D).transpose(0, 2, 1, 3))
    sh['w_q'] = np.ascontiguousarray(f32(inp['w_q']).reshape(NL, 8, 128, 2 * D).transpose(0, 2, 1, 3))
    sh['skT'] = np.ascontiguousarray(f32(inp['sub_keys']).reshape(NL, 16, 128, 128).transpose(0, 1, 3, 2))
    sh['puT'] = np.ascontiguousarray(f32(inp['peer_u']).reshape(NL, 32, 512, 8, 128).transpose(0, 1, 4, 3, 2))
    sh['pv'] = np.ascontiguousarray(f32(inp['peer_v']).reshape(NL, 32, 4, 128, D).transpose(0, 1, 3, 2, 4))
    sh['consts'] = make_consts()
    return sh


def prepare_core(inp, b):
    f32 = lambda a: np.asarray(a, np.float32)
    xT = np.ascontiguousarray(np.concatenate([f32(inp['ctx'])[b], f32(inp['x'])[b]], axis=0).T)
    cc = np.stack([_fm(f32(inp['c'])[b]), _fm(f32(inp['c_ctx']))], axis=-1)
    return {'xT': xT, 'cc': np.ascontiguousarray(cc)}


def kernel(**inputs):
    B, SEQ, _ = inputs['x'].shape
    CTX = inputs['ctx'].shape[1]
    NL = inputs['w_in'].shape[0]
    cfg = dict(CTX=CTX, SEQ=SEQ, NL=NL)
    nc = build(cfg)
    sh = prepare_shared(inputs, NL)
    in_maps = []
    for b in range(B):
        m = dict(sh)
        m.update(prepare_core(inputs, b))
        in_maps.append(m)
    res = run_bass_kernel_spmd(nc, in_maps, core_ids=list(range(B)))
    out = np.stack([np.ascontiguousarray(np.asarray(r['outT']).T) for r in res.results], axis=0)
    return out.astype(np.float32)
```

```python
from contextlib import ExitStack
import math
import numpy as np
import concourse.bass as bass
import concourse.mybir as mybir
from concourse.bass_utils import run_bass_kernel_spmd

F32 = mybir.dt.float32
BF16 = mybir.dt.bfloat16
AF = mybir.ActivationFunctionType
ALU = mybir.AluOpType
AX = mybir.AxisListType

D = 1024
NCH = 8
HEADS = 16
HD = 64
LORA_G = 160
CONV_K = 31
GRID_W = 64
P_IN_PAD = 7680
NFC = 60
PH, PNK, PHALF, PTOPK = 8, 128, 128, 16
NEXP = PNK * PNK
NORM_EPS = 1e-6
LN_EPS = 1e-5
GN_EPS = HD * 1e-5
CH = 64
EXPM05 = math.exp(-0.5)
F32R = mybir.dt.float32r
RWKV_F32R = True


class Sched:
    GEN = 20000

    def __init__(self, nc, stack, ndma=24):
        self.nc = nc
        self.stack = stack
        self.eng = {'pe': nc.tensor, 'act': nc.scalar, 'dve': nc.vector, 'pool': nc.gpsimd, 'sp': nc.sync}
        self.prog = {e: [] for e in self.eng}
        self.cnt = {e: 0 for e in self.eng}
        self.gen = {e: -1 for e in self.eng}
        self.semh = {}
        for e in self.eng:
            self._newgen(e)
        self.ndma = ndma
        self.dmaval = [0] * ndma
        self.dmarr = 0
        for i in range(ndma):
            self.semh[('dma', i)] = stack.enter_context(nc.semaphore(f"sdma{i}"))
        self.waited = {}
        self.lastw = {}
        self.readers = {}
        self.nops = 0

    def _newgen(self, e):
        self.gen[e] += 1
        self.cnt[e] = 0
        self.semh[(e, self.gen[e])] = self.stack.enter_context(self.nc.semaphore(f"s{e}{self.gen[e]}"))

    def _wait(self, eng, key, val):
        if self.waited.get((eng, key), 0) >= val:
            return
        self.waited[(eng, key)] = val
        self.prog[eng].append(('wait', self.semh[key], val))

    def _deps(self, eng, reads, writes):
        toks = {}

        def add(d):
            for k, v in d.items():
                if toks.get(k, 0) < v:
                    toks[k] = v
        for b in reads:
            add(self.lastw.get(b, {}))
        for b in writes:
            add(self.lastw.get(b, {}))
            add(self.readers.get(b, {}))
        for k, v in toks.items():
            if eng == 'pe' and k[0] == 'pe':
                continue
            self._wait(eng, k, v)

    def _record(self, key, val, reads, writes):
        for b in writes:
            self.lastw[b] = {key: val}
            self.readers[b] = {}
        for b in reads:
            if b in writes:
                continue
            r = self.readers.setdefault(b, {})
            if r.get(key, 0) < val:
                r[key] = val

    def op(self, eng, fn, reads, writes):
        pr = [r for r in reads if r.startswith('pb') and r not in writes]
        if pr:
            writes = list(writes) + pr
        self._deps(eng, reads, writes)
        if self.cnt[eng] >= self.GEN:
            self._newgen(eng)
        self.cnt[eng] += 1
        key = (eng, self.gen[eng])
        val = self.cnt[eng]
        self.prog[eng].append(('op', fn, self.semh[key]))
        self._record(key, val, reads, writes)
        self.nops += 1

    def dma(self, q, out, in_, reads=None, writes=None):
        reads = [in_.tensor.name] if reads is None else reads
        writes = [out.tensor.name] if writes is None else writes
        self._deps(q, reads, writes)
        i = self.dmarr
        self.dmarr = (i + 1) % self.ndma
        key = ('dma', i)
        self._wait(q, key, self.dmaval[i])
        self.dmaval[i] += 16
        self.prog[q].append(('dma', out, in_, self.semh[key]))
        self._record(key, self.dmaval[i], reads, writes)
        self.nops += 1

    def barrier(self):
        for e in self.eng:
            for o in self.eng:
                if o != e and self.cnt[o] > 0:
                    self._wait(e, (o, self.gen[o]), self.cnt[o])
            for i in range(self.ndma):
                if self.dmaval[i] > 0:
                    self._wait(e, ('dma', i), self.dmaval[i])

    def finish(self, q='sp'):
        for i in range(self.ndma):
            self._wait(q, ('dma', i), self.dmaval[i])
        for e in self.eng:
            if e != q and self.cnt[e] > 0:
                self._wait(q, (e, self.gen[e]), self.cnt[e])

    def emit(self):
        nc = self.nc
        prog = self.prog

        def replay(e, items):
            for it in items:
                if it[0] == 'wait':
                    e.wait_ge(it[1], it[2])
                elif it[0] == 'op':
                    it[1](e).then_inc(it[2], 1)
                else:
                    e.dma_start(out=it[1], in_=it[2]).then_inc(it[3], 16)
        with nc.Block() as block:
            @block.tensor
            def _(e):
                replay(e, prog['pe'])

            @block.scalar
            def _(e):
                replay(e, prog['act'])

            @block.vector
            def _(e):
                replay(e, prog['dve'])

            @block.gpsimd
            def _(e):
                replay(e, prog['pool'])

            @block.sync
            def _(e):
                replay(e, prog['sp'])


_ALIAS = {}


def _nm(*aps):
    out = []
    for a in aps:
        if hasattr(a, 'tensor'):
            n = a.tensor.name
            n = _ALIAS.get(n, n)
            if n not in out:
                out.append(n)
    return out


class K:
    SB_BASE, SB_LIMIT = 16512, 229344

    def __init__(self, nc, S, stack):
        self.nc, self.S, self.stack = nc, S, stack
        self.rr = 0
        self.top = self.SB_BASE
        self.marks = []
        self.uid = 0

    def sb(self, name, shape, dt=F32):
        isz = 2 if dt == BF16 else 4
        size = isz
        for s_ in shape[1:]:
            size *= s_
        size = (size + 63) // 64 * 64
        off = self.top
        self.top += size
        assert self.top <= self.SB_LIMIT, f"SBUF arena overflow allocating {name} {shape}: top={self.top}"
        self.uid += 1
        return self.nc.alloc_sbuf_tensor_at(f"{name}_u{self.uid}", list(shape), dt, offset=off)

    def push(self):
        self.marks.append(self.top)

    def pop(self):
        self.top = self.marks.pop()
        self.S.barrier()

    def ps(self, name, shape, dt=F32):
        return self.stack.enter_context(self.nc.psum_tensor(name, list(shape), dt))

    def dram(self, name, shape, dt=F32, kind="Internal"):
        return self.nc.dram_tensor(name, list(shape), dt, kind=kind)

    def ve(self):
        self.rr ^= 1
        return 'dve' if self.rr else 'pool'

    def mm(self, out, lhsT, rhs, start=True, stop=True, skip=False):
        if skip:
            self.S.op('pe', lambda e: e.matmul(out, lhsT, rhs, start=start, stop=stop, skip_group_check=True),
                      _nm(lhsT, rhs), _nm(out))
        else:
            self.S.op('pe', lambda e: e.matmul(out, lhsT, rhs, start=start, stop=stop), _nm(lhsT, rhs), _nm(out))

    def tr(self, out, in_, ident):
        self.S.op('pe', lambda e: e.transpose(out, in_, ident), _nm(in_, ident), _nm(out))

    def act(self, out, in_, func, bias=None, scale=None, accum_out=None):
        kw = {}
        if bias is not None:
            kw['bias'] = bias
        if scale is not None:
            kw['scale'] = scale
        if accum_out is not None:
            kw['accum_out'] = accum_out
        self.S.op('act', lambda e: e.activation(out, in_, func, **kw), _nm(in_, bias, scale), _nm(out, accum_out))

    def tt(self, eng, out, in0, in1, op):
        self.S.op(eng, lambda e: e.tensor_tensor(out, in0, in1, op), _nm(in0, in1), _nm(out))

    def ts(self, eng, out, in0, s1, op0, s2=None, op1=None, accum_out=None):
        kw = {}
        if accum_out is not None:
            kw['accum_out'] = accum_out
        if op1 is None:
            self.S.op(eng, lambda e: e.tensor_scalar(out, in0, s1, None, op0, **kw), _nm(in0, s1), _nm(out, accum_out))
        else:
            self.S.op(eng, lambda e: e.tensor_scalar(out, in0, s1, s2, op0, op1, **kw), _nm(in0, s1, s2), _nm(out, accum_out))

    def stt(self, eng, out, in0, scalar, in1, op0, op1, accum_out=None):
        eng = 'dve'
        kw = {}
        if accum_out is not None:
            kw['accum_out'] = accum_out
        self.S.op(eng, lambda e: e.scalar_tensor_tensor(out, in0, scalar, in1, op0, op1, **kw),
                  _nm(in0, scalar, in1), _nm(out, accum_out))

    def copy(self, eng, out, in_):
        if eng == 'act':
            self.S.op('act', lambda e: e.copy(out, in_), _nm(in_), _nm(out))
        else:
            self.S.op(eng, lambda e: e.tensor_copy(out, in_), _nm(in_), _nm(out))

    def recip(self, out, in_):
        self.S.op('dve', lambda e: e.reciprocal(out, in_), _nm(in_), _nm(out))

    def max8(self, out, in_):
        self.S.op('dve', lambda e: e.max(out, in_), _nm(in_), _nm(out))

    def match_replace(self, out, in_to_replace, in_values, imm):
        self.S.op('dve', lambda e: e.match_replace(out, in_to_replace, in_values, imm), _nm(in_to_replace, in_values), _nm(out))

    def scan(self, eng, out, d0, d1, init, op0, op1):
        eng = 'dve'
        self.S.op(eng, lambda e: e.tensor_tensor_scan(out, d0, d1, init, op0, op1), _nm(d0, d1), _nm(out))

    def reduce(self, eng, out, in_, axis, op):
        self.S.op(eng, lambda e: e.tensor_reduce(out, in_, axis, op), _nm(in_), _nm(out))

    def memset(self, eng, out, val):
        self.S.op(eng, lambda e: e.memset(out, val), [], _nm(out))

    def dma(self, out, in_, q='sp'):
        self.S.dma(q, out, in_)


VEC = {}
_off = 0
for _n, _w in [('norm1_g', 8), ('norm2_g', 8), ('k_k', 8), ('k_a', 8), ('omka', 8), ('r_k', 8), ('lnx_g', 8), ('lnx_b', 8),
               ('cnorm_g', 8), ('cnorm_b', 8), ('gate_b', 16), ('w0', 16), ('a0', 16), ('final_g', 8),
               ('mu0', 28), ('mu1', 28), ('muc', 28), ('ada_b', 48), ('conv_w', 8 * CONV_K)]:
    VEC[_n] = (_off, _w)
    _off += _w
NV = _off


def token_blocks(CTX, SEQ, bs=512):
    blocks = []
    t = 0
    while t < CTX:
        n = min(bs, CTX - t)
        blocks.append((t, n, 0))
        t += n
    while t < CTX + SEQ:
        n = min(bs, CTX + SEQ - t)
        blocks.append((t, n, 1))
        t += n
    return blocks


def build(cfg):
    CTX, SEQ, NL = cfg['CTX'], cfg['SEQ'], cfg['NL']
    stop_after = cfg.get('stop_after', None)
    T = CTX + SEQ
    nc = bass.Bass("TRN2", target_bir_lowering=False)
    stack = ExitStack()
    with stack:
        S = Sched(nc, stack)
        k = K(nc, S, stack)
        stack.enter_context(nc.allow_low_precision("bf16 matmuls with fp32 accumulation (tolerance allows)"))
        inp = lambda name, shape: nc.dram_tensor(name, list(shape), F32, kind="ExternalInput").ap()
        xT = inp("xT", [D, T])
        cc = inp("cc", [128, NCH, 2])
        ada_w = inp("ada_w", [NL, 48, 128, NCH, 128])
        vec = inp("vec", [NL, 128, NV])
        w_in = inp("w_in", [NL, NFC, 128, NCH, 128])
        w2 = inp("w2", [NL, 128, D])
        a2 = inp("a2", [NL, 128, D])
        g2 = inp("g2", [NL, 2, 128, D])
        w_oA = inp("w_oA", [NL, 128, NCH, D])
        w_oB = inp("w_oB", [NL, 128, NCH, D])
        w_out = inp("w_out", [NL, 128, NCH, D])
        w_q = inp("w_q", [NL, 128, NCH, 2 * D])
        skT = inp("skT", [NL, 16, 128, 128])
        puT = inp("puT", [NL, 32, 128, NCH, 512])
        pv = inp("pv", [NL, 32, 128, 4, D])
        consts = inp("consts", [128, 10, 128])
        outT = nc.dram_tensor("outT", [D, SEQ], F32, kind="ExternalOutput").ap()
        dbg = {}

        def scratch(name, shape, dt=F32):
            kind = "ExternalOutput" if cfg.get('debug') else "Internal"
            t_ = nc.dram_tensor(name, list(shape), dt, kind=kind).ap()
            dbg[name] = t_
            return t_
        xres = scratch("xres", [D, T])
        zT = scratch("zT", [P_IN_PAD, T])
        y0T = scratch("y0T", [D, T])
        bv0T = scratch("bv0T", [D, T])
        ogT = scratch("ogT", [D, T])
        cvT = scratch("cvT", [D, T])
        puTb = scratch("puTb", [32, 128, NCH, 512], BF16)
        pvb = scratch("pvb", [32, 128, 4, D], BF16)

        cst = k.sb("cst", [128, 10, 128])
        k.dma(cst[:], consts)
        ident = cst[:, 0, :]
        onesblk = cst[:, 1, :]
        onesD = cst[:, 2, :]
        identblk = cst[:, 7, 0:64]
        ones64 = cst[:, 8, :]
        rst = k.sb("rst", [128, 512])
        k.memset('dve', rst[:], 1.0)
        k.memset('dve', rst[:].rearrange("p (c j) -> p c j", j=CH)[:, :, 0:1], 0.0)
        mA = k.sb("mA", [128, 2, 2, 128])
        mB = k.sb("mB", [128, 2, 2, 128])
        mT = k.sb("mT", [128, 2, 128])
        for d in range(2):
            k.copy('dve', mA[:, d, 0, :], cst[:, 3 + 2 * d, :])
            k.ts('dve', mA[:, d, 1, :], cst[:, 4 + 2 * d, :], -1.0, ALU.mult)
            k.copy('dve', mB[:, d, 0, :], cst[:, 3 + 2 * d, :])
            k.copy('dve', mB[:, d, 1, :], cst[:, 4 + 2 * d, :])
            k.copy('dve', mT[:, d, :], cst[:, 5 - 2 * d, :])

        pb = [k.ps(f"pb{i}", [128, 512]) for i in range(8)]
        ccs = k.sb("ccs", [128, NCH, 2])
        k.dma(ccs[:], cc)
        k.act(ccs[:], ccs[:], AF.Silu)
        vecs = k.sb("vecs", [128, NV])
        modT = k.sb("modT", [128, 48, 2])

        def V(name, j=0, w=1):
            o, _ = VEC[name]
            return vecs[:, o + j:o + j + w]

        blocks = token_blocks(CTX, SEQ)
        sqs = k.sb("sqs", [128, 512])
        rstd = k.sb("rstd", [128, 512])
        xn = k.sb("xn", [128, 512])
        epsn = k.sb("epsn", [128, 1])
        k.memset('dve', epsn[:], NORM_EPS)
        gm = k.sb("gm", [128, 2, NCH, 2])

        for l in range(NL):
            last = (l == NL - 1)
            src = xT if l == 0 else xres
            k.dma(vecs[:], vec[l])
            if True:
                k.push()
                aw = [k.sb(f"aw{l}_{i}", [128, NCH, 128]) for i in range(2)]
                for j in range(48):
                    a_ = aw[j % 2]
                    k.dma(a_[:], ada_w[l, j])
                    pm = pb[j % 2]
                    for dh in range(NCH):
                        k.mm(pm[:, 0:2], a_[:, dh, :], ccs[:, dh, :], start=(dh == 0), stop=(dh == NCH - 1))
                    k.ts('dve', modT[:, j, :], pm[:, 0:2], V('ada_b', j), ALU.add)
                for w_, (nn, sci) in enumerate((('norm1_g', 1), ('norm2_g', 4))):
                    for c in range(NCH):
                        k.ts('dve', gm[:, w_, c, :], modT[:, sci * 8 + c, :], 1.0, ALU.add, V(nn, c), ALU.mult)
                k.pop()

            def modulate_block(dst, xb, n, which, seg, tagp):
                pss = pb[7]
                for c in range(NCH):
                    k.act(sqs[:, :n], xb[:, c, :n], AF.Square)
                    k.mm(pss[:, :n], onesD, sqs[:, :n], start=(c == 0), stop=(c == NCH - 1))
                k.act(rstd[:, :n], pss[:, :n], AF.Sqrt, bias=epsn[:, 0:1])
                k.recip(rstd[:, :n], rstd[:, :n])
                shi = 0 if which == 0 else 3
                for c in range(NCH):
                    e_ = k.ve()
                    k.tt(e_, xn[:, :n], xb[:, c, :n], rstd[:, :n], ALU.mult)
                    k.ts(e_, dst[:, c, :n], xn[:, :n], gm[:, which, c, seg:seg + 1], ALU.mult,
                         modT[:, shi * 8 + c, seg:seg + 1], ALU.add)


            if True:
                k.push()
                HT = k.sb(f"HT{l}", [128, NCH, T], BF16)
                xb = [k.sb(f"xb{l}_{i}", [128, NCH, 512]) for i in range(2)]
                for bi, (t0, n, seg) in enumerate(blocks):
                    xb_ = xb[bi % 2]
                    k.dma(xb_[:, :, :n], src[:, t0:t0 + n].rearrange("(c p) t -> p c t", p=128))
                    if l == 0:
                        k.dma(xres[:, t0:t0 + n].rearrange("(c p) t -> p c t", p=128), xb_[:, :, :n], q='act')
                    modulate_block(HT[:, :, t0:t0 + n], xb_, n, 0, 1 - seg, f"A{l}")
                wf = [k.sb(f"wf{l}_{i}", [128, NCH, 128]) for i in range(2)]
                wb = [k.sb(f"wb{l}_{i}", [128, NCH, 128], BF16) for i in range(2)]
                zst = [k.sb(f"zst{l}_{i}", [128, 512]) for i in range(4)]
                cnt = 0
                nfc = 28 if (last and False) else NFC
                for fc in range(nfc):
                    wf_, wb_ = wf[fc % 2], wb[fc % 2]
                    k.dma(wf_[:], w_in[l, fc])
                    k.copy('pool', wb_[:], wf_[:])
                    for (t0, n, seg) in blocks:
                        if last and seg == 0 and fc >= 28:
                            continue
                        pz = pb[cnt % 4]
                        for dh in range(NCH):
                            k.mm(pz[:, :n], wb_[:, dh, :], HT[:, dh, t0:t0 + n], start=(dh == 0), stop=(dh == NCH - 1))
                        z_ = zst[cnt % 4]
                        k.copy('act' if cnt % 2 else 'dve', z_[:, :n], pz[:, :n])
                        k.dma(zT[fc * 128:(fc + 1) * 128, t0:t0 + n], z_[:, :n], q='act')
                        cnt += 1
                k.pop()
            if stop_after == 'B':
                break
            k.push()
            k.tt('dve', V('muc', 0, 28), V('mu0', 0, 28), V('mu1', 0, 28), ALU.add)
            k.ts('dve', V('muc', 0, 28), V('muc', 0, 28), -1.0, ALU.mult, 1.0, ALU.add)
            k.ts('dve', V('omka', 0, 8), V('k_a', 0, 8), -1.0, ALU.mult, 1.0, ALU.add)
            TW = T + 4
            zw = [k.sb(f"zw{l}_{i}", [128, TW]) for i in range(2)]
            zo = [k.sb(f"zo{l}_{i}", [128, TW]) for i in range(2)]
            for i in range(2):
                k.memset('pool', zw[i][:, 0:1], 0.0)
                k.memset('pool', zw[i][:, CTX + 1:CTX + 3], 0.0)
                k.memset('pool', zw[i][:, TW - 1:TW], 0.0)

            def b2_load(fc):
                rows = slice(fc * 128, (fc + 1) * 128)
                k.dma(zw[fc % 2][:, 1:1 + CTX], zT[rows, 0:CTX])
                k.dma(zw[fc % 2][:, CTX + 3:CTX + 3 + SEQ], zT[rows, CTX:T])
            b2_load(0)
            for fc in range(28):
                rows = slice(fc * 128, (fc + 1) * 128)
                if fc + 1 < 28:
                    b2_load(fc + 1)
                zw_, zo_ = zw[fc % 2], zo[fc % 2]
                k.ts('dve', zo_[:, 1:TW - 1], zw_[:, 1:TW - 1], V('muc', fc), ALU.mult)
                k.stt('pool', zo_[:, 1:TW - 1], zw_[:, 0:TW - 2], V('mu0', fc), zo_[:, 1:TW - 1], ALU.mult, ALU.add)
                k.stt('dve', zo_[:, 1:TW - 1], zw_[:, 2:TW], V('mu1', fc), zo_[:, 1:TW - 1], ALU.mult, ALU.add)
                if fc == 24:
                    k.act(zo_[:, 1:TW - 1], zo_[:, 1:TW - 1], AF.Tanh)
                if fc in (26, 27):
                    k.act(zo_[:, 1:TW - 1], zo_[:, 1:TW - 1], AF.Sigmoid)
                k.dma(zT[rows, 0:CTX], zo_[:, 1:1 + CTX], q='act')
                k.dma(zT[rows, CTX:T], zo_[:, CTX + 3:CTX + 3 + SEQ], q='act')
            k.pop()
            if stop_after == 'B2':
                break

            k.push()
            w2s = k.sb(f"w2s{l}", [128, D])
            a2s = k.sb(f"a2s{l}", [128, D])
            g2s = k.sb(f"g2s{l}", [128, 2, D])
            k.dma(w2s[:], w2[l])
            k.dma(a2s[:], a2[l])
            k.dma(g2s[:], g2[l].rearrange("c p f -> p c f"))
            RDT = F32R if RWKV_F32R else F32
            Sst = [k.sb(f"Sst{l}_{g}", [128, HD], RDT) for g in range(8)]
            gneps = k.sb(f"gneps{l}", [128, 1])
            k.memset('dve', gneps[:], GN_EPS)
            NS = 2
            hmask = [onesblk[:, 0:1], onesblk[:, 64:65]]

            class Obj:
                pass
            Ps, Us = [], []
            for s in range(NS):
                P = Obj()
                for nm_ in ('rr', 'kx', 'vv', 'kk', 'aa', 'kd', 'bb', 'lw', 'lam', 'pex', 'rem', 'Lb', 'BQ', 'nBQP', 'KQP',
                            'BQ0', 'BQ1', 'KQ0', 'KQ1', 'KP0', 'KP1',
                            't1', 't2', 'e1', 'e3', 'Yb', 'bv'):
                    setattr(P, nm_, k.sb(f"P{l}_{s}_{nm_}", [128, 512],
                                         RDT if nm_ in ('BQ', 'BQ0', 'BQ1', 'KQ0', 'KQ1', 'KP0', 'KP1') else F32))
                P.e2, P.e4, P.Lb = P.aa, P.t1, P.kx
                P.KR = k.sb(f"P{l}_{s}_KR", [128, 2, 512], RDT)
                P.PC = k.sb(f"P{l}_{s}_PC", [128, 8])
                Ps.append(P)
                U = Obj()
                U.TM = k.sb(f"U{l}_{s}_TM", [128, 4, 128], RDT)
                U.ZA = k.sb(f"U{l}_{s}_ZA", [128, 2, 2, 128], RDT)
                U.ZB = k.sb(f"U{l}_{s}_ZB", [128, 2, 2, 128], RDT)
                U.AA = k.sb(f"U{l}_{s}_AA", [128, 2, 128], RDT)
                U.ZZ = [k.sb(f"U{l}_{s}_ZZ{j}", [128, 2, 2, 128], RDT) for j in range(5)]
                U.X = [k.sb(f"U{l}_{s}_X{j}", [128, 2, 128], RDT) for j in range(2)]
                U.MT = k.sb(f"U{l}_{s}_MT", [128, 2, 64], RDT)
                U.NN = k.sb(f"U{l}_{s}_NN", [128, 2, 64], RDT)
                U.RPp = k.sb(f"U{l}_{s}_RPp", [128, 128], RDT)
                U.banks = [pb[2 + 3 * s], pb[3 + 3 * s]]
                U.ybank = pb[4 + 3 * s]
                U.bi = 0
                Us.append(U)
            lor = [[k.sb(f"lor{l}_{i}_{j}", [128, 512]) for j in range(4)] for i in range(1)]
            prep_cnt = [0]

            class KR:
                def __getattr__(self, a):
                    return getattr(k, a)

                def mm(self, out, lhsT, rhs, start=True, stop=True, skip=False):
                    if out.base_partition() != 0 or out.shape[0] != 128:
                        lhsT, rhs = lhsT.bitcast(F32), rhs.bitcast(F32)
                    k.mm(out, lhsT, rhs, start=start, stop=stop, skip=skip)
            kr = KR()

            def prep_bank():
                prep_cnt[0] += 1
                return pb[prep_cnt[0] % 2]

            def ubank(U):
                U.bi += 1
                return U.banks[U.bi % 2]

            def prep(P, d, t0, n, g, tw, awt):
                k.dma(P.rr[:, :n], zT[g * 128:(g + 1) * 128, t0:t0 + n])
                k.dma(P.kx[:, :n], zT[(8 + g) * 128:(9 + g) * 128, t0:t0 + n])
                k.dma(P.vv[:, :n], zT[(16 + g) * 128:(17 + g) * 128, t0:t0 + n])
                k.ts('pool', P.t1[:, :n], P.kx[:, :n], V('k_k', g), ALU.mult)
                k.tt('pool', P.t2[:, :n], P.t1[:, :n], P.t1[:, :n], ALU.mult)
                pq = prep_bank()
                k.mm(pq[:, :n], onesblk, P.t2[:, :n])
                k.act(P.t2[:, :n], pq[:, :n], AF.Sqrt)
                k.ts('dve', P.t2[:, :n], P.t2[:, :n], 1e-12, ALU.max)
                k.recip(P.t2[:, :n], P.t2[:, :n])
                k.tt('pool', P.kk[:, :n], P.t1[:, :n], P.t2[:, :n], ALU.mult)
                ds = slice(d * 64, (d + 1) * 64)
                gs = slice(g * 128, (g + 1) * 128)
                pq = prep_bank()
                k.mm(pq[:, :n], w2s[ds, gs], tw[ds, :n])
                k.act(P.lw[:, :n], pq[:, :n], AF.Sigmoid, bias=V('w0', d * 8 + g))
                k.ts('pool', P.lw[:, :n], P.lw[:, :n], -EXPM05, ALU.mult)
                pq = prep_bank()
                k.mm(pq[:, :n], a2s[ds, gs], awt[ds, :n])
                k.act(P.aa[:, :n], pq[:, :n], AF.Sigmoid, bias=V('a0', d * 8 + g))
                k.ts('pool', P.t1[:, :n], P.aa[:, :n], V('k_a', g), ALU.mult, V('omka', g), ALU.add)
                k.tt('pool', P.kd[:, :n], P.t1[:, :n], P.kx[:, :n], ALU.mult)
                k.tt('pool', P.bb[:, :n], P.aa[:, :n], P.kk[:, :n], ALU.mult)
                k.tt('pool', P.t1[:, :n], P.rr[:, :n], P.kd[:, :n], ALU.mult)
                k.ts('pool', P.t1[:, :n], P.t1[:, :n], V('r_k', g), ALU.mult)
                pq = prep_bank()
                k.mm(pq[:, :n], onesblk, P.t1[:, :n])
                k.tt('dve', P.bv[:, :n], pq[:, :n], P.vv[:, :n], ALU.mult)
                nchk = n // CH
                k.scan('dve', P.lam[:, :n], rst[:, :n], P.lw[:, :n], 0.0, ALU.mult, ALU.add)
                lam3 = P.lam[:, :n].rearrange("p (c j) -> p c j", j=CH)
                tot_b = lam3[:, :, CH - 1:CH].to_broadcast([128, nchk, CH])
                k.tt('pool', P.pex[:, :n], P.lam[:, :n], P.lw[:, :n], ALU.subtract)
                k.tt('pool', P.rem[:, :n].rearrange("p (c j) -> p c j", j=CH), tot_b, lam3, ALU.subtract)
                if d == 0:
                    L, Lex, Lrem = P.lam, P.pex, P.rem
                else:
                    k.tt('pool', P.Lb[:, :n], P.rem[:, :n], P.lw[:, :n], ALU.add)
                    L, Lex, Lrem = P.Lb, P.rem, P.pex
                k.act(P.e1[:, :n], Lex[:, :n], AF.Exp)
                k.tt('pool', P.KR[:, 0, :n], P.kk[:, :n], P.e1[:, :n], ALU.mult)
                k.stt('dve', P.KP0[:, :n], P.kk[:, :n], hmask[0], P.e1[:, :n], ALU.mult, ALU.mult)
                k.stt('dve', P.KP1[:, :n], P.kk[:, :n], hmask[1], P.e1[:, :n], ALU.mult, ALU.mult)
                k.act(P.e2[:, :n], L[:, :n], AF.Exp)
                k.tt('pool', P.KR[:, 1, :n], P.rr[:, :n], P.e2[:, :n], ALU.mult)
                k.act(P.e3[:, :n], L[:, :n], AF.Exp, scale=-1.0)
                k.tt('pool', P.BQ[:, :n], P.bb[:, :n], P.e3[:, :n], ALU.mult)
                k.stt('dve', P.BQ0[:, :n], P.bb[:, :n], hmask[0], P.e3[:, :n], ALU.mult, ALU.mult)
                k.stt('dve', P.BQ1[:, :n], P.bb[:, :n], hmask[1], P.e3[:, :n], ALU.mult, ALU.mult)
                k.stt('dve', P.KQ0[:, :n], P.kd[:, :n], hmask[0], P.e3[:, :n], ALU.mult, ALU.mult)
                k.stt('dve', P.KQ1[:, :n], P.kd[:, :n], hmask[1], P.e3[:, :n], ALU.mult, ALU.mult)
                k.act(P.e4[:, :n], Lrem[:, :n], AF.Exp)
                k.stt('dve', P.nBQP[:, :n], P.bb[:, :n], -1.0, P.e4[:, :n], ALU.mult, ALU.mult)
                k.tt('pool', P.KQP[:, :n], P.kd[:, :n], P.e4[:, :n], ALU.mult)
                k.act(P.PC[:, :nchk], lam3[:, :, CH - 1], AF.Exp)

            def unit(P, U, d, g, ti):
                k = kr
                sl = slice(ti * 128, (ti + 1) * 128)
                H = [slice(0, 64), slice(64, 128)]
                pT = ubank(U)
                pT4 = pT[:, :].rearrange("p (a t) -> p a t", a=4)
                k.tr(pT4[:, 0, :], P.vv[:, sl], ident)
                k.tr(pT4[:, 1, :], P.KR[:, 0, sl].bitcast(F32), ident)
                k.tr(pT4[:, 2, :], P.nBQP[:, sl], ident)
                k.tr(pT4[:, 3, :], P.KQP[:, sl], ident)
                k.copy('act', U.TM[:], pT4)
                yield
                pA = ubank(U)
                pA4 = pA[:, :].rearrange("p (h w t) -> p h w t", h=2, w=2)
                BQh, KQh, KPh = [P.BQ0, P.BQ1], [P.KQ0, P.KQ1], [P.KP0, P.KP1]
                for hh in range(2):
                    k.mm(pA4[:, hh], BQh[hh][:, sl], P.KR[:, :, sl])
                k.tt('dve', U.ZA[:], pA4, mA[:, d].unsqueeze(1).to_broadcast([128, 2, 2, 128]), ALU.mult)
                pB = ubank(U)
                pB4 = pB[:, :].rearrange("p (h w t) -> p h w t", h=2, w=2)
                for hh in range(2):
                    k.mm(pB4[:, hh], KQh[hh][:, sl], P.KR[:, :, sl])
                k.tt('dve', U.ZB[:], pB4, mB[:, d].unsqueeze(1).to_broadcast([128, 2, 2, 128]), ALU.mult)
                pC = ubank(U)
                pC3 = pC[:, 0:256].rearrange("p (h t) -> p h t", h=2)
                for hh in range(2):
                    k.mm(pC3[:, hh], KPh[hh][:, sl], P.BQ[:, sl])
                k.tt('dve', U.AA[:], pC3, mT[:, d].unsqueeze(1).to_broadcast([128, 2, 128]), ALU.mult)
                yield
                Zc = [U.ZA[:, 0, 0, :], U.ZA[:, 1, 0, :]]
                Ac = [U.AA[:, 0, :], U.AA[:, 1, :]]
                Zp = [Zc]
                for lev in range(5):
                    pQ = ubank(U)
                    pQ4 = pQ[:, :].rearrange("p (w h t) -> p w h t", w=2, h=2)
                    for hh in range(2):
                        k.mm(pQ4[:, 0, hh], Ac[hh], Zc[hh])
                        if lev < 4:
                            k.mm(pQ4[:, 1, hh], Zc[hh], Ac[hh])
                    ZZl = U.ZZ[lev]
                    if lev < 4:
                        k.copy('act' if lev % 2 else 'dve', ZZl[:], pQ4)
                    else:
                        k.copy('dve', ZZl[:, 0], pQ4[:, 0])
                    Zc = [ZZl[:, 0, 0, :], ZZl[:, 0, 1, :]]
                    Ac = [ZZl[:, 1, 0, :], ZZl[:, 1, 1, :]]
                    Zp.append(Zc)
                    yield
                pX = ubank(U)
                pX3 = pX[:, 0:256].rearrange("p (h c) -> p h c", h=2)
                for hh in range(2):
                    k.mm(pX3[:, hh, 0:64], U.ZB[:, hh, 0, :], U.TM[:, 0, H[hh]])
                X = U.X[0]
                k.copy('act', X[:, :, 0:64], pX3[:, :, 0:64])
                k.copy('pool', X[:, :, 64:128], U.TM[:, 1, :].rearrange("p (h c) -> p h c", h=2))
                yield
                for lev in range(6):
                    pP = ubank(U)
                    pP3 = pP[:, 0:256].rearrange("p (h c) -> p h c", h=2)
                    for hh in range(2):
                        k.mm(pP3[:, hh, :], Zp[lev][hh], X[:, hh, :])
                    Xn = U.X[(lev + 1) % 2]
                    k.tt('dve', Xn[:], X[:], pP3, ALU.subtract if lev == 0 else ALU.add)
                    X = Xn
                    yield
                pMNs = [ubank(U), ubank(U)]
                for c2 in range(2):
                    cs = H[c2]
                    pMN = pMNs[c2]
                    for hh in range(2):
                        k.mm(pMN[H[hh], 0:64], X[cs, hh, 64:128], U.TM[cs, 2, H[hh]])
                        k.mm(pMN[H[hh], 64:128], U.TM[cs, 3, H[hh]], U.TM[cs, 0, H[hh]], start=True, stop=False)
                        k.mm(pMN[H[hh], 64:128], U.TM[cs, 2, H[hh]], X[cs, hh, 0:64], start=False, stop=True)
                for c2 in range(2):
                    k.stt('dve', U.MT[:, c2, :], identblk, P.PC[:, ti * 2 + c2:ti * 2 + c2 + 1],
                          pMNs[c2][:, 0:64], ALU.mult, ALU.add)
                    k.copy('act', U.NN[:, c2, :], pMNs[c2][:, 64:128])
                pR = ubank(U)
                for hh in range(2):
                    k.mm(pR[H[hh], 0:128], X[:, hh, 64:128], U.ZA[:, hh, 1, :])
                k.tt('dve', U.RPp[:], P.KR[:, 1, sl], pR[:, 0:128], ALU.add)
                pY = U.ybank
                for hh in range(2):
                    k.mm(pY[H[hh], 0:128], U.TM[:, 0, H[hh]], U.ZB[:, hh, 1, :], start=True, stop=False, skip=True)
                    k.mm(pY[H[hh], 0:128], X[:, hh, 0:64], U.ZA[:, hh, 1, :], start=False, stop=False, skip=True)
                yield
                for c2 in ((0, 1) if d == 0 else (1, 0)):
                    cs = H[c2]
                    for hh in range(2):
                        k.mm(pY[H[hh], cs], Sst[g][H[hh], :], U.RPp[H[hh], cs], start=False, stop=True, skip=True)
                    pS = ubank(U)
                    for hh in range(2):
                        k.mm(pS[H[hh], 0:64], U.MT[H[hh], c2, :], Sst[g][H[hh], :])
                    k.tt('dve', Sst[g][:], pS[:, 0:64], U.NN[:, c2, :], ALU.add)
                    yield
                k.copy('act', P.Yb[:, sl], pY[:, 0:128])
                yield

            def block_gen(P, U, d, g, n):
                tiles = list(range(n // 128))
                if d == 1:
                    tiles = tiles[::-1]
                for ti in tiles:
                    yield from unit(P, U, d, g, ti)

            def finalize(P, d, t0, n, g, sga, sgb):
                rows = slice(g * 128, (g + 1) * 128)
                if d == 0:
                    k.dma(y0T[rows, t0:t0 + n], P.Yb[:, :n], q='act')
                    k.dma(bv0T[rows, t0:t0 + n], P.bv[:, :n], q='act')
                    return
                k.dma(P.t1[:, :n], y0T[rows, t0:t0 + n])
                k.dma(P.t2[:, :n], bv0T[rows, t0:t0 + n])
                k.tt('pool', P.Yb[:, :n], P.Yb[:, :n], P.t1[:, :n], ALU.add)
                k.tt('pool', P.bv[:, :n], P.bv[:, :n], P.t2[:, :n], ALU.add)
                pq = prep_bank()
                k.mm(pq[:, :n], ones64, P.Yb[:, :n])
                k.copy('act', P.e1[:, :n], pq[:, :n])
                k.tt('pool', P.kk[:, :n], P.Yb[:, :n], P.Yb[:, :n], ALU.mult)
                pq2 = prep_bank()
                k.mm(pq2[:, :n], ones64, P.kk[:, :n])
                k.tt('pool', P.e3[:, :n], P.e1[:, :n], P.e1[:, :n], ALU.mult)
                k.tt('dve', P.e3[:, :n], pq2[:, :n], P.e3[:, :n], ALU.subtract)
                k.act(P.e3[:, :n], P.e3[:, :n], AF.Sqrt, bias=gneps[:, 0:1])
                k.recip(P.e3[:, :n], P.e3[:, :n])
                k.tt('pool', P.kd[:, :n], P.Yb[:, :n], P.e1[:, :n], ALU.subtract)
                k.tt('pool', P.kd[:, :n], P.kd[:, :n], P.e3[:, :n], ALU.mult)
                k.ts('pool', P.kd[:, :n], P.kd[:, :n], V('lnx_g', g), ALU.mult, V('lnx_b', g), ALU.add)
                k.tt('pool', P.kd[:, :n], P.kd[:, :n], P.bv[:, :n], ALU.add)
                pq3 = prep_bank()
                k.mm(pq3[:, :n], g2s[:, 0, rows], sga[:, :n], start=True, stop=False)
                k.mm(pq3[:, :n], g2s[0:32, 1, rows], sgb[0:32, :n], start=False, stop=True)
                k.tt('dve', P.e1[:, :n], P.kd[:, :n], pq3[:, :n], ALU.mult)
                k.dma(ogT[rows, t0:t0 + n], P.e1[:, :n], q='act')

            ctxb = [b_ for b_ in blocks if b_[2] == 0]
            latb = [b_ for b_ in blocks if b_[2] == 1]
            for d in range(2):
                for g in range(8):
                    k.memset('pool', Sst[g][:].bitcast(F32), 0.0)
                order = (ctxb + latb) if d == 0 else (ctxb[::-1] + latb[::-1])
                for bi, (t0, n, seg) in enumerate(order):
                    tw, awt, sga, sgb = lor[0]
                    k.dma(tw[:, :n], zT[24 * 128:25 * 128, t0:t0 + n])
                    k.dma(awt[:, :n], zT[25 * 128:26 * 128, t0:t0 + n])
                    if d == 1:
                        k.dma(sga[:, :n], zT[26 * 128:27 * 128, t0:t0 + n])
                        k.dma(sgb[:, :n], zT[27 * 128:28 * 128, t0:t0 + n])
                    for g0 in range(0, 8, NS):
                        gens = []
                        for s in range(NS):
                            prep(Ps[s], d, t0, n, g0 + s, tw, awt)
                            gens.append(block_gen(Ps[s], Us[s], d, g0 + s, n))
                        alive = list(range(NS))
                        while alive:
                            for s in list(alive):
                                try:
                                    next(gens[s])
                                except StopIteration:
                                    alive.remove(s)
                        for s in range(NS):
                            finalize(Ps[s], d, t0, n, g0 + s, sga, sgb)
            k.pop()
            if stop_after == 'C':
                break
            act_blocks = [b_ for b_ in blocks if not (last and b_[2] == 0)]
            k.push()
            cza = k.sb(f"cza{l}", [128, T])
            czb = k.sb(f"czb{l}", [128, T])
            cacc = [k.sb(f"cacc{l}_{i}", [128, T]) for i in range(2)]
            tlo = CTX if last else 0
            for c in range(NCH):
                k.dma(cza[:, tlo:T], zT[(28 + c) * 128:(29 + c) * 128, tlo:T])
                k.dma(czb[:, tlo:T], zT[(36 + c) * 128:(37 + c) * 128, tlo:T])
                k.act(czb[:, tlo:T], czb[:, tlo:T], AF.Sigmoid)
                k.tt('pool', cza[:, tlo:T], cza[:, tlo:T], czb[:, tlo:T], ALU.mult)
                segs = [(CTX, SEQ, GRID_W)] + ([] if last else [(0, CTX, 1)])
                for (s0, sl_, stride) in segs:
                    used = [False, False]
                    for kk_ in range(CONV_K):
                        off = (kk_ - CONV_K // 2) * stride
                        lo, hi = max(0, -off), min(sl_, sl_ - off)
                        if hi <= lo:
                            continue
                        ai = 0 if kk_ == CONV_K // 2 else 1 + 0 * kk_
                        ai = kk_ % 2 if kk_ != CONV_K // 2 else 0
                        e_ = 'dve' if ai == 0 else 'pool'
                        wcol = V('conv_w', c * CONV_K + kk_)
                        if kk_ == CONV_K // 2:
                            pass
                        dst = cacc[ai][:, s0 + lo:s0 + hi]
                        srcu = cza[:, s0 + lo + off:s0 + hi + off]
                        if not used[ai]:
                            k.memset(e_, cacc[ai][:, s0:s0 + sl_], 0.0)
                            used[ai] = True
                        k.stt(e_, dst, srcu, wcol, dst, ALU.mult, ALU.add)
                    if used[1]:
                        k.tt('dve', cacc[0][:, s0:s0 + sl_], cacc[0][:, s0:s0 + sl_], cacc[1][:, s0:s0 + sl_], ALU.add)
                k.dma(cvT[c * 128:(c + 1) * 128, tlo:T], cacc[0][:, tlo:T], q='act')
            k.pop()
            if stop_after == 'D':
                break

            k.push()
            wstage = k.sb(f"wstage{l}", [128, NCH, D])
            wts = {}
            for nme, src_w in (('oA', w_oA), ('oB', w_oB), ('out', w_out)):
                wts[nme] = k.sb(f"w{nme}{l}", [128, NCH, D], BF16)
                k.dma(wstage[:], src_w[l])
                k.copy('pool', wts[nme][:], wstage[:])
            lneps = k.sb(f"lneps{l}", [128, 1])
            k.memset('dve', lneps[:], LN_EPS)
            cvb = k.sb(f"cvb{l}", [128, NCH, 512])
            ogb = k.sb(f"ogb{l}", [128, NCH, 512])
            xbe = k.sb(f"xbe{l}", [128, NCH, 512])
            sB = k.sb(f"sB{l}", [128, NCH, 512], BF16)
            oB = k.sb(f"oB{l}", [128, NCH, 512], BF16)
            mTb = k.sb(f"mTb{l}", [128, NCH, 512], BF16)
            gat = [k.sb(f"gat{l}_{i}", [128, 512]) for i in range(4)]
            tE = [k.sb(f"tE{l}_{i}", [128, 512]) for i in range(4)]
            mean = k.sb(f"mean{l}", [128, 512])
            rsd = k.sb(f"rsd{l}", [128, 512])
            for (t0, n, seg) in act_blocks:
                sg = 1 - seg
                k.dma(cvb[:, :, :n], cvT[:, t0:t0 + n].rearrange("(c p) t -> p c t", p=128))
                k.dma(ogb[:, :, :n], ogT[:, t0:t0 + n].rearrange("(c p) t -> p c t", p=128))
                k.dma(xbe[:, :, :n], xres[:, t0:t0 + n].rearrange("(c p) t -> p c t", p=128))
                pm_, pq_ = pb[0], pb[1]
                for c in range(NCH):
                    k.mm(pm_[:, :n], onesD, cvb[:, c, :n], start=(c == 0), stop=(c == NCH - 1))
                for c in range(NCH):
                    k.act(tE[c % 2][:, :n], cvb[:, c, :n], AF.Square)
                    k.mm(pq_[:, :n], onesD, tE[c % 2][:, :n], start=(c == 0), stop=(c == NCH - 1))
                k.copy('act', mean[:, :n], pm_[:, :n])
                k.tt('pool', rsd[:, :n], mean[:, :n], mean[:, :n], ALU.mult)
                k.tt('dve', rsd[:, :n], pq_[:, :n], rsd[:, :n], ALU.subtract)
                k.act(rsd[:, :n], rsd[:, :n], AF.Sqrt, bias=lneps[:, 0:1])
                k.recip(rsd[:, :n], rsd[:, :n])
                for c in range(NCH):
                    e_ = k.ve()
                    t_ = tE[2 + c % 2]
                    k.tt(e_, t_[:, :n], cvb[:, c, :n], mean[:, :n], ALU.subtract)
                    k.tt(e_, t_[:, :n], t_[:, :n], rsd[:, :n], ALU.mult)
                    k.ts(e_, t_[:, :n], t_[:, :n], V('cnorm_g', c), ALU.mult, V('cnorm_b', c), ALU.add)
                    k.act(sB[:, c, :n], t_[:, :n], AF.Silu)
                    k.copy(e_, oB[:, c, :n], ogb[:, c, :n])
                for oc in range(NCH):
                    ocs = slice(oc * 128, (oc + 1) * 128)
                    ga_, gb_ = gat[(oc % 2) * 2], gat[(oc % 2) * 2 + 1]
                    k.dma(ga_[:, :n], zT[(44 + oc) * 128:(45 + oc) * 128, t0:t0 + n])
                    k.dma(gb_[:, :n], zT[(52 + oc) * 128:(53 + oc) * 128, t0:t0 + n])
                    k.act(ga_[:, :n], ga_[:, :n], AF.Sigmoid, bias=V('gate_b', oc))
                    k.act(gb_[:, :n], gb_[:, :n], AF.Sigmoid, bias=V('gate_b', 8 + oc))
                    pa_, pb_ = pb[2 + (oc % 2) * 2], pb[3 + (oc % 2) * 2]
                    for c in range(NCH):
                        k.mm(pa_[:, :n], wts['oA'][:, c, ocs], oB[:, c, :n], start=(c == 0), stop=(c == NCH - 1))
                    for c in range(NCH):
                        k.mm(pb_[:, :n], wts['oB'][:, c, ocs], sB[:, c, :n], start=(c == 0), stop=(c == NCH - 1))
                    k.tt('dve', ga_[:, :n], pa_[:, :n], ga_[:, :n], ALU.mult)
                    k.tt('dve', gb_[:, :n], pb_[:, :n], gb_[:, :n], ALU.mult)
                    k.tt('pool', mTb[:, oc, :n], ga_[:, :n], gb_[:, :n], ALU.add)
                for oc in range(NCH):
                    ocs = slice(oc * 128, (oc + 1) * 128)
                    po_ = pb[6 + oc % 2]
                    for c in range(NCH):
                        k.mm(po_[:, :n], wts['out'][:, c, ocs], mTb[:, c, :n], start=(c == 0), stop=(c == NCH - 1))
                    k.stt('dve', xbe[:, oc, :n], po_[:, :n], modT[:, 2 * 8 + oc, sg:sg + 1], xbe[:, oc, :n], ALU.mult, ALU.add)
                k.dma(xres[:, t0:t0 + n].rearrange("(c p) t -> p c t", p=128), xbe[:, :, :n], q='act')
            k.pop()
            if stop_after == 'E':
                break
            k.push()
            cf = [k.sb(f"pcf{l}_{i}", [128, 4096]) for i in range(2)]
            cbf = [k.sb(f"pcb{l}_{i}", [128, 4096], BF16) for i in range(2)]
            ci = 0
            for grp in range(32):
                for which in range(2):
                    f_, b_ = cf[ci % 2], cbf[ci % 2]
                    if which == 0:
                        k.dma(f_[:, :].rearrange("p (a b) -> p a b", a=NCH), puT[l, grp])
                    else:
                        k.dma(f_[:, :].rearrange("p (a b) -> p a b", a=4), pv[l, grp])
                    k.copy('pool' if ci % 2 else 'dve', b_[:], f_[:])
                    if which == 0:
                        k.dma(puTb[grp], b_[:, :].rearrange("p (a b) -> p a b", a=NCH), q='act')
                    else:
                        k.dma(pvb[grp], b_[:, :].rearrange("p (a b) -> p a b", a=4), q='act')
                    ci += 1
            k.pop()
            k.push()
            wqs = k.sb(f"wqs{l}", [128, NCH, 2 * D], BF16)
            skTs = k.sb(f"skTs{l}", [128, 16, 128], BF16)
            identb = k.sb(f"identb{l}", [128, 128], BF16)
            k.copy('dve', identb[:], ident)
            k.push()
            wqst = [k.sb(f"wqst{l}_{i}", [128, NCH, 512]) for i in range(2)]
            for j in range(4):
                k.dma(wqst[j % 2][:], w_q[l][:, :, j * 512:(j + 1) * 512])
                k.copy('pool' if j % 2 else 'dve', wqs[:, :, j * 512:(j + 1) * 512], wqst[j % 2][:])
            skst = k.sb(f"skst{l}", [128, 16, 128])
            k.dma(skst[:], skT[l].rearrange("q d j -> d q j"))
            k.copy('dve', skTs[:], skst[:])
            k.pop()
            Gt = [k.sb(f"G{l}_{i}", [128, NEXP], BF16) for i in range(2)]
            xbf = k.sb(f"xbf{l}", [128, NCH, 256])
            h2 = k.sb(f"h2{l}", [128, NCH, 256], BF16)
            qT = k.sb(f"qT{l}", [128, 16, 256], BF16)
            ssb = k.sb(f"ssb{l}", [128, 16, 128])
            qTf = qT[:, :, :].bitcast(F32)
            thn2 = k.sb(f"thn2{l}", [128, 2, 8])
            T16 = k.sb(f"T16{l}", [128, 16, 16])
            tmpk = k.sb(f"tmpk{l}", [128, 128])
            negm = k.sb(f"negm{l}", [128, 16])
            cand = k.sb(f"cand{l}", [128, 256])
            candt = k.sb(f"candt{l}", [128, 256])
            c16 = k.sb(f"c16{l}", [128, 8, 16])
            w16 = k.sb(f"w16{l}", [128, 8, 16])
            smx = k.sb(f"smx{l}", [128, 8])
            Zs = k.sb(f"Zs{l}", [128, 8])
            rZ = k.sb(f"rZ{l}", [128, 8])
            thn = k.sb(f"thn{l}", [128, 8])
            Pq = [k.sb(f"Pq{l}_{i}", [128, 8, 128]) for i in range(2)]
            Gh = [k.sb(f"Gh{l}_{i}", [128, 8, 128], BF16) for i in range(2)]
            UTs = [k.sb(f"UTs{l}_{i}", [128, NCH, 512], BF16) for i in range(2)]
            Vs = [k.sb(f"Vs{l}_{i}", [128, 4, D], BF16) for i in range(2)]
            gab = [k.sb(f"gab{l}_{i}", [128, 256]) for i in range(2)]
            WT = [k.sb(f"WT{l}_{i}", [128, 256], BF16) for i in range(2)]
            pblocks = [b_ for b_ in token_blocks(CTX, SEQ, bs=256) if not (last and b_[2] == 0)]
            for (t0, n, seg) in pblocks:
                sg = 1 - seg
                ntile = n // 128
                k.dma(xbf[:, :, :n], xres[:, t0:t0 + n].rearrange("(c p) t -> p c t", p=128))
                modulate_block(h2, xbf, n, 1, sg, f"F{l}")
                for qc in range(16):
                    pq = pb[4 + qc % 4]
                    for dh in range(NCH):
                        k.mm(pq[:, :n], wqs[:, dh, qc * 128:(qc + 1) * 128], h2[:, dh, :n], start=(dh == 0), stop=(dh == NCH - 1))
                    k.copy('act' if qc % 2 else 'dve', qT[:, qc, :n], pq[:, :n])
                Ebs = [ssb, qT[:, :, :].bitcast(F32).rearrange("p q (a j) -> p (q a) j", j=128) if False else None]
                Ebs[1] = qTf
                for ti in range(ntile):
                    tsl = slice(ti * 128, (ti + 1) * 128)
                    Eb = Ebs[ti]
                    for qc in range(16):
                        k.mm(pb[qc // 4][:, (qc % 4) * 128:(qc % 4 + 1) * 128], qT[:, qc, tsl], skTs[:, qc, :])
                    for b4 in range(4):
                        k.copy('act' if b4 % 2 else 'dve', Eb[:, b4 * 4:(b4 + 1) * 4, :],
                               pb[b4][:, :].rearrange("p (q j) -> p q j", q=4))
                for ti in range(ntile):
                    Eb = Ebs[ti]
                    for qc in range(16):
                        k.max8(T16[:, qc, 0:8], Eb[:, qc, :])
                        k.match_replace(tmpk[:], T16[:, qc, 0:8], Eb[:, qc, :], -1e30)
                        k.max8(T16[:, qc, 8:16], tmpk[:])
                    T16v = T16[:, :, :].rearrange("p (h c) k -> p h c k", c=2)
                    for h in range(PH):
                        k.tt('pool', cand[:, :].rearrange("p (a b) -> p a b", a=16),
                             T16v[:, h, 0, :].unsqueeze(2).to_broadcast([128, 16, 16]),
                             T16v[:, h, 1, :].unsqueeze(1).to_broadcast([128, 16, 16]), ALU.add)
                        k.max8(c16[:, h, 0:8], cand[:, :])
                        k.match_replace(candt[:], c16[:, h, 0:8], cand[:, :], -1e30)
                        k.max8(c16[:, h, 8:16], candt[:])
                    k.tt('pool', smx[:], T16v[:, :, 0, 0], T16v[:, :, 1, 0], ALU.add)
                    k.tt('pool', w16[:], c16[:], smx[:, :].unsqueeze(2).to_broadcast([128, 8, 16]), ALU.subtract)
                    k.act(w16[:], w16[:], AF.Exp)
                    k.reduce('dve', Zs[:], w16[:], AX.X, ALU.add)
                    k.recip(rZ[:], Zs[:])
                    k.stt('pool', thn2[:, ti, :], w16[:, :, 15], 0.999, rZ[:], ALU.mult, ALU.mult)
                    k.ts('pool', negm[:], T16[:, :, 0], -1.0, ALU.mult)
                    for qc in range(16):
                        k.act(Eb[:, qc, :], Eb[:, qc, :], AF.Exp, bias=negm[:, qc:qc + 1])
                    for h in range(PH):
                        k.ts('pool', Eb[:, 2 * h, :], Eb[:, 2 * h, :], rZ[:, h:h + 1], ALU.mult)
                cnt_p = [0]

                def gbuild(sl16):
                    isl = slice(sl16 * 8, (sl16 + 1) * 8)
                    for ti in range(ntile):
                        Eb = Ebs[ti]
                        _ALIAS[Gt[ti][:, 0:1].tensor.name] = f"Gt{ti}_s{sl16}"
                        Gs = Gt[ti][:, sl16 * 1024:(sl16 + 1) * 1024].rearrange("p (i j) -> p i j", i=8)
                        for h in range(PH):
                            P_, Gh_ = Pq[cnt_p[0] % 2], Gh[cnt_p[0] % 2]
                            cnt_p[0] += 1
                            k.tt('pool' if cnt_p[0] % 5 in (0, 2, 4) else 'dve', P_[:],
                                 Eb[:, 2 * h, isl].unsqueeze(2).to_broadcast([128, 8, 128]),
                                 Eb[:, 2 * h + 1, :].unsqueeze(1).to_broadcast([128, 8, 128]), ALU.mult)
                            if h == 0:
                                k.stt('dve', Gs, P_[:], thn2[:, ti, h:h + 1], P_[:], ALU.is_ge, ALU.mult)
                            else:
                                k.stt('dve', Gh_[:], P_[:], thn2[:, ti, h:h + 1], P_[:], ALU.is_ge, ALU.mult)
                                k.tt('pool' if h in (2, 5, 7) else 'dve', Gs, Gs, Gh_[:], ALU.add)

                def eloop(sl16):
                    for grp in (2 * sl16, 2 * sl16 + 1):
                        UT_, V_ = UTs[grp % 2], Vs[grp % 2]
                        k.dma(UT_[:], puTb[grp])
                        k.dma(V_[:], pvb[grp])
                        for ec in range(4):
                            e = grp * 4 + ec
                            pa = pb[e % 2]
                            for dh in range(NCH):
                                k.mm(pa[:, :n], UT_[:, dh, ec * 128:(ec + 1) * 128], h2[:, dh, :n], start=(dh == 0), stop=(dh == NCH - 1))
                            ga_ = gab[e % 2]
                            k.act(ga_[:, :n], pa[:, :n], AF.Gelu)
                            pgb = pb[2 + e % 2][:, :].bitcast(BF16)
                            for ti in range(ntile):
                                _ALIAS[Gt[ti][:, 0:1].tensor.name] = f"Gt{ti}_s{sl16}"
                                k.tr(pgb[:, ti * 128:(ti + 1) * 128], Gt[ti][:, e * 128:(e + 1) * 128], identb[:])
                            WT_ = WT[e % 2]
                            k.tt('dve', WT_[:, :n], ga_[:, :n], pgb[:, :n], ALU.mult)
                            for oc in range(NCH):
                                acc = pb[4 + oc // 2][:, (oc % 2) * 256:(oc % 2) * 256 + n]
                                k.mm(acc, V_[:, ec, oc * 128:(oc + 1) * 128], WT_[:, :n],
                                     start=(e == 0 and oc % 2 == 0), stop=(e == 127), skip=True)
                gbuild(0)
                gbuild(1)
                for sl16 in range(16):
                    eloop(sl16)
                    if sl16 + 2 < 16:
                        gbuild(sl16 + 2)
                for oc in range(NCH):
                    acc = pb[4 + oc // 2][:, (oc % 2) * 256:(oc % 2) * 256 + n]
                    k.stt('dve', xbf[:, oc, :n], acc, modT[:, 5 * 8 + oc, sg:sg + 1], xbf[:, oc, :n], ALU.mult, ALU.add)
                k.dma(xres[:, t0:t0 + n].rearrange("(c p) t -> p c t", p=128), xbf[:, :, :n], q='act')
            k.pop()
            if stop_after == 'F':
                break

        if stop_after is None:
            k.push()
            l = NL
            xfb = [k.sb(f"xfb{i}", [128, NCH, 512]) for i in range(2)]
            for bi, (t0, n, seg) in enumerate([b_ for b_ in blocks if b_[2] == 1]):
                xb_ = xfb[bi % 2]
                k.dma(xb_[:, :, :n], xres[:, t0:t0 + n].rearrange("(c p) t -> p c t", p=128))
                pss = pb[bi % 2]
                for c in range(NCH):
                    k.act(sqs[:, :n], xb_[:, c, :n], AF.Square)
                    k.mm(pss[:, :n], onesD, sqs[:, :n], start=(c == 0), stop=(c == NCH - 1))
                k.act(rstd[:, :n], pss[:, :n], AF.Sqrt, bias=epsn[:, 0:1])
                k.recip(rstd[:, :n], rstd[:, :n])
                for c in range(NCH):
                    k.stt(k.ve(), xb_[:, c, :n], xb_[:, c, :n], V('final_g', c), rstd[:, :n], ALU.mult, ALU.mult)
                k.dma(outT[:, t0 - CTX:t0 - CTX + n].rearrange("(c p) t -> p c t", p=128), xb_[:, :, :n], q='act')
            k.pop()
        S.finish()
        S.emit()
        nc._dbg = dbg
        nc._nops = S.nops
    return nc


def _fm(v):
    v = np.asarray(v, np.float32).reshape(-1, 128)
    return np.ascontiguousarray(v.T)


def make_consts():
    c = np.zeros((128, 10, 128), np.float32)
    i = np.arange(128)[:, None]
    t = np.arange(128)[None, :]
    same = (i // 64) == (t // 64)
    c[:, 0, :] = (i == t)
    c[:, 1, :] = same
    c[:, 2, :] = 1.0 / D
    c[:, 3, :] = same & (i < t)
    c[:, 4, :] = same & (i <= t)
    c[:, 5, :] = same & (i > t)
    c[:, 6, :] = same & (i >= t)
    c[:, 7, :] = ((i % 64) == t)
    c[:, 8, :] = same / 64.0
    return c


def prepare_shared(inp, NL):
    f32 = lambda a: np.asarray(a, np.float32)
    sh = {}
    ada_w = f32(inp['ada_w'])
    sh['ada_w'] = np.ascontiguousarray(ada_w.reshape(NL, 8, 128, 48, 128).transpose(0, 3, 2, 1, 4))
    vec = np.zeros((NL, 128, NV), np.float32)

    def put(l, name, arr):
        o, w = VEC[name]
        assert arr.shape == (128, w), (name, arr.shape, w)
        vec[l, :, o:o + w] = arr
    for l in range(NL):
        for nme in ('norm1_g', 'norm2_g', 'k_k', 'k_a', 'lnx_g', 'lnx_b', 'cnorm_g', 'cnorm_b', 'gate_b'):
            put(l, nme, _fm(f32(inp[nme])[l]))
        put(l, 'r_k', _fm(f32(inp['r_k'])[l].reshape(-1)))
        put(l, 'w0', _fm(f32(inp['w0'])[l].reshape(-1)))
        put(l, 'a0', _fm(f32(inp['a0'])[l].reshape(-1)))
        put(l, 'final_g', _fm(f32(inp['final_g'])))
        mu = np.zeros((2, 3584), np.float32)
        mu[:, :3488] = f32(inp['shift_mu'])[l]
        put(l, 'mu0', _fm(mu[0]))
        put(l, 'mu1', _fm(mu[1]))
        put(l, 'ada_b', _fm(f32(inp['ada_b'])[l]))
        cw = f32(inp['conv_w'])[l]
        put(l, 'conv_w', np.ascontiguousarray(cw.T.reshape(8, 128, CONV_K).transpose(1, 0, 2)).reshape(128, 8 * CONV_K))
    sh['vec'] = vec
    w_in = f32(inp['w_in'])
    wp = np.zeros((NL, D, P_IN_PAD), np.float32)
    wp[:, :, :3488] = w_in[:, :, :3488]
    wp[:, :, 3584:] = w_in[:, :, 3488:]
    sh['w_in'] = np.ascontiguousarray(wp.reshape(NL, 8, 128, NFC, 128).transpose(0, 3, 2, 1, 4))
    sh['w2'] = np.ascontiguousarray(f32(inp['w2']).reshape(NL, 128, D))
    sh['a2'] = np.ascontiguousarray(f32(inp['a2']).reshape(NL, 128, D))
    g2p = np.zeros((NL, 256, D), np.float32)
    g2p[:, :LORA_G] = f32(inp['g2'])
    sh['g2'] = g2p.reshape(NL, 2, 128, D)
    for nme in ('w_oA', 'w_oB', 'w_out'):
        sh[nme] = np.ascontiguousarray(f32(inp[nme]).reshape(NL, 8, 128, D).transpose(0, 2, 1, 3))
    sh['w_q'] = np.ascontiguousarray(f32(inp['w_q']).reshape(NL, 8, 128, 2 * D).transpose(0, 2, 1, 3))
    sh['skT'] = np.ascontiguousarray(f32(inp['sub_keys']).reshape(NL, 16, 128, 128).transpose(0, 1, 3, 2))
    sh['puT'] = np.ascontiguousarray(f32(inp['peer_u']).reshape(NL, 32, 512, 8, 128).transpose(0, 1, 4, 3, 2))
    sh['pv'] = np.ascontiguousarray(f32(inp['peer_v']).reshape(NL, 32, 4, 128, D).transpose(0, 1, 3, 2, 4))
    sh['consts'] = make_consts()
    return sh


def prepare_core(inp, b):
    f32 = lambda a: np.asarray(a, np.float32)
    xT = np.ascontiguousarray(np.concatenate([f32(inp['ctx'])[b], f32(inp['x'])[b]], axis=0).T)
    cc = np.stack([_fm(f32(inp['c'])[b]), _fm(f32(inp['c_ctx']))], axis=-1)
    return {'xT': xT, 'cc': np.ascontiguousarray(cc)}


def kernel(**inputs):
    B, SEQ, _ = inputs['x'].shape
    CTX = inputs['ctx'].shape[1]
    NL = inputs['w_in'].shape[0]
    cfg = dict(CTX=CTX, SEQ=SEQ, NL=NL)
    nc = build(cfg)
    sh = prepare_shared(inputs, NL)
    in_maps = []
    for b in range(B):
        m = dict(sh)
        m.update(prepare_core(inputs, b))
        in_maps.append(m)
    res = run_bass_kernel_spmd(nc, in_maps, core_ids=list(range(B)))
    out = np.stack([np.ascontiguousarray(np.asarray(r['outT']).T) for r in res.results], axis=0)
    return out.astype(np.float32)
```

```python
from contextlib import ExitStack
import math
import numpy as np
import concourse.bass as bass
import concourse.mybir as mybir
from concourse.bass_utils import run_bass_kernel_spmd

F32 = mybir.dt.float32
BF16 = mybir.dt.bfloat16
AF = mybir.ActivationFunctionType
ALU = mybir.AluOpType
AX = mybir.AxisListType

D = 1024
NCH = 8
HEADS = 16
HD = 64
LORA_G = 160
CONV_K = 31
GRID_W = 64
P_IN_PAD = 7680
NFC = 60
PH, PNK, PHALF, PTOPK = 8, 128, 128, 16
NEXP = PNK * PNK
NORM_EPS = 1e-6
LN_EPS = 1e-5
GN_EPS = HD * 1e-5
CH = 64
EXPM05 = math.exp(-0.5)
F32R = mybir.dt.float32r
RWKV_F32R = True


class Sched:
    GEN = 20000

    def __init__(self, nc, stack, ndma=24):
        self.nc = nc
        self.stack = stack
        self.eng = {'pe': nc.tensor, 'act': nc.scalar, 'dve': nc.vector, 'pool': nc.gpsimd, 'sp': nc.sync}
        self.prog = {e: [] for e in self.eng}
        self.cnt = {e: 0 for e in self.eng}
        self.gen = {e: -1 for e in self.eng}
        self.semh = {}
        for e in self.eng:
            self._newgen(e)
        self.ndma = ndma
        self.dmaval = [0] * ndma
        self.dmarr = 0
        for i in range(ndma):
            self.semh[('dma', i)] = stack.enter_context(nc.semaphore(f"sdma{i}"))
        self.waited = {}
        self.lastw = {}
        self.readers = {}
        self.nops = 0

    def _newgen(self, e):
        self.gen[e] += 1
        self.cnt[e] = 0
        self.semh[(e, self.gen[e])] = self.stack.enter_context(self.nc.semaphore(f"s{e}{self.gen[e]}"))

    def _wait(self, eng, key, val):
        if self.waited.get((eng, key), 0) >= val:
            return
        self.waited[(eng, key)] = val
        self.prog[eng].append(('wait', self.semh[key], val))

    def _deps(self, eng, reads, writes):
        toks = {}

        def add(d):
            for k, v in d.items():
                if toks.get(k, 0) < v:
                    toks[k] = v
        for b in reads:
            add(self.lastw.get(b, {}))
        for b in writes:
            add(self.lastw.get(b, {}))
            add(self.readers.get(b, {}))
        for k, v in toks.items():
            if eng == 'pe' and k[0] == 'pe':
                continue
            self._wait(eng, k, v)

    def _record(self, key, val, reads, writes):
        for b in writes:
            self.lastw[b] = {key: val}
            self.readers[b] = {}
        for b in reads:
            if b in writes:
                continue
            r = self.readers.setdefault(b, {})
            if r.get(key, 0) < val:
                r[key] = val

    def op(self, eng, fn, reads, writes):
        pr = [r for r in reads if r.startswith('pb') and r not in writes]
        if pr:
            writes = list(writes) + pr
        self._deps(eng, reads, writes)
        if self.cnt[eng] >= self.GEN:
            self._newgen(eng)
        self.cnt[eng] += 1
        key = (eng, self.gen[eng])
        val = self.cnt[eng]
        self.prog[eng].append(('op', fn, self.semh[key]))
        self._record(key, val, reads, writes)
        self.nops += 1

    def dma(self, q, out, in_, reads=None, writes=None):
        reads = [in_.tensor.name] if reads is None else reads
        writes = [out.tensor.name] if writes is None else writes
        self._deps(q, reads, writes)
        i = self.dmarr
        self.dmarr = (i + 1) % self.ndma
        key = ('dma', i)
        self._wait(q, key, self.dmaval[i])
        self.dmaval[i] += 16
        self.prog[q].append(('dma', out, in_, self.semh[key]))
        self._record(key, self.dmaval[i], reads, writes)
        self.nops += 1

    def barrier(self):
        for e in self.eng:
            for o in self.eng:
                if o != e and self.cnt[o] > 0:
                    self._wait(e, (o, self.gen[o]), self.cnt[o])
            for i in range(self.ndma):
                if self.dmaval[i] > 0:
                    self._wait(e, ('dma', i), self.dmaval[i])

    def finish(self, q='sp'):
        for i in range(self.ndma):
            self._wait(q, ('dma', i), self.dmaval[i])
        for e in self.eng:
            if e != q and self.cnt[e] > 0:
                self._wait(q, (e, self.gen[e]), self.cnt[e])

    def emit(self):
        nc = self.nc
        prog = self.prog

        def replay(e, items):
            for it in items:
                if it[0] == 'wait':
                    e.wait_ge(it[1], it[2])
                elif it[0] == 'op':
                    it[1](e).then_inc(it[2], 1)
                else:
                    e.dma_start(out=it[1], in_=it[2]).then_inc(it[3], 16)
        with nc.Block() as block:
            @block.tensor
            def _(e):
                replay(e, prog['pe'])

            @block.scalar
            def _(e):
                replay(e, prog['act'])

            @block.vector
            def _(e):
                replay(e, prog['dve'])

            @block.gpsimd
            def _(e):
                replay(e, prog['pool'])

            @block.sync
            def _(e):
                replay(e, prog['sp'])


_ALIAS = {}


def _nm(*aps):
    out = []
    for a in aps:
        if hasattr(a, 'tensor'):
            n = a.tensor.name
            n = _ALIAS.get(n, n)
            if n not in out:
                out.append(n)
    return out


class K:
    SB_BASE, SB_LIMIT = 16512, 229344

    def __init__(self, nc, S, stack):
        self.nc, self.S, self.stack = nc, S, stack
        self.rr = 0
        self.top = self.SB_BASE
        self.marks = []
        self.uid = 0

    def sb(self, name, shape, dt=F32):
        isz = 2 if dt == BF16 else 4
        size = isz
        for s_ in shape[1:]:
            size *= s_
        size = (size + 63) // 64 * 64
        off = self.top
        self.top += size
        assert self.top <= self.SB_LIMIT, f"SBUF arena overflow allocating {name} {shape}: top={self.top}"
        self.uid += 1
        return self.nc.alloc_sbuf_tensor_at(f"{name}_u{self.uid}", list(shape), dt, offset=off)

    def push(self):
        self.marks.append(self.top)

    def pop(self):
        self.top = self.marks.pop()
        self.S.barrier()

    def ps(self, name, shape, dt=F32):
        return self.stack.enter_context(self.nc.psum_tensor(name, list(shape), dt))

    def dram(self, name, shape, dt=F32, kind="Internal"):
        return self.nc.dram_tensor(name, list(shape), dt, kind=kind)

    def ve(self):
        self.rr ^= 1
        return 'dve' if self.rr else 'pool'

    def mm(self, out, lhsT, rhs, start=True, stop=True, skip=False):
        if skip:
            self.S.op('pe', lambda e: e.matmul(out, lhsT, rhs, start=start, stop=stop, skip_group_check=True),
                      _nm(lhsT, rhs), _nm(out))
        else:
            self.S.op('pe', lambda e: e.matmul(out, lhsT, rhs, start=start, stop=stop), _nm(lhsT, rhs), _nm(out))

    def tr(self, out, in_, ident):
        self.S.op('pe', lambda e: e.transpose(out, in_, ident), _nm(in_, ident), _nm(out))

    def act(self, out, in_, func, bias=None, scale=None, accum_out=None):
        kw = {}
        if bias is not None:
            kw['bias'] = bias
        if scale is not None:
            kw['scale'] = scale
        if accum_out is not None:
            kw['accum_out'] = accum_out
        self.S.op('act', lambda e: e.activation(out, in_, func, **kw), _nm(in_, bias, scale), _nm(out, accum_out))

    def tt(self, eng, out, in0, in1, op):
        self.S.op(eng, lambda e: e.tensor_tensor(out, in0, in1, op), _nm(in0, in1), _nm(out))

    def ts(self, eng, out, in0, s1, op0, s2=None, op1=None, accum_out=None):
        kw = {}
        if accum_out is not None:
            kw['accum_out'] = accum_out
        if op1 is None:
            self.S.op(eng, lambda e: e.tensor_scalar(out, in0, s1, None, op0, **kw), _nm(in0, s1), _nm(out, accum_out))
        else:
            self.S.op(eng, lambda e: e.tensor_scalar(out, in0, s1, s2, op0, op1, **kw), _nm(in0, s1, s2), _nm(out, accum_out))

    def stt(self, eng, out, in0, scalar, in1, op0, op1, accum_out=None):
        eng = 'dve'
        kw = {}
        if accum_out is not None:
            kw['accum_out'] = accum_out
        self.S.op(eng, lambda e: e.scalar_tensor_tensor(out, in0, scalar, in1, op0, op1, **kw),
                  _nm(in0, scalar, in1), _nm(out, accum_out))

    def copy(self, eng, out, in_):
        if eng == 'act':
            self.S.op('act', lambda e: e.copy(out, in_), _nm(in_), _nm(out))
        else:
            self.S.op(eng, lambda e: e.tensor_copy(out, in_), _nm(in_), _nm(out))

    def recip(self, out, in_):
        self.S.op('dve', lambda e: e.reciprocal(out, in_), _nm(in_), _nm(out))

    def max8(self, out, in_):
        self.S.op('dve', lambda e: e.max(out, in_), _nm(in_), _nm(out))

    def match_replace(self, out, in_to_replace, in_values, imm):
        self.S.op('dve', lambda e: e.match_replace(out, in_to_replace, in_values, imm), _nm(in_to_replace, in_values), _nm(out))

    def scan(self, eng, out, d0, d1, init, op0, op1):
        eng = 'dve'
        self.S.op(eng, lambda e: e.tensor_tensor_scan(out, d0, d1, init, op0, op1), _nm(d0, d1), _nm(out))

    def reduce(self, eng, out, in_, axis, op):
        self.S.op(eng, lambda e: e.tensor_reduce(out, in_, axis, op), _nm(in_), _nm(out))

    def memset(self, eng, out, val):
        self.S.op(eng, lambda e: e.memset(out, val), [], _nm(out))

    def dma(self, out, in_, q='sp'):
        self.S.dma(q, out, in_)


VEC = {}
_off = 0
for _n, _w in [('norm1_g', 8), ('norm2_g', 8), ('k_k', 8), ('k_a', 8), ('omka', 8), ('r_k', 8), ('lnx_g', 8), ('lnx_b', 8),
               ('cnorm_g', 8), ('cnorm_b', 8), ('gate_b', 16), ('w0', 16), ('a0', 16), ('final_g', 8),
               ('mu0', 28), ('mu1', 28), ('muc', 28), ('ada_b', 48), ('conv_w', 8 * CONV_K)]:
    VEC[_n] = (_off, _w)
    _off += _w
NV = _off


def token_blocks(CTX, SEQ, bs=512):
    blocks = []
    t = 0
    while t < CTX:
        n = min(bs, CTX - t)
        blocks.append((t, n, 0))
        t += n
    while t < CTX + SEQ:
        n = min(bs, CTX + SEQ - t)
        blocks.append((t, n, 1))
        t += n
    return blocks


def build(cfg):
    CTX, SEQ, NL = cfg['CTX'], cfg['SEQ'], cfg['NL']
    stop_after = cfg.get('stop_after', None)
    T = CTX + SEQ
    nc = bass.Bass("TRN2", target_bir_lowering=False)
    stack = ExitStack()
    with stack:
        S = Sched(nc, stack)
        k = K(nc, S, stack)
        stack.enter_context(nc.allow_low_precision("bf16 matmuls with fp32 accumulation (tolerance allows)"))
        inp = lambda name, shape: nc.dram_tensor(name, list(shape), F32, kind="ExternalInput").ap()
        xT = inp("xT", [D, T])
        cc = inp("cc", [128, NCH, 2])
        ada_w = inp("ada_w", [NL, 48, 128, NCH, 128])
        vec = inp("vec", [NL, 128, NV])
        w_in = inp("w_in", [NL, NFC, 128, NCH, 128])
        w2 = inp("w2", [NL, 128, D])
        a2 = inp("a2", [NL, 128, D])
        g2 = inp("g2", [NL, 2, 128, D])
        w_oA = inp("w_oA", [NL, 128, NCH, D])
        w_oB = inp("w_oB", [NL, 128, NCH, D])
        w_out = inp("w_out", [NL, 128, NCH, D])
        w_q = inp("w_q", [NL, 128, NCH, 2 * D])
        skT = inp("skT", [NL, 16, 128, 128])
        puT = inp("puT", [NL, 32, 128, NCH, 512])
        pv = inp("pv", [NL, 32, 128, 4, D])
        consts = inp("consts", [128, 10, 128])
        outT = nc.dram_tensor("outT", [D, SEQ], F32, kind="ExternalOutput").ap()
        dbg = {}

        def scratch(name, shape, dt=F32):
            kind = "ExternalOutput" if cfg.get('debug') else "Internal"
            t_ = nc.dram_tensor(name, list(shape), dt, kind=kind).ap()
            dbg[name] = t_
            return t_
        xres = scratch("xres", [D, T])
        zT = scratch("zT", [P_IN_PAD, T])
        y0T = scratch("y0T", [D, T])
        bv0T = scratch("bv0T", [D, T])
        ogT = scratch("ogT", [D, T])
        cvT = scratch("cvT", [D, T])
        puTb = scratch("puTb", [32, 128, NCH, 512], BF16)
        pvb = scratch("pvb", [32, 128, 4, D], BF16)

        cst = k.sb("cst", [128, 10, 128])
        k.dma(cst[:], consts)
        ident = cst[:, 0, :]
        onesblk = cst[:, 1, :]
        onesD = cst[:, 2, :]
        identblk = cst[:, 7, 0:64]
        ones64 = cst[:, 8, :]
        rst = k.sb("rst", [128, 512])
        k.memset('dve', rst[:], 1.0)
        k.memset('dve', rst[:].rearrange("p (c j) -> p c j", j=CH)[:, :, 0:1], 0.0)
        mA = k.sb("mA", [128, 2, 2, 128])
        mB = k.sb("mB", [128, 2, 2, 128])
        mT = k.sb("mT", [128, 2, 128])
        for d in range(2):
            k.copy('dve', mA[:, d, 0, :], cst[:, 3 + 2 * d, :])
            k.ts('dve', mA[:, d, 1, :], cst[:, 4 + 2 * d, :], -1.0, ALU.mult)
            k.copy('dve', mB[:, d, 0, :], cst[:, 3 + 2 * d, :])
            k.copy('dve', mB[:, d, 1, :], cst[:, 4 + 2 * d, :])
            k.copy('dve', mT[:, d, :], cst[:, 5 - 2 * d, :])

        pb = [k.ps(f"pb{i}", [128, 512]) for i in range(8)]
        ccs = k.sb("ccs", [128, NCH, 2])
        k.dma(ccs[:], cc)
        k.act(ccs[:], ccs[:], AF.Silu)
        vecs = k.sb("vecs", [128, NV])
        modT = k.sb("modT", [128, 48, 2])

        def V(name, j=0, w=1):
            o, _ = VEC[name]
            return vecs[:, o + j:o + j + w]

        blocks = token_blocks(CTX, SEQ)
        sqs = k.sb("sqs", [128, 512])
        rstd = k.sb("rstd", [128, 512])
        xn = k.sb("xn", [128, 512])
        epsn = k.sb("epsn", [128, 1])
        k.memset('dve', epsn[:], NORM_EPS)
        gm = k.sb("gm", [128, 2, NCH, 2])

        for l in range(NL):
            last = (l == NL - 1)
            src = xT if l == 0 else xres
            k.dma(vecs[:], vec[l])
            if True:
                k.push()
                aw = [k.sb(f"aw{l}_{i}", [128, NCH, 128]) for i in range(2)]
                for j in range(48):
                    a_ = aw[j % 2]
                    k.dma(a_[:], ada_w[l, j])
                    pm = pb[j % 2]
                    for dh in range(NCH):
                        k.mm(pm[:, 0:2], a_[:, dh, :], ccs[:, dh, :], start=(dh == 0), stop=(dh == NCH - 1))
                    k.ts('dve', modT[:, j, :], pm[:, 0:2], V('ada_b', j), ALU.add)
                for w_, (nn, sci) in enumerate((('norm1_g', 1), ('norm2_g', 4))):
                    for c in range(NCH):
                        k.ts('dve', gm[:, w_, c, :], modT[:, sci * 8 + c, :], 1.0, ALU.add, V(nn, c), ALU.mult)
                k.pop()

            def modulate_block(dst, xb, n, which, seg, tagp):
                pss = pb[7]
                for c in range(NCH):
                    k.act(sqs[:, :n], xb[:, c, :n], AF.Square)
                    k.mm(pss[:, :n], onesD, sqs[:, :n], start=(c == 0), stop=(c == NCH - 1))
                k.act(rstd[:, :n], pss[:, :n], AF.Sqrt, bias=epsn[:, 0:1])
                k.recip(rstd[:, :n], rstd[:, :n])
                shi = 0 if which == 0 else 3
                for c in range(NCH):
                    e_ = k.ve()
                    k.tt(e_, xn[:, :n], xb[:, c, :n], rstd[:, :n], ALU.mult)
                    k.ts(e_, dst[:, c, :n], xn[:, :n], gm[:, which, c, seg:seg + 1], ALU.mult,
                         modT[:, shi * 8 + c, seg:seg + 1], ALU.add)


            if True:
                k.push()
                HT = k.sb(f"HT{l}", [128, NCH, T], BF16)
                xb = [k.sb(f"xb{l}_{i}", [128, NCH, 512]) for i in range(2)]
                for bi, (t0, n, seg) in enumerate(blocks):
                    xb_ = xb[bi % 2]
                    k.dma(xb_[:, :, :n], src[:, t0:t0 + n].rearrange("(c p) t -> p c t", p=128))
                    if l == 0:
                        k.dma(xres[:, t0:t0 + n].rearrange("(c p) t -> p c t", p=128), xb_[:, :, :n], q='act')
                    modulate_block(HT[:, :, t0:t0 + n], xb_, n, 0, 1 - seg, f"A{l}")
                wf = [k.sb(f"wf{l}_{i}", [128, NCH, 128]) for i in range(2)]
                wb = [k.sb(f"wb{l}_{i}", [128, NCH, 128], BF16) for i in range(2)]
                zst = [k.sb(f"zst{l}_{i}", [128, 512]) for i in range(4)]
                cnt = 0
                nfc = 28 if (last and False) else NFC
                for fc in range(nfc):
                    wf_, wb_ = wf[fc % 2], wb[fc % 2]
                    k.dma(wf_[:], w_in[l, fc])
                    k.copy('pool', wb_[:], wf_[:])
                    for (t0, n, seg) in blocks:
                        if last and seg == 0 and fc >= 28:
                            continue
                        pz = pb[cnt % 4]
                        for dh in range(NCH):
                            k.mm(pz[:, :n], wb_[:, dh, :], HT[:, dh, t0:t0 + n], start=(dh == 0), stop=(dh == NCH - 1))
                        z_ = zst[cnt % 4]
                        k.copy('act' if cnt % 2 else 'dve', z_[:, :n], pz[:, :n])
                        k.dma(zT[fc * 128:(fc + 1) * 128, t0:t0 + n], z_[:, :n], q='act')
                        cnt += 1
                k.pop()
            if stop_after == 'B':
                break
            k.push()
            k.tt('dve', V('muc', 0, 28), V('mu0', 0, 28), V('mu1', 0, 28), ALU.add)
            k.ts('dve', V('muc', 0, 28), V('muc', 0, 28), -1.0, ALU.mult, 1.0, ALU.add)
            k.ts('dve', V('omka', 0, 8), V('k_a', 0, 8), -1.0, ALU.mult, 1.0, ALU.add)
            TW = T + 4
            zw = [k.sb(f"zw{l}_{i}", [128, TW]) for i in range(2)]
            zo = [k.sb(f"zo{l}_{i}", [128, TW]) for i in range(2)]
            for i in range(2):
                k.memset('pool', zw[i][:, 0:1], 0.0)
                k.memset('pool', zw[i][:, CTX + 1:CTX + 3], 0.0)
                k.memset('pool', zw[i][:, TW - 1:TW], 0.0)

            def b2_load(fc):
                rows = slice(fc * 128, (fc + 1) * 128)
                k.dma(zw[fc % 2][:, 1:1 + CTX], zT[rows, 0:CTX])
                k.dma(zw[fc % 2][:, CTX + 3:CTX + 3 + SEQ], zT[rows, CTX:T])
            b2_load(0)
            for fc in range(28):
                rows = slice(fc * 128, (fc + 1) * 128)
                if fc + 1 < 28:
                    b2_load(fc + 1)
                zw_, zo_ = zw[fc % 2], zo[fc % 2]
                k.ts('dve', zo_[:, 1:TW - 1], zw_[:, 1:TW - 1], V('muc', fc), ALU.mult)
                k.stt('pool', zo_[:, 1:TW - 1], zw_[:, 0:TW - 2], V('mu0', fc), zo_[:, 1:TW - 1], ALU.mult, ALU.add)
                k.stt('dve', zo_[:, 1:TW - 1], zw_[:, 2:TW], V('mu1', fc), zo_[:, 1:TW - 1], ALU.mult, ALU.add)
                if fc == 24:
                    k.act(zo_[:, 1:TW - 1], zo_[:, 1:TW - 1], AF.Tanh)
                if fc in (26, 27):
                    k.act(zo_[:, 1:TW - 1], zo_[:, 1:TW - 1], AF.Sigmoid)
                k.dma(zT[rows, 0:CTX], zo_[:, 1:1 + CTX], q='act')
                k.dma(zT[rows, CTX:T], zo_[:, CTX + 3:CTX + 3 + SEQ], q='act')
            k.pop()
            if stop_after == 'B2':
                break

            k.push()
            w2s = k.sb(f"w2s{l}", [128, D])
            a2s = k.sb(f"a2s{l}", [128, D])
            g2s = k.sb(f"g2s{l}", [128, 2, D])
            k.dma(w2s[:], w2[l])
            k.dma(a2s[:], a2[l])
            k.dma(g2s[:], g2[l].rearrange("c p f -> p c f"))
            RDT = F32R if RWKV_F32R else F32
            Sst = [k.sb(f"Sst{l}_{g}", [128, HD], RDT) for g in range(8)]
            gneps = k.sb(f"gneps{l}", [128, 1])
            k.memset('dve', gneps[:], GN_EPS)
            NS = 2
            hmask = [onesblk[:, 0:1], onesblk[:, 64:65]]

            class Obj:
                pass
            Ps, Us = [], []
            for s in range(NS):
                P = Obj()
                for nm_ in ('rr', 'kx', 'vv', 'kk', 'aa', 'kd', 'bb', 'lw', 'lam', 'pex', 'rem', 'Lb', 'BQ', 'nBQP', 'KQP',
                            'BQ0', 'BQ1', 'KQ0', 'KQ1', 'KP0', 'KP1',
                            't1', 't2', 'e1', 'e3', 'Yb', 'bv'):
                    setattr(P, nm_, k.sb(f"P{l}_{s}_{nm_}", [128, 512],
                                         RDT if nm_ in ('BQ', 'BQ0', 'BQ1', 'KQ0', 'KQ1', 'KP0', 'KP1') else F32))
                P.e2, P.e4, P.Lb = P.aa, P.t1, P.kx
                P.KR = k.sb(f"P{l}_{s}_KR", [128, 2, 512], RDT)
                P.PC = k.sb(f"P{l}_{s}_PC", [128, 8])
                Ps.append(P)
                U = Obj()
                U.TM = k.sb(f"U{l}_{s}_TM", [128, 4, 128], RDT)
                U.ZA = k.sb(f"U{l}_{s}_ZA", [128, 2, 2, 128], RDT)
                U.ZB = k.sb(f"U{l}_{s}_ZB", [128, 2, 2, 128], RDT)
                U.AA = k.sb(f"U{l}_{s}_AA", [128, 2, 128], RDT)
                U.ZZ = [k.sb(f"U{l}_{s}_ZZ{j}", [128, 2, 2, 128], RDT) for j in range(5)]
                U.X = [k.sb(f"U{l}_{s}_X{j}", [128, 2, 128], RDT) for j in range(2)]
                U.MT = k.sb(f"U{l}_{s}_MT", [128, 2, 64], RDT)
                U.NN = k.sb(f"U{l}_{s}_NN", [128, 2, 64], RDT)
                U.RPp = k.sb(f"U{l}_{s}_RPp", [128, 128], RDT)
                U.banks = [pb[2 + 3 * s], pb[3 + 3 * s]]
                U.ybank = pb[4 + 3 * s]
                U.bi = 0
                Us.append(U)
            lor = [[k.sb(f"lor{l}_{i}_{j}", [128, 512]) for j in range(4)] for i in range(1)]
            prep_cnt = [0]

            class KR:
                def __getattr__(self, a):
                    return getattr(k, a)

                def mm(self, out, lhsT, rhs, start=True, stop=True, skip=False):
                    if out.base_partition() != 0 or out.shape[0] != 128:
                        lhsT, rhs = lhsT.bitcast(F32), rhs.bitcast(F32)
                    k.mm(out, lhsT, rhs, start=start, stop=stop, skip=skip)
            kr = KR()

            def prep_bank():
                prep_cnt[0] += 1
                return pb[prep_cnt[0] % 2]

            def ubank(U):
                U.bi += 1
                return U.banks[U.bi % 2]

            def prep(P, d, t0, n, g, tw, awt):
                k.dma(P.rr[:, :n], zT[g * 128:(g + 1) * 128, t0:t0 + n])
                k.dma(P.kx[:, :n], zT[(8 + g) * 128:(9 + g) * 128, t0:t0 + n])
                k.dma(P.vv[:, :n], zT[(16 + g) * 128:(17 + g) * 128, t0:t0 + n])
                k.ts('pool', P.t1[:, :n], P.kx[:, :n], V('k_k', g), ALU.mult)
                k.tt('pool', P.t2[:, :n], P.t1[:, :n], P.t1[:, :n], ALU.mult)
                pq = prep_bank()
                k.mm(pq[:, :n], onesblk, P.t2[:, :n])
                k.act(P.t2[:, :n], pq[:, :n], AF.Sqrt)
                k.ts('dve', P.t2[:, :n], P.t2[:, :n], 1e-12, ALU.max)
                k.recip(P.t2[:, :n], P.t2[:, :n])
                k.tt('pool', P.kk[:, :n], P.t1[:, :n], P.t2[:, :n], ALU.mult)
                ds = slice(d * 64, (d + 1) * 64)
                gs = slice(g * 128, (g + 1) * 128)
                pq = prep_bank()
                k.mm(pq[:, :n], w2s[ds, gs], tw[ds, :n])
                k.act(P.lw[:, :n], pq[:, :n], AF.Sigmoid, bias=V('w0', d * 8 + g))
                k.ts('pool', P.lw[:, :n], P.lw[:, :n], -EXPM05, ALU.mult)
                pq = prep_bank()
                k.mm(pq[:, :n], a2s[ds, gs], awt[ds, :n])
                k.act(P.aa[:, :n], pq[:, :n], AF.Sigmoid, bias=V('a0', d * 8 + g))
                k.ts('pool', P.t1[:, :n], P.aa[:, :n], V('k_a', g), ALU.mult, V('omka', g), ALU.add)
                k.tt('pool', P.kd[:, :n], P.t1[:, :n], P.kx[:, :n], ALU.mult)
                k.tt('pool', P.bb[:, :n], P.aa[:, :n], P.kk[:, :n], ALU.mult)
                k.tt('pool', P.t1[:, :n], P.rr[:, :n], P.kd[:, :n], ALU.mult)
                k.ts('pool', P.t1[:, :n], P.t1[:, :n], V('r_k', g), ALU.mult)
                pq = prep_bank()
                k.mm(pq[:, :n], onesblk, P.t1[:, :n])
                k.tt('dve', P.bv[:, :n], pq[:, :n], P.vv[:, :n], ALU.mult)
                nchk = n // CH
                k.scan('dve', P.lam[:, :n], rst[:, :n], P.lw[:, :n], 0.0, ALU.mult, ALU.add)
                lam3 = P.lam[:, :n].rearrange("p (c j) -> p c j", j=CH)
                tot_b = lam3[:, :, CH - 1:CH].to_broadcast([128, nchk, CH])
                k.tt('pool', P.pex[:, :n], P.lam[:, :n], P.lw[:, :n], ALU.subtract)
                k.tt('pool', P.rem[:, :n].rearrange("p (c j) -> p c j", j=CH), tot_b, lam3, ALU.subtract)
                if d == 0:
                    L, Lex, Lrem = P.lam, P.pex, P.rem
                else:
                    k.tt('pool', P.Lb[:, :n], P.rem[:, :n], P.lw[:, :n], ALU.add)
                    L, Lex, Lrem = P.Lb, P.rem, P.pex
                k.act(P.e1[:, :n], Lex[:, :n], AF.Exp)
                k.tt('pool', P.KR[:, 0, :n], P.kk[:, :n], P.e1[:, :n], ALU.mult)
                k.stt('dve', P.KP0[:, :n], P.kk[:, :n], hmask[0], P.e1[:, :n], ALU.mult, ALU.mult)
                k.stt('dve', P.KP1[:, :n], P.kk[:, :n], hmask[1], P.e1[:, :n], ALU.mult, ALU.mult)
                k.act(P.e2[:, :n], L[:, :n], AF.Exp)
                k.tt('pool', P.KR[:, 1, :n], P.rr[:, :n], P.e2[:, :n], ALU.mult)
                k.act(P.e3[:, :n], L[:, :n], AF.Exp, scale=-1.0)
                k.tt('pool', P.BQ[:, :n], P.bb[:, :n], P.e3[:, :n], ALU.mult)
                k.stt('dve', P.BQ0[:, :n], P.bb[:, :n], hmask[0], P.e3[:, :n], ALU.mult, ALU.mult)
                k.stt('dve', P.BQ1[:, :n], P.bb[:, :n], hmask[1], P.e3[:, :n], ALU.mult, ALU.mult)
                k.stt('dve', P.KQ0[:, :n], P.kd[:, :n], hmask[0], P.e3[:, :n], ALU.mult, ALU.mult)
                k.stt('dve', P.KQ1[:, :n], P.kd[:, :n], hmask[1], P.e3[:, :n], ALU.mult, ALU.mult)
                k.act(P.e4[:, :n], Lrem[:, :n], AF.Exp)
                k.stt('dve', P.nBQP[:, :n], P.bb[:, :n], -1.0, P.e4[:, :n], ALU.mult, ALU.mult)
                k.tt('pool', P.KQP[:, :n], P.kd[:, :n], P.e4[:, :n], ALU.mult)
                k.act(P.PC[:, :nchk], lam3[:, :, CH - 1], AF.Exp)

            def unit(P, U, d, g, ti):
                k = kr
                sl = slice(ti * 128, (ti + 1) * 128)
                H = [slice(0, 64), slice(64, 128)]
                pT = ubank(U)
                pT4 = pT[:, :].rearrange("p (a t) -> p a t", a=4)
                k.tr(pT4[:, 0, :], P.vv[:, sl], ident)
                k.tr(pT4[:, 1, :], P.KR[:, 0, sl].bitcast(F32), ident)
                k.tr(pT4[:, 2, :], P.nBQP[:, sl], ident)
                k.tr(pT4[:, 3, :], P.KQP[:, sl], ident)
                k.copy('act', U.TM[:], pT4)
                yield
                pA = ubank(U)
                pA4 = pA[:, :].rearrange("p (h w t) -> p h w t", h=2, w=2)
                BQh, KQh, KPh = [P.BQ0, P.BQ1], [P.KQ0, P.KQ1], [P.KP0, P.KP1]
                for hh in range(2):
                    k.mm(pA4[:, hh], BQh[hh][:, sl], P.KR[:, :, sl])
                k.tt('dve', U.ZA[:], pA4, mA[:, d].unsqueeze(1).to_broadcast([128, 2, 2, 128]), ALU.mult)
                pB = ubank(U)
                pB4 = pB[:, :].rearrange("p (h w t) -> p h w t", h=2, w=2)
                for hh in range(2):
                    k.mm(pB4[:, hh], KQh[hh][:, sl], P.KR[:, :, sl])
                k.tt('dve', U.ZB[:], pB4, mB[:, d].unsqueeze(1).to_broadcast([128, 2, 2, 128]), ALU.mult)
                pC = ubank(U)
                pC3 = pC[:, 0:256].rearrange("p (h t) -> p h t", h=2)
                for hh in range(2):
                    k.mm(pC3[:, hh], KPh[hh][:, sl], P.BQ[:, sl])
                k.tt('dve', U.AA[:], pC3, mT[:, d].unsqueeze(1).to_broadcast([128, 2, 128]), ALU.mult)
                yield
                Zc = [U.ZA[:, 0, 0, :], U.ZA[:, 1, 0, :]]
                Ac = [U.AA[:, 0, :], U.AA[:, 1, :]]
                Zp = [Zc]
                for lev in range(5):
                    pQ = ubank(U)
                    pQ4 = pQ[:, :].rearrange("p (w h t) -> p w h t", w=2, h=2)
                    for hh in range(2):
                        k.mm(pQ4[:, 0, hh], Ac[hh], Zc[hh])
                        if lev < 4:
                            k.mm(pQ4[:, 1, hh], Zc[hh], Ac[hh])
                    ZZl = U.ZZ[lev]
                    if lev < 4:
                        k.copy('act' if lev % 2 else 'dve', ZZl[:], pQ4)
                    else:
                        k.copy('dve', ZZl[:, 0], pQ4[:, 0])
                    Zc = [ZZl[:, 0, 0, :], ZZl[:, 0, 1, :]]
                    Ac = [ZZl[:, 1, 0, :], ZZl[:, 1, 1, :]]
                    Zp.append(Zc)
                    yield
                pX = ubank(U)
                pX3 = pX[:, 0:256].rearrange("p (h c) -> p h c", h=2)
                for hh in range(2):
                    k.mm(pX3[:, hh, 0:64], U.ZB[:, hh, 0, :], U.TM[:, 0, H[hh]])
                X = U.X[0]
                k.copy('act', X[:, :, 0:64], pX3[:, :, 0:64])
                k.copy('pool', X[:, :, 64:128], U.TM[:, 1, :].rearrange("p (h c) -> p h c", h=2))
                yield
                for lev in range(6):
                    pP = ubank(U)
                    pP3 = pP[:, 0:256].rearrange("p (h c) -> p h c", h=2)
                    for hh in range(2):
                        k.mm(pP3[:, hh, :], Zp[lev][hh], X[:, hh, :])
                    Xn = U.X[(lev + 1) % 2]
                    k.tt('dve', Xn[:], X[:], pP3, ALU.subtract if lev == 0 else ALU.add)
                    X = Xn
                    yield
                pMNs = [ubank(U), ubank(U)]
                for c2 in range(2):
                    cs = H[c2]
                    pMN = pMNs[c2]
                    for hh in range(2):
                        k.mm(pMN[H[hh], 0:64], X[cs, hh, 64:128], U.TM[cs, 2, H[hh]])
                        k.mm(pMN[H[hh], 64:128], U.TM[cs, 3, H[hh]], U.TM[cs, 0, H[hh]], start=True, stop=False)
                        k.mm(pMN[H[hh], 64:128], U.TM[cs, 2, H[hh]], X[cs, hh, 0:64], start=False, stop=True)
                for c2 in range(2):
                    k.stt('dve', U.MT[:, c2, :], identblk, P.PC[:, ti * 2 + c2:ti * 2 + c2 + 1],
                          pMNs[c2][:, 0:64], ALU.mult, ALU.add)
                    k.copy('act', U.NN[:, c2, :], pMNs[c2][:, 64:128])
                pR = ubank(U)
                for hh in range(2):
                    k.mm(pR[H[hh], 0:128], X[:, hh, 64:128], U.ZA[:, hh, 1, :])
                k.tt('dve', U.RPp[:], P.KR[:, 1, sl], pR[:, 0:128], ALU.add)
                pY = U.ybank
                for hh in range(2):
                    k.mm(pY[H[hh], 0:128], U.TM[:, 0, H[hh]], U.ZB[:, hh, 1, :], start=True, stop=False, skip=True)
                    k.mm(pY[H[hh], 0:128], X[:, hh, 0:64], U.ZA[:, hh, 1, :], start=False, stop=False, skip=True)
                yield
                for c2 in ((0, 1) if d == 0 else (1, 0)):
                    cs = H[c2]
                    for hh in range(2):
                        k.mm(pY[H[hh], cs], Sst[g][H[hh], :], U.RPp[H[hh], cs], start=False, stop=True, skip=True)
                    pS = ubank(U)
                    for hh in range(2):
                        k.mm(pS[H[hh], 0:64], U.MT[H[hh], c2, :], Sst[g][H[hh], :])
                    k.tt('dve', Sst[g][:], pS[:, 0:64], U.NN[:, c2, :], ALU.add)
                    yield
                k.copy('act', P.Yb[:, sl], pY[:, 0:128])
                yield

            def block_gen(P, U, d, g, n):
                tiles = list(range(n // 128))
                if d == 1:
                    tiles = tiles[::-1]
                for ti in tiles:
                    yield from unit(P, U, d, g, ti)

            def finalize(P, d, t0, n, g, sga, sgb):
                rows = slice(g * 128, (g + 1) * 128)
                if d == 0:
                    k.dma(y0T[rows, t0:t0 + n], P.Yb[:, :n], q='act')
                    k.dma(bv0T[rows, t0:t0 + n], P.bv[:, :n], q='act')
                    return
                k.dma(P.t1[:, :n], y0T[rows, t0:t0 + n])
                k.dma(P.t2[:, :n], bv0T[rows, t0:t0 + n])
                k.tt('pool', P.Yb[:, :n], P.Yb[:, :n], P.t1[:, :n], ALU.add)
                k.tt('pool', P.bv[:, :n], P.bv[:, :n], P.t2[:, :n], ALU.add)
                pq = prep_bank()
                k.mm(pq[:, :n], ones64, P.Yb[:, :n])
                k.copy('act', P.e1[:, :n], pq[:, :n])
                k.tt('pool', P.kk[:, :n], P.Yb[:, :n], P.Yb[:, :n], ALU.mult)
                pq2 = prep_bank()
                k.mm(pq2[:, :n], ones64, P.kk[:, :n])
                k.tt('pool', P.e3[:, :n], P.e1[:, :n], P.e1[:, :n], ALU.mult)
                k.tt('dve', P.e3[:, :n], pq2[:, :n], P.e3[:, :n], ALU.subtract)
                k.act(P.e3[:, :n], P.e3[:, :n], AF.Sqrt, bias=gneps[:, 0:1])
                k.recip(P.e3[:, :n], P.e3[:, :n])
                k.tt('pool', P.kd[:, :n], P.Yb[:, :n], P.e1[:, :n], ALU.subtract)
                k.tt('pool', P.kd[:, :n], P.kd[:, :n], P.e3[:, :n], ALU.mult)
                k.ts('pool', P.kd[:, :n], P.kd[:, :n], V('lnx_g', g), ALU.mult, V('lnx_b', g), ALU.add)
                k.tt('pool', P.kd[:, :n], P.kd[:, :n], P.bv[:, :n], ALU.add)
                pq3 = prep_bank()
                k.mm(pq3[:, :n], g2s[:, 0, rows], sga[:, :n], start=True, stop=False)
                k.mm(pq3[:, :n], g2s[0:32, 1, rows], sgb[0:32, :n], start=False, stop=True)
                k.tt('dve', P.e1[:, :n], P.kd[:, :n], pq3[:, :n], ALU.mult)
                k.dma(ogT[rows, t0:t0 + n], P.e1[:, :n], q='act')

            ctxb = [b_ for b_ in blocks if b_[2] == 0]
            latb = [b_ for b_ in blocks if b_[2] == 1]
            for d in range(2):
                for g in range(8):
                    k.memset('pool', Sst[g][:].bitcast(F32), 0.0)
                order = (ctxb + latb) if d == 0 else (ctxb[::-1] + latb[::-1])
                for bi, (t0, n, seg) in enumerate(order):
                    tw, awt, sga, sgb = lor[0]
                    k.dma(tw[:, :n], zT[24 * 128:25 * 128, t0:t0 + n])
                    k.dma(awt[:, :n], zT[25 * 128:26 * 128, t0:t0 + n])
                    if d == 1:
                        k.dma(sga[:, :n], zT[26 * 128:27 * 128, t0:t0 + n])
                        k.dma(sgb[:, :n], zT[27 * 128:28 * 128, t0:t0 + n])
                    for g0 in range(0, 8, NS):
                        gens = []
                        for s in range(NS):
                            prep(Ps[s], d, t0, n, g0 + s, tw, awt)
                            gens.append(block_gen(Ps[s], Us[s], d, g0 + s, n))
                        alive = list(range(NS))
                        while alive:
                            for s in list(alive):
                                try:
                                    next(gens[s])
                                except StopIteration:
                                    alive.remove(s)
                        for s in range(NS):
                            finalize(Ps[s], d, t0, n, g0 + s, sga, sgb)
            k.pop()
            if stop_after == 'C':
                break
            act_blocks = [b_ for b_ in blocks if not (last and b_[2] == 0)]
            k.push()
            cza = k.sb(f"cza{l}", [128, T])
            czb = k.sb(f"czb{l}", [128, T])
            cacc = [k.sb(f"cacc{l}_{i}", [128, T]) for i in range(2)]
            tlo = CTX if last else 0
            for c in range(NCH):
                k.dma(cza[:, tlo:T], zT[(28 + c) * 128:(29 + c) * 128, tlo:T])
                k.dma(czb[:, tlo:T], zT[(36 + c) * 128:(37 + c) * 128, tlo:T])
                k.act(czb[:, tlo:T], czb[:, tlo:T], AF.Sigmoid)
                k.tt('pool', cza[:, tlo:T], cza[:, tlo:T], czb[:, tlo:T], ALU.mult)
                segs = [(CTX, SEQ, GRID_W)] + ([] if last else [(0, CTX, 1)])
                for (s0, sl_, stride) in segs:
                    used = [False, False]
                    for kk_ in range(CONV_K):
                        off = (kk_ - CONV_K // 2) * stride
                        lo, hi = max(0, -off), min(sl_, sl_ - off)
                        if hi <= lo:
                            continue
                        ai = 0 if kk_ == CONV_K // 2 else 1 + 0 * kk_
                        ai = kk_ % 2 if kk_ != CONV_K // 2 else 0
                        e_ = 'dve' if ai == 0 else 'pool'
                        wcol = V('conv_w', c * CONV_K + kk_)
                        if kk_ == CONV_K // 2:
                            pass
                        dst = cacc[ai][:, s0 + lo:s0 + hi]
                        srcu = cza[:, s0 + lo + off:s0 + hi + off]
                        if not used[ai]:
                            k.memset(e_, cacc[ai][:, s0:s0 + sl_], 0.0)
                            used[ai] = True
                        k.stt(e_, dst, srcu, wcol, dst, ALU.mult, ALU.add)
                    if used[1]:
                        k.tt('dve', cacc[0][:, s0:s0 + sl_], cacc[0][:, s0:s0 + sl_], cacc[1][:, s0:s0 + sl_], ALU.add)
                k.dma(cvT[c * 128:(c + 1) * 128, tlo:T], cacc[0][:, tlo:T], q='act')
            k.pop()
            if stop_after == 'D':
                break

            k.push()
            wstage = k.sb(f"wstage{l}", [128, NCH, D])
            wts = {}
            for nme, src_w in (('oA', w_oA), ('oB', w_oB), ('out', w_out)):
                wts[nme] = k.sb(f"w{nme}{l}", [128, NCH, D], BF16)
                k.dma(wstage[:], src_w[l])
                k.copy('pool', wts[nme][:], wstage[:])
            lneps = k.sb(f"lneps{l}", [128, 1])
            k.memset('dve', lneps[:], LN_EPS)
            cvb = k.sb(f"cvb{l}", [128, NCH, 512])
            ogb = k.sb(f"ogb{l}", [128, NCH, 512])
            xbe = k.sb(f"xbe{l}", [128, NCH, 512])
            sB = k.sb(f"sB{l}", [128, NCH, 512], BF16)
            oB = k.sb(f"oB{l}", [128, NCH, 512], BF16)
            mTb = k.sb(f"mTb{l}", [128, NCH, 512], BF16)
            gat = [k.sb(f"gat{l}_{i}", [128, 512]) for i in range(4)]
            tE = [k.sb(f"tE{l}_{i}", [128, 512]) for i in range(4)]
            mean = k.sb(f"mean{l}", [128, 512])
            rsd = k.sb(f"rsd{l}", [128, 512])
            for (t0, n, seg) in act_blocks:
                sg = 1 - seg
                k.dma(cvb[:, :, :n], cvT[:, t0:t0 + n].rearrange("(c p) t -> p c t", p=128))
                k.dma(ogb[:, :, :n], ogT[:, t0:t0 + n].rearrange("(c p) t -> p c t", p=128))
                k.dma(xbe[:, :, :n], xres[:, t0:t0 + n].rearrange("(c p) t -> p c t", p=128))
                pm_, pq_ = pb[0], pb[1]
                for c in range(NCH):
                    k.mm(pm_[:, :n], onesD, cvb[:, c, :n], start=(c == 0), stop=(c == NCH - 1))
                for c in range(NCH):
                    k.act(tE[c % 2][:, :n], cvb[:, c, :n], AF.Square)
                    k.mm(pq_[:, :n], onesD, tE[c % 2][:, :n], start=(c == 0), stop=(c == NCH - 1))
                k.copy('act', mean[:, :n], pm_[:, :n])
                k.tt('pool', rsd[:, :n], mean[:, :n], mean[:, :n], ALU.mult)
                k.tt('dve', rsd[:, :n], pq_[:, :n], rsd[:, :n], ALU.subtract)
                k.act(rsd[:, :n], rsd[:, :n], AF.Sqrt, bias=lneps[:, 0:1])
                k.recip(rsd[:, :n], rsd[:, :n])
                for c in range(NCH):
                    e_ = k.ve()
                    t_ = tE[2 + c % 2]
                    k.tt(e_, t_[:, :n], cvb[:, c, :n], mean[:, :n], ALU.subtract)
                    k.tt(e_, t_[:, :n], t_[:, :n], rsd[:, :n], ALU.mult)
                    k.ts(e_, t_[:, :n], t_[:, :n], V('cnorm_g', c), ALU.mult, V('cnorm_b', c), ALU.add)
                    k.act(sB[:, c, :n], t_[:, :n], AF.Silu)
                    k.copy(e_, oB[:, c, :n], ogb[:, c, :n])
                for oc in range(NCH):
                    ocs = slice(oc * 128, (oc + 1) * 128)
                    ga_, gb_ = gat[(oc % 2) * 2], gat[(oc % 2) * 2 + 1]
                    k.dma(ga_[:, :n], zT[(44 + oc) * 128:(45 + oc) * 128, t0:t0 + n])
                    k.dma(gb_[:, :n], zT[(52 + oc) * 128:(53 + oc) * 128, t0:t0 + n])
                    k.act(ga_[:, :n], ga_[:, :n], AF.Sigmoid, bias=V('gate_b', oc))
                    k.act(gb_[:, :n], gb_[:, :n], AF.Sigmoid, bias=V('gate_b', 8 + oc))
                    pa_, pb_ = pb[2 + (oc % 2) * 2], pb[3 + (oc % 2) * 2]
                    for c in range(NCH):
                        k.mm(pa_[:, :n], wts['oA'][:, c, ocs], oB[:, c, :n], start=(c == 0), stop=(c == NCH - 1))
                    for c in range(NCH):
                        k.mm(pb_[:, :n], wts['oB'][:, c, ocs], sB[:, c, :n], start=(c == 0), stop=(c == NCH - 1))
                    k.tt('dve', ga_[:, :n], pa_[:, :n], ga_[:, :n], ALU.mult)
                    k.tt('dve', gb_[:, :n], pb_[:, :n], gb_[:, :n], ALU.mult)
                    k.tt('pool', mTb[:, oc, :n], ga_[:, :n], gb_[:, :n], ALU.add)
                for oc in range(NCH):
                    ocs = slice(oc * 128, (oc + 1) * 128)
                    po_ = pb[6 + oc % 2]
                    for c in range(NCH):
                        k.mm(po_[:, :n], wts['out'][:, c, ocs], mTb[:, c, :n], start=(c == 0), stop=(c == NCH - 1))
                    k.stt('dve', xbe[:, oc, :n], po_[:, :n], modT[:, 2 * 8 + oc, sg:sg + 1], xbe[:, oc, :n], ALU.mult, ALU.add)
                k.dma(xres[:, t0:t0 + n].rearrange("(c p) t -> p c t", p=128), xbe[:, :, :n], q='act')
            k.pop()
            if stop_after == 'E':
                break
            k.push()
            cf = [k.sb(f"pcf{l}_{i}", [128, 4096]) for i in range(2)]
            cbf = [k.sb(f"pcb{l}_{i}", [128, 4096], BF16) for i in range(2)]
            ci = 0
            for grp in range(32):
                for which in range(2):
                    f_, b_ = cf[ci % 2], cbf[ci % 2]
                    if which == 0:
                        k.dma(f_[:, :].rearrange("p (a b) -> p a b", a=NCH), puT[l, grp])
                    else:
                        k.dma(f_[:, :].rearrange("p (a b) -> p a b", a=4), pv[l, grp])
                    k.copy('pool' if ci % 2 else 'dve', b_[:], f_[:])
                    if which == 0:
                        k.dma(puTb[grp], b_[:, :].rearrange("p (a b) -> p a b", a=NCH), q='act')
                    else:
                        k.dma(pvb[grp], b_[:, :].rearrange("p (a b) -> p a b", a=4), q='act')
                    ci += 1
            k.pop()
            k.push()
            wqs = k.sb(f"wqs{l}", [128, NCH, 2 * D], BF16)
            skTs = k.sb(f"skTs{l}", [128, 16, 128], BF16)
            identb = k.sb(f"identb{l}", [128, 128], BF16)
            k.copy('dve', identb[:], ident)
            k.push()
            wqst = [k.sb(f"wqst{l}_{i}", [128, NCH, 512]) for i in range(2)]
            for j in range(4):
                k.dma(wqst[j % 2][:], w_q[l][:, :, j * 512:(j + 1) * 512])
                k.copy('pool' if j % 2 else 'dve', wqs[:, :, j * 512:(j + 1) * 512], wqst[j % 2][:])
            skst = k.sb(f"skst{l}", [128, 16, 128])
            k.dma(skst[:], skT[l].rearrange("q d j -> d q j"))
            k.copy('dve', skTs[:], skst[:])
            k.pop()
            Gt = [k.sb(f"G{l}_{i}", [128, NEXP], BF16) for i in range(2)]
            xbf = k.sb(f"xbf{l}", [128, NCH, 256])
            h2 = k.sb(f"h2{l}", [128, NCH, 256], BF16)
            qT = k.sb(f"qT{l}", [128, 16, 256], BF16)
            ssb = k.sb(f"ssb{l}", [128, 16, 128])
            qTf = qT[:, :, :].bitcast(F32)
            thn2 = k.sb(f"thn2{l}", [128, 2, 8])
            T16 = k.sb(f"T16{l}", [128, 16, 16])
            tmpk = k.sb(f"tmpk{l}", [128, 128])
            negm = k.sb(f"negm{l}", [128, 16])
            cand = k.sb(f"cand{l}", [128, 256])
            candt = k.sb(f"candt{l}", [128, 256])
            c16 = k.sb(f"c16{l}", [128, 8, 16])
            w16 = k.sb(f"w16{l}", [128, 8, 16])
            smx = k.sb(f"smx{l}", [128, 8])
            Zs = k.sb(f"Zs{l}", [128, 8])
            rZ = k.sb(f"rZ{l}", [128, 8])
            thn = k.sb(f"thn{l}", [128, 8])
            Pq = [k.sb(f"Pq{l}_{i}", [128, 8, 128]) for i in range(2)]
            Gh = [k.sb(f"Gh{l}_{i}", [128, 8, 128], BF16) for i in range(2)]
            UTs = [k.sb(f"UTs{l}_{i}", [128, NCH, 512], BF16) for i in range(2)]
            Vs = [k.sb(f"Vs{l}_{i}", [128, 4, D], BF16) for i in range(2)]
            gab = [k.sb(f"gab{l}_{i}", [128, 256]) for i in range(2)]
            WT = [k.sb(f"WT{l}_{i}", [128, 256], BF16) for i in range(2)]
            pblocks = [b_ for b_ in token_blocks(CTX, SEQ, bs=256) if not (last and b_[2] == 0)]
            for (t0, n, seg) in pblocks:
                sg = 1 - seg
                ntile = n // 128
                k.dma(xbf[:, :, :n], xres[:, t0:t0 + n].rearrange("(c p) t -> p c t", p=128))
                modulate_block(h2, xbf, n, 1, sg, f"F{l}")
                for qc in range(16):
                    pq = pb[4 + qc % 4]
                    for dh in range(NCH):
                        k.mm(pq[:, :n], wqs[:, dh, qc * 128:(qc + 1) * 128], h2[:, dh, :n], start=(dh == 0), stop=(dh == NCH - 1))
                    k.copy('act' if qc % 2 else 'dve', qT[:, qc, :n], pq[:, :n])
                Ebs = [ssb, qT[:, :, :].bitcast(F32).rearrange("p q (a j) -> p (q a) j", j=128) if False else None]
                Ebs[1] = qTf
                for ti in range(ntile):
                    tsl = slice(ti * 128, (ti + 1) * 128)
                    Eb = Ebs[ti]
                    for qc in range(16):
                        k.mm(pb[qc // 4][:, (qc % 4) * 128:(qc % 4 + 1) * 128], qT[:, qc, tsl], skTs[:, qc, :])
                    for b4 in range(4):
                        k.copy('act' if b4 % 2 else 'dve', Eb[:, b4 * 4:(b4 + 1) * 4, :],
                               pb[b4][:, :].rearrange("p (q j) -> p q j", q=4))
                for ti in range(ntile):
                    Eb = Ebs[ti]
                    for qc in range(16):
                        k.max8(T16[:, qc, 0:8], Eb[:, qc, :])
                        k.match_replace(tmpk[:], T16[:, qc, 0:8], Eb[:, qc, :], -1e30)
                        k.max8(T16[:, qc, 8:16], tmpk[:])
                    T16v = T16[:, :, :].rearrange("p (h c) k -> p h c k", c=2)
                    for h in range(PH):
                        k.tt('pool', cand[:, :].rearrange("p (a b) -> p a b", a=16),
                             T16v[:, h, 0, :].unsqueeze(2).to_broadcast([128, 16, 16]),
                             T16v[:, h, 1, :].unsqueeze(1).to_broadcast([128, 16, 16]), ALU.add)
                        k.max8(c16[:, h, 0:8], cand[:, :])
                        k.match_replace(candt[:], c16[:, h, 0:8], cand[:, :], -1e30)
                        k.max8(c16[:, h, 8:16], candt[:])
                    k.tt('pool', smx[:], T16v[:, :, 0, 0], T16v[:, :, 1, 0], ALU.add)
                    k.tt('pool', w16[:], c16[:], smx[:, :].unsqueeze(2).to_broadcast([128, 8, 16]), ALU.subtract)
                    k.act(w16[:], w16[:], AF.Exp)
                    k.reduce('dve', Zs[:], w16[:], AX.X, ALU.add)
                    k.recip(rZ[:], Zs[:])
                    k.stt('pool', thn2[:, ti, :], w16[:, :, 15], 0.999, rZ[:], ALU.mult, ALU.mult)
                    k.ts('pool', negm[:], T16[:, :, 0], -1.0, ALU.mult)
                    for qc in range(16):
                        k.act(Eb[:, qc, :], Eb[:, qc, :], AF.Exp, bias=negm[:, qc:qc + 1])
                    for h in range(PH):
                        k.ts('pool', Eb[:, 2 * h, :], Eb[:, 2 * h, :], rZ[:, h:h + 1], ALU.mult)
                cnt_p = [0]

                def gbuild(sl16):
                    isl = slice(sl16 * 8, (sl16 + 1) * 8)
                    for ti in range(ntile):
                        Eb = Ebs[ti]
                        _ALIAS[Gt[ti][:, 0:1].tensor.name] = f"Gt{ti}_s{sl16}"
                        Gs = Gt[ti][:, sl16 * 1024:(sl16 + 1) * 1024].rearrange("p (i j) -> p i j", i=8)
                        for h in range(PH):
                            P_, Gh_ = Pq[cnt_p[0] % 2], Gh[cnt_p[0] % 2]
                            cnt_p[0] += 1
                            k.tt('pool' if cnt_p[0] % 5 in (0, 2, 4) else 'dve', P_[:],
                                 Eb[:, 2 * h, isl].unsqueeze(2).to_broadcast([128, 8, 128]),
                                 Eb[:, 2 * h + 1, :].unsqueeze(1).to_broadcast([128, 8, 128]), ALU.mult)
                            if h == 0:
                                k.stt('dve', Gs, P_[:], thn2[:, ti, h:h + 1], P_[:], ALU.is_ge, ALU.mult)
                            else:
                                k.stt('dve', Gh_[:], P_[:], thn2[:, ti, h:h + 1], P_[:], ALU.is_ge, ALU.mult)
                                k.tt('pool' if h in (2, 5, 7) else 'dve', Gs, Gs, Gh_[:], ALU.add)

                def eloop(sl16):
                    chunks = [(grp, ec) for grp in (2 * sl16, 2 * sl16 + 1) for ec in range(4)]

                    def stage1(grp, ec):
                        UT_, V_ = UTs[grp % 2], Vs[grp % 2]
                        if ec == 0:
                            k.dma(UT_[:], puTb[grp])
                            k.dma(V_[:], pvb[grp])
                        e = grp * 4 + ec
                        pa = pb[e % 2]
                        for dh in range(NCH):
                            k.mm(pa[:, :n], UT_[:, dh, ec * 128:(ec + 1) * 128], h2[:, dh, :n], start=(dh == 0), stop=(dh == NCH - 1))
                        ga_ = gab[e % 2]
                        k.act(ga_[:, :n], pa[:, :n], AF.Gelu)
                        pgb = pb[2 + e % 2][:, :].bitcast(BF16)
                        for ti in range(ntile):
                            _ALIAS[Gt[ti][:, 0:1].tensor.name] = f"Gt{ti}_s{sl16}"
                            k.tr(pgb[:, ti * 128:(ti + 1) * 128], Gt[ti][:, e * 128:(e + 1) * 128], identb[:])
                        WT_ = WT[e % 2]
                        k.tt('dve', WT_[:, :n], ga_[:, :n], pgb[:, :n], ALU.mult)

                    def stage2(grp, ec):
                        V_ = Vs[grp % 2]
                        e = grp * 4 + ec
                        WT_ = WT[e % 2]
                        for oc in range(NCH):
                            acc = pb[4 + oc // 2][:, (oc % 2) * 256:(oc % 2) * 256 + n]
                            k.mm(acc, V_[:, ec, oc * 128:(oc + 1) * 128], WT_[:, :n],
                                 start=(e == 0 and oc % 2 == 0), stop=(e == 127), skip=True)
                    stage1(*chunks[0])
                    for ci in range(len(chunks)):
                        if ci + 1 < len(chunks):
                            stage1(*chunks[ci + 1])
                        stage2(*chunks[ci])
                gbuild(0)
                gbuild(1)
                for sl16 in range(16):
                    eloop(sl16)
                    if sl16 + 2 < 16:
                        gbuild(sl16 + 2)
                for oc in range(NCH):
                    acc = pb[4 + oc // 2][:, (oc % 2) * 256:(oc % 2) * 256 + n]
                    k.stt('dve', xbf[:, oc, :n], acc, modT[:, 5 * 8 + oc, sg:sg + 1], xbf[:, oc, :n], ALU.mult, ALU.add)
                k.dma(xres[:, t0:t0 + n].rearrange("(c p) t -> p c t", p=128), xbf[:, :, :n], q='act')
            k.pop()
            if stop_after == 'F':
                break

        if stop_after is None:
            k.push()
            l = NL
            xfb = [k.sb(f"xfb{i}", [128, NCH, 512]) for i in range(2)]
            for bi, (t0, n, seg) in enumerate([b_ for b_ in blocks if b_[2] == 1]):
                xb_ = xfb[bi % 2]
                k.dma(xb_[:, :, :n], xres[:, t0:t0 + n].rearrange("(c p) t -> p c t", p=128))
                pss = pb[bi % 2]
                for c in range(NCH):
                    k.act(sqs[:, :n], xb_[:, c, :n], AF.Square)
                    k.mm(pss[:, :n], onesD, sqs[:, :n], start=(c == 0), stop=(c == NCH - 1))
                k.act(rstd[:, :n], pss[:, :n], AF.Sqrt, bias=epsn[:, 0:1])
                k.recip(rstd[:, :n], rstd[:, :n])
                for c in range(NCH):
                    k.stt(k.ve(), xb_[:, c, :n], xb_[:, c, :n], V('final_g', c), rstd[:, :n], ALU.mult, ALU.mult)
                k.dma(outT[:, t0 - CTX:t0 - CTX + n].rearrange("(c p) t -> p c t", p=128), xb_[:, :, :n], q='act')
            k.pop()
        S.finish()
        S.emit()
        nc._dbg = dbg
        nc._nops = S.nops
    return nc


def _fm(v):
    v = np.asarray(v, np.float32).reshape(-1, 128)
    return np.ascontiguousarray(v.T)


def make_consts():
    c = np.zeros((128, 10, 128), np.float32)
    i = np.arange(128)[:, None]
    t = np.arange(128)[None, :]
    same = (i // 64) == (t // 64)
    c[:, 0, :] = (i == t)
    c[:, 1, :] = same
    c[:, 2, :] = 1.0 / D
    c[:, 3, :] = same & (i < t)
    c[:, 4, :] = same & (i <= t)
    c[:, 5, :] = same & (i > t)
    c[:, 6, :] = same & (i >= t)
    c[:, 7, :] = ((i % 64) == t)
    c[:, 8, :] = same / 64.0
    return c


def prepare_shared(inp, NL):
    f32 = lambda a: np.asarray(a, np.float32)
    sh = {}
    ada_w = f32(inp['ada_w'])
    sh['ada_w'] = np.ascontiguousarray(ada_w.reshape(NL, 8, 128, 48, 128).transpose(0, 3, 2, 1, 4))
    vec = np.zeros((NL, 128, NV), np.float32)

    def put(l, name, arr):
        o, w = VEC[name]
        assert arr.shape == (128, w), (name, arr.shape, w)
        vec[l, :, o:o + w] = arr
    for l in range(NL):
        for nme in ('norm1_g', 'norm2_g', 'k_k', 'k_a', 'lnx_g', 'lnx_b', 'cnorm_g', 'cnorm_b', 'gate_b'):
            put(l, nme, _fm(f32(inp[nme])[l]))
        put(l, 'r_k', _fm(f32(inp['r_k'])[l].reshape(-1)))
        put(l, 'w0', _fm(f32(inp['w0'])[l].reshape(-1)))
        put(l, 'a0', _fm(f32(inp['a0'])[l].reshape(-1)))
        put(l, 'final_g', _fm(f32(inp['final_g'])))
        mu = np.zeros((2, 3584), np.float32)
        mu[:, :3488] = f32(inp['shift_mu'])[l]
        put(l, 'mu0', _fm(mu[0]))
        put(l, 'mu1', _fm(mu[1]))
        put(l, 'ada_b', _fm(f32(inp['ada_b'])[l]))
        cw = f32(inp['conv_w'])[l]
        put(l, 'conv_w', np.ascontiguousarray(cw.T.reshape(8, 128, CONV_K).transpose(1, 0, 2)).reshape(128, 8 * CONV_K))
    sh['vec'] = vec
    w_in = f32(inp['w_in'])
    wp = np.zeros((NL, D, P_IN_PAD), np.float32)
    wp[:, :, :3488] = w_in[:, :, :3488]
    wp[:, :, 3584:] = w_in[:, :, 3488:]
    sh['w_in'] = np.ascontiguousarray(wp.reshape(NL, 8, 128, NFC, 128).transpose(0, 3, 2, 1, 4))
    sh['w2'] = np.ascontiguousarray(f32(inp['w2']).reshape(NL, 128, D))
    sh['a2'] = np.ascontiguousarray(f32(inp['a2']).reshape(NL, 128, D))
    g2p = np.zeros((NL, 256, D), np.float32)
    g2p[:, :LORA_G] = f32(inp['g2'])
    sh['g2'] = g2p.reshape(NL, 2, 128, D)
    for nme in ('w_oA', 'w_oB', 'w_out'):
        sh[nme] = np.ascontiguousarray(f32(inp[nme]).reshape(NL, 8, 128, D).transpose(0, 2, 1, 3))
    sh['w_q'] = np.ascontiguousarray(f32(inp['w_q']).reshape(NL, 8, 128, 2 * D).transpose(0, 2, 1, 3))
    sh['skT'] = np.ascontiguousarray(f32(inp['sub_keys']).reshape(NL, 16, 128, 128).transpose(0, 1, 3, 2))
    sh['puT'] = np.ascontiguousarray(f32(inp['peer_u']).reshape(NL, 32, 512, 8, 128).transpose(0, 1, 4, 3, 2))
    sh['pv'] = np.ascontiguousarray(f32(inp['peer_v']).reshape(NL, 32, 4, 128, D).transpose(0, 1, 3, 2, 4))
    sh['consts'] = make_consts()
    return sh


def prepare_core(inp, b):
    f32 = lambda a: np.asarray(a, np.float32)
    xT = np.ascontiguousarray(np.concatenate([f32(inp['ctx'])[b], f32(inp['x'])[b]], axis=0).T)
    cc = np.stack([_fm(f32(inp['c'])[b]), _fm(f32(inp['c_ctx']))], axis=-1)
    return {'xT': xT, 'cc': np.ascontiguousarray(cc)}


def kernel(**inputs):
    B, SEQ, _ = inputs['x'].shape
    CTX = inputs['ctx'].shape[1]
    NL = inputs['w_in'].shape[0]
    cfg = dict(CTX=CTX, SEQ=SEQ, NL=NL)
    nc = build(cfg)
    sh = prepare_shared(inputs, NL)
    in_maps = []
    for b in range(B):
        m = dict(sh)
        m.update(prepare_core(inputs, b))
        in_maps.append(m)
    res = run_bass_kernel_spmd(nc, in_maps, core_ids=list(range(B)))
    out = np.stack([np.ascontiguousarray(np.asarray(r['outT']).T) for r in res.results], axis=0)
    return out.astype(np.float32)
```
